# Optimizing a Trainium2 kernel written in Bass

```python
import math
import jax
import jax.numpy as jnp
from jax import lax
import numpy as np

D_MODEL = 1024
BATCH = 4
SEQ = 4096
DEPTH = 4

N_A_LAYERS = DEPTH // 2
N_B_LAYERS = DEPTH - N_A_LAYERS
N_DENSE = (DEPTH + 1) // 2
N_MOE = DEPTH // 2

CONV_K = 31

N_HEADS = 16
HEAD_DIM = D_MODEL // N_HEADS
MOBA_BLOCK = 256
MOBA_TOPK = 3
Q_CHUNK = 32

D_FF = ((8 * D_MODEL // 3 + 255) // 256) * 256
N_EXPERTS = 8
TOP_K_EXPERTS = 2
D_FF_EXPERT = 7 * D_MODEL // 2

ALPHA = (2.0 * DEPTH) ** 0.25
BETA = (8.0 * DEPTH) ** -0.25

LN_EPS = 1e-5
NEG_INF = -1e30
ADA_INIT = 0.5

kernel_name = "yoco_conformer_moba_deepnorm_moe"


def layer_norm(x, g, b):
    xf = x.astype(jnp.float32)
    mu = jnp.mean(xf, axis=-1, keepdims=True)
    xc = xf - mu
    var = jnp.mean(xc * xc, axis=-1, keepdims=True)
    y = xc * lax.rsqrt(var + LN_EPS)
    return (y * g.astype(jnp.float32) + b.astype(jnp.float32)).astype(x.dtype)


def alibi_slopes(n_heads):
    return jnp.exp2(-8.0 * jnp.arange(1, n_heads + 1, dtype=jnp.float32) / n_heads)


def swiglu(h, w13, w2):
    a, b = jnp.split(h @ w13, 2, axis=-1)
    return (jax.nn.silu(a) * b) @ w2


def conv_module(h, w_in, b_in, w_dw, b_dw, g_n, b_n, w_out, b_out):
    a, g = jnp.split(h @ w_in + b_in, 2, axis=-1)
    u = a * jax.nn.sigmoid(g)
    u = lax.conv_general_dilated(
        u, w_dw[:, None, :], window_strides=(1,), padding=[(CONV_K - 1, 0)],
        dimension_numbers=("NWC", "WIO", "NWC"),
        feature_group_count=u.shape[-1]) + b_dw
    u = jax.nn.silu(layer_norm(u, g_n, b_n))
    return u @ w_out + b_out


def moe_swiglu(h, router_w, router_b, w13, w2):
    logits = (h @ router_w).astype(jnp.float32) + router_b.astype(jnp.float32)
    top_val, top_idx = lax.top_k(logits, TOP_K_EXPERTS)
    top_w = jax.nn.softmax(top_val, axis=-1)
    gates = jnp.sum(jax.nn.one_hot(top_idx, N_EXPERTS, dtype=jnp.float32) * top_w[..., None], axis=-2)
    gates = gates.astype(h.dtype)
    y = jnp.zeros_like(h)
    for e in range(N_EXPERTS):
        y = y + gates[..., e:e + 1] * swiglu(h, w13[e], w2[e])
    return y


def shared_kv(h, w_kv):
    B, S, _ = h.shape
    n_blk = -(-S // MOBA_BLOCK)
    pad = n_blk * MOBA_BLOCK - S
    k, v = jnp.split(h @ w_kv, 2, axis=-1)
    k = k.reshape(B, S, N_HEADS, HEAD_DIM).transpose(0, 2, 1, 3)
    v = v.reshape(B, S, N_HEADS, HEAD_DIM).transpose(0, 2, 1, 3)
    k_pad = jnp.pad(k, ((0, 0), (0, 0), (0, pad), (0, 0)))
    v_pad = jnp.pad(v, ((0, 0), (0, 0), (0, pad), (0, 0)))
    k_mean = jnp.mean(k_pad.reshape(B, N_HEADS, n_blk, MOBA_BLOCK, HEAD_DIM).astype(jnp.float32), axis=3)
    return k_pad, v_pad, k_mean


def moba_attention(q, k_pad, v_pad, k_mean, slopes):
    B, S, H, Dh = q.shape
    n_blk = k_mean.shape[2]
    topk = min(MOBA_TOPK, n_blk)
    kb = k_pad.reshape(B, H, n_blk, MOBA_BLOCK, Dh)
    vb = v_pad.reshape(B, H, n_blk, MOBA_BLOCK, Dh)
    n_chunk = S // Q_CHUNK
    q_chunks = q.transpose(0, 2, 1, 3).reshape(B, H, n_chunk, Q_CHUNK, Dh).transpose(2, 0, 1, 3, 4)
    scale = Dh ** -0.5
    b_idx = jnp.arange(B)[:, None, None, None]
    h_idx = jnp.arange(H)[None, :, None, None]
    blk_pos = jnp.arange(MOBA_BLOCK)

    def chunk(args):
        ci, qc = args
        start = ci * Q_CHUNK
        blk = start // MOBA_BLOCK
        t = start + jnp.arange(Q_CHUNK)
        gate = jnp.einsum("bhqd,bhnd->bhqn", qc.astype(jnp.float32), k_mean)
        gate = jnp.where(jnp.arange(n_blk) < blk, gate, -jnp.inf)
        _, idx = lax.top_k(gate, topk)
        valid = idx < blk
        k_sel = kb[b_idx, h_idx, idx]
        v_sel = vb[b_idx, h_idx, idx]
        dist_sel = (t[:, None, None] - (idx[..., None] * MOBA_BLOCK + blk_pos)).astype(jnp.float32)
        s_sel = jnp.einsum("bhqd,bhqnkd->bhqnk", qc, k_sel).astype(jnp.float32) * scale
        s_sel = s_sel - slopes[:, None, None, None] * dist_sel
        s_sel = jnp.where(valid[..., None], s_sel, NEG_INF)
        k_own = lax.dynamic_slice_in_dim(k_pad, blk * MOBA_BLOCK, MOBA_BLOCK, axis=2)
        v_own = lax.dynamic_slice_in_dim(v_pad, blk * MOBA_BLOCK, MOBA_BLOCK, axis=2)
        dist_own = t[:, None] - (blk * MOBA_BLOCK + blk_pos)[None, :]
        s_own = jnp.einsum("bhqd,bhkd->bhqk", qc, k_own).astype(jnp.float32) * scale
        s_own = s_own - slopes[:, None, None] * dist_own.astype(jnp.float32)
        s_own = jnp.where(dist_own >= 0, s_own, NEG_INF)
        scores = jnp.concatenate([s_sel.reshape(B, H, Q_CHUNK, topk * MOBA_BLOCK), s_own], axis=-1)
        p = jax.nn.softmax(scores, axis=-1).astype(v_pad.dtype)
        p_sel = p[..., :topk * MOBA_BLOCK].reshape(B, H, Q_CHUNK, topk, MOBA_BLOCK)
        p_own = p[..., topk * MOBA_BLOCK:]
        return (jnp.einsum("bhqnk,bhqnkd->bhqd", p_sel, v_sel)
                + jnp.einsum("bhqk,bhkd->bhqd", p_own, v_own))

    outs = lax.map(chunk, (jnp.arange(n_chunk), q_chunks))
    return outs.transpose(1, 0, 3, 2, 4).reshape(B, S, H * Dh)


def setup_inputs(seed: int = 0) -> dict:
    key = jax.random.key(seed)
    ks = jax.random.split(key, 32)
    D = D_MODEL

    def nrm(k, shape, std):
        return jax.random.normal(k, shape, dtype=jnp.float32) * std

    return {
        "x": nrm(ks[0], (BATCH, SEQ, D), 1.0),
        "c": nrm(ks[1], (BATCH, D), 1.0),
        "ada_w": nrm(ks[2], (DEPTH, D, 6 * D), ADA_INIT * D ** -0.5),
        "ada_b": nrm(ks[3], (DEPTH, 6 * D), 0.01),
        "ln_g": 1.0 + nrm(ks[4], (DEPTH, 2, D), 0.05),
        "ln_b": nrm(ks[5], (DEPTH, 2, D), 0.01),
        "conv_in_w": nrm(ks[6], (N_A_LAYERS, D, 2 * D), D ** -0.5),
        "conv_in_b": nrm(ks[7], (N_A_LAYERS, 2 * D), 0.01),
        "conv_dw_w": nrm(ks[8], (N_A_LAYERS, CONV_K, D), CONV_K ** -0.5),
        "conv_dw_b": nrm(ks[9], (N_A_LAYERS, D), 0.01),
        "conv_norm_g": 1.0 + nrm(ks[10], (N_A_LAYERS, D), 0.05),
        "conv_norm_b": nrm(ks[11], (N_A_LAYERS, D), 0.01),
        "conv_out_w": nrm(ks[12], (N_A_LAYERS, D, D), BETA * D ** -0.5),
        "conv_out_b": nrm(ks[13], (N_A_LAYERS, D), 0.01),
        "kv_ada_w": nrm(ks[14], (D, 2 * D), ADA_INIT * D ** -0.5),
        "kv_ada_b": nrm(ks[15], (2 * D,), 0.01),
        "w_kv": nrm(ks[16], (D, 2 * D), D ** -0.5),
        "w_q": nrm(ks[17], (N_B_LAYERS, D, D), D ** -0.5),
        "w_o": nrm(ks[18], (N_B_LAYERS, D, D), BETA * D ** -0.5),
        "ffn_w13": nrm(ks[19], (N_DENSE, D, 2 * D_FF), D ** -0.5),
        "ffn_w2": nrm(ks[20], (N_DENSE, D_FF, D), BETA * D_FF ** -0.5),
        "router_w": nrm(ks[21], (N_MOE, D, N_EXPERTS), D ** -0.5),
        "router_b": nrm(ks[22], (N_MOE, N_EXPERTS), 0.01),
        "moe_w13": nrm(ks[23], (N_MOE, N_EXPERTS, D, 2 * D_FF_EXPERT), D ** -0.5),
        "moe_w2": nrm(ks[24], (N_MOE, N_EXPERTS, D_FF_EXPERT, D), BETA * D_FF_EXPERT ** -0.5),
    }


def reference(x, c, ada_w, ada_b, ln_g, ln_b, conv_in_w, conv_in_b, conv_dw_w, conv_dw_b,
              conv_norm_g, conv_norm_b, conv_out_w, conv_out_b, kv_ada_w, kv_ada_b, w_kv,
              w_q, w_o, ffn_w13, ffn_w2, router_w, router_b, moe_w13, moe_w2):
    B, S, D = x.shape
    slopes = alibi_slopes(N_HEADS)
    c_act = jax.nn.silu(c)
    k_pad = v_pad = k_mean = None
    for l in range(DEPTH):
        if l == N_A_LAYERS:
            kv_shift, kv_scale = jnp.split(c_act @ kv_ada_w + kv_ada_b, 2, axis=-1)
            h_kv = x * (1.0 + kv_scale[:, None, :]) + kv_shift[:, None, :]
            k_pad, v_pad, k_mean = shared_kv(h_kv, w_kv)
        sh1, sc1, g1, sh2, sc2, g2 = jnp.split(c_act @ ada_w[l] + ada_b[l], 6, axis=-1)
        h = x * (1.0 + sc1[:, None, :]) + sh1[:, None, :]
        if l < N_A_LAYERS:
            out = conv_module(h, conv_in_w[l], conv_in_b[l], conv_dw_w[l], conv_dw_b[l],
                              conv_norm_g[l], conv_norm_b[l], conv_out_w[l], conv_out_b[l])
        else:
            j = l - N_A_LAYERS
            q = (h @ w_q[j]).reshape(B, S, N_HEADS, HEAD_DIM)
            out = moba_attention(q, k_pad, v_pad, k_mean, slopes) @ w_o[j]
        x = layer_norm(ALPHA * x + (1.0 + g1[:, None, :]) * out, ln_g[l, 0], ln_b[l, 0])
        h = x * (1.0 + sc2[:, None, :]) + sh2[:, None, :]
        if l % 2 == 0:
            f = swiglu(h, ffn_w13[l // 2], ffn_w2[l // 2])
        else:
            f = moe_swiglu(h, router_w[l // 2], router_b[l // 2], moe_w13[l // 2], moe_w2[l // 2])
        x = layer_norm(ALPHA * x + (1.0 + g2[:, None, :]) * f, ln_g[l, 1], ln_b[l, 1])
    return x
```

```python
import numpy as np
import ml_dtypes
import concourse.bass as bass
import concourse.mybir as mybir
from concourse.bass_utils import run_bass_kernel_spmd

F32 = mybir.dt.float32
BF16 = mybir.dt.bfloat16
ALU = mybir.AluOpType
AF = mybir.ActivationFunctionType
AX = mybir.AxisListType

D = 1024
SEQ = 4096
NB = 4
NCORES = 8
TOK = 2048
NT = 16
HALO = 64
H = 16
DH = 64
BLK = 256
DEPTH = 4
NA = 2
FD = 2816
FE = 3584
NE = 8
CK = 31
ALPHA = (2.0 * DEPTH) ** 0.25
EPS = 1e-5
BIG = 30000.0

ENGS = ["pe", "act", "dve", "pool", "sp"]


class Res:
    __slots__ = ("w", "r")

    def __init__(self):
        self.w = None
        self.r = {}


class Prog:
    def __init__(self):
        self.q = {e: [] for e in ENGS}
        self.cnt = {}
        self.seen = {e: {} for e in ENGS}
        self.names = []

    def _sem(self, name):
        if name not in self.cnt:
            self.cnt[name] = 0
            self.names.append(name)

    def _wait(self, eng, deps):
        for name, val in deps:
            if self.seen[eng].get(name, 0) < val:
                self.seen[eng][name] = val
                self.q[eng].append((0, name, val))

    @staticmethod
    def _collect(reads, writes):
        deps = []
        for r in reads:
            if r.w is not None:
                deps.append(r.w)
        for w in writes:
            if w.w is not None:
                deps.append(w.w)
            deps.extend(w.r.items())
        return deps

    @staticmethod
    def _mark(t, reads, writes):
        for r in reads:
            if r.r.get(t[0], 0) < t[1]:
                r.r[t[0]] = t[1]
        for w in writes:
            w.w = t
            w.r = {}

    def op(self, eng, fns, reads=(), writes=(), noself=False):
        if isinstance(fns, tuple):
            fns = [fns]
        deps = self._collect(reads, writes)
        if noself:
            deps = [d for d in deps if d[0] != "c_" + eng]
        self._wait(eng, deps)
        name = "c_" + eng
        self._sem(name)
        self.cnt[name] += 1
        t = (name, self.cnt[name])
        self.q[eng].append((1, fns, name, 1))
        self._mark(t, reads, writes)
        return t

    def dma(self, eng, fn, sem, reads=(), writes=()):
        self._wait(eng, self._collect(reads, writes))
        self._sem(sem)
        self.cnt[sem] += 16
        t = (sem, self.cnt[sem])
        self.q[eng].append((1, [fn], sem, 16))
        self._mark(t, reads, writes)
        return t

    def barrier(self):
        allt = [(n, self.cnt[n]) for n in self.names if self.cnt[n] > 0]
        for e in ENGS:
            self._wait(e, allt)

    def wait_on(self, eng, tickets):
        self._wait(eng, tickets)


def I(name, *a, **k):
    return (name, a, k)


def _replay(e, items, sems):
    for it in items:
        if it[0] == 0:
            e.wait_ge(sems[it[1]], it[2])
        else:
            fns = it[1]
            for f in fns[:-1]:
                getattr(e, f[0])(*f[1], **f[2])
            f = fns[-1]
            getattr(e, f[0])(*f[1], **f[2]).then_inc(sems[it[2]], it[3])


class Builder:
    def __init__(self, part):
        self.part = part
        self.nc = bass.Bass("TRN2", target_bir_lowering=False)
        self.P = Prog()
        self.dram = {}
        self.dmasem_i = 0

    def din(self, name, shape, dt=F32):
        t = self.nc.dram_tensor(name, list(shape), dt, kind="ExternalInput").ap()
        self.dram[name] = t
        return t

    def dout(self, name, shape, dt=F32):
        t = self.nc.dram_tensor(name, list(shape), dt, kind="ExternalOutput").ap()
        self.dram[name] = t
        return t

    def arena_reset(self):
        self.P.barrier()
        self.aoff = 0

    def alloc(self, shape, dt):
        n = int(np.prod(shape[1:]))
        nb = n * (4 if dt == F32 else 2)
        nb = (nb + 63) // 64 * 64
        off = self.aoff
        self.aoff += nb // 2
        assert self.aoff <= self.arena_elems, f"arena overflow {self.aoff*2} > {self.arena_elems*2}"
        self.last_off = off
        return self.view(off, shape, dt)

    def view(self, off, shape, dt):
        n = int(np.prod(shape[1:]))
        nb = n * (4 if dt == F32 else 2)
        nb = (nb + 63) // 64 * 64
        v = self.arena[:, off:off + nb // 2]
        if dt == F32:
            v = v.bitcast(F32)
        v = v[:, 0:n]
        if len(shape) == 3:
            v = v.rearrange("p (a b) -> p a b", b=shape[2])
        elif len(shape) == 4:
            v = v.rearrange("p (a b c) -> p a b c", b=shape[2], c=shape[3])
        return v

    def build(self):
        nc = self.nc
        from contextlib import ExitStack
        with ExitStack() as es:
            def sb(name, shape, dt):
                return es.enter_context(nc.sbuf_tensor(name, list(shape), dt))

            def ps(name, shape, dt):
                return es.enter_context(nc.psum_tensor(name, list(shape), dt))

            self.x_tm = sb("x_tm", [128, NT + 1, D], F32)
            self.h_fm = sb("h_fm", [128, 8, TOK + HALO], BF16)
            self.bc = sb("bc", [128, 6, D], F32)
            self.ident = sb("ident", [128, 128], BF16)
            self.ones = sb("ones", [128, 128], BF16)
            self.cbc = sb("cbc", [128, 8, 128], BF16)
            self.cfm = sb("cfm_sb", [128, 16], F32)
            self.small = sb("small", [128, 512], F32)
            self.gates = sb("gates", [128, NT, NE], F32)
            self.hmask = sb("hmask_sb", [128, 2], F32)
            self.arena_elems = 38 * 1024
            self.arena = sb("arena", [128, self.arena_elems], BF16)
            self.aoff = 0
            self.psA = [ps(f"psA{i}", [128, 512], F32) for i in range(4)]
            self.psO = [ps(f"psO{i}", [128, 512], F32) for i in range(2)]
            self.psT = [ps(f"psT{i}", [128, 8, 128], BF16) for i in range(2)]
            self.rA = [Res() for _ in range(4)]
            self.rO = [Res() for _ in range(2)]
            self.rT = [Res() for _ in range(2)]
            self.rX = [Res() for _ in range(NT + 1)]
            self.rH = [Res() for _ in range(NT + 1)]
            self.rBC = [Res() for _ in range(6)]
            self.rConst = Res()
            self.rCbc = Res()
            self.rGates = Res()
            self.rSmall = Res()
            self.iA = 0
            self.iO = 0
            self.iT = 0
            self.out_tickets = []

            if self.part == "A":
                self.emit_A()
            elif self.part == "B":
                self.emit_B()
            else:
                self.emit_F()

            P = self.P
            P.wait_on("sp", self.out_tickets)
            sems = {n: es.enter_context(nc.semaphore(n)) for n in P.names}
            with nc.Block() as block:
                @block.tensor
                def _(e):
                    _replay(e, P.q["pe"], sems)

                @block.scalar
                def _(e):
                    _replay(e, P.q["act"], sems)

                @block.vector
                def _(e):
                    _replay(e, P.q["dve"], sems)

                @block.gpsimd
                def _(e):
                    _replay(e, P.q["pool"], sems)

                @block.sync
                def _(e):
                    _replay(e, P.q["sp"], sems)
        return nc

    def tile_np(self, t):
        return HALO if t == NT else 128

    def tok0(self, t):
        return TOK if t == NT else t * 128

    def nextA(self):
        i = self.iA
        self.iA = (i + 1) % 4
        return self.psA[i], self.rA[i]

    def nextO(self):
        i = self.iO
        self.iO = (i + 1) % 2
        return self.psO[i], self.rO[i]

    def nextT(self):
        i = self.iT
        self.iT = (i + 1) % 2
        return self.psT[i], self.rT[i]

    def dsem(self, base):
        return "d_" + base

    def load_x(self, x_in):
        for t in range(NT + 1):
            self.P.dma("sp", I("dma_start", out=self.x_tm[:, t, :], in_=x_in[t]), "d_x", writes=[self.rX[t]])

    def setup_common(self, load_x=True):
        P = self.P
        x_in = self.din("xin", [NT + 1, 128, D])
        ident_in = self.din("ident_in", [128, 128], BF16)
        cfm_in = self.din("cfm", [128, 8])
        hm_in = self.din("hmask", [128, 1])
        self.adaw = self.din("adaw", [26, 128, 8, D])
        self.adab = self.din("adab", [26, D])
        self.lng = self.din("lng", [8, D])
        self.lnb = self.din("lnb", [8, D])
        if load_x:
            self.load_x(x_in)
        P.dma("sp", I("dma_start", out=self.ident[:], in_=ident_in), "d_c", writes=[self.rConst])
        P.dma("sp", I("dma_start", out=self.cfm[:, 0:8], in_=cfm_in), "d_c", writes=[self.rSmall])
        P.dma("sp", I("dma_start", out=self.hmask[:, 0:1], in_=hm_in), "d_c", writes=[self.rConst])
        P.op("pool", I("memset", self.hmask[:, 1:2], 0.0), writes=[self.rConst])
        self.hm_ap = self.hmask[:, 0:1]
        P.op("pool", I("memset", self.ones[:], 1.0), writes=[self.rConst])
        P.op("act", I("activation", out=self.cfm[:, 8:16], in_=self.cfm[:, 0:8], func=AF.Silu),
             reads=[self.rSmall], writes=[self.rSmall])
        P.op("dve", I("tensor_copy", out=self.cbc[:], in_=self.cfm[:, 8:16].unsqueeze(2).to_broadcast([128, 8, 128])),
             reads=[self.rSmall], writes=[self.rCbc])

    def ada_vec(self, idx, slot, plus_one, wbuf, rW):
        P = self.P
        wide = wbuf.shape[2] >= 1024
        P.dma("sp", I("dma_start", out=self.bc[:, slot, :], in_=self.adab[idx:idx + 1, :].partition_broadcast(128)),
              "d_adab", writes=[self.rBC[slot]])
        for n in range(2):
            wb = wbuf[:, :, n * 512:(n + 1) * 512] if wide else wbuf[:, :, 0:512]
            P.dma("pool", I("dma_start", out=wb, in_=self.adaw[idx][:, :, n * 512:(n + 1) * 512]), "d_adaw", writes=[rW])
            pt, rp = self.nextA()
            fns = [I("matmul", pt[:], self.cbc[:, k, :], wb[:, k, :], start=(k == 0), stop=(k == 7)) for k in range(8)]
            P.op("pe", fns, reads=[rW, self.rCbc], writes=[rp])
            P.op("dve", I("scalar_tensor_tensor",
                out=self.bc[:, slot, n * 512:(n + 1) * 512], in0=pt[:], scalar=(1.0 if plus_one else 0.0),
                in1=self.bc[:, slot, n * 512:(n + 1) * 512], op0=ALU.add, op1=ALU.add),
                reads=[rp], writes=[self.rBC[slot]])

    def load_row_bc(self, src_row_ap, slot):
        self.P.dma("sp", I("dma_start", out=self.bc[:, slot, :], in_=src_row_ap.partition_broadcast(128)),
                   "d_row", writes=[self.rBC[slot]])

    def h_tile(self, t, Gs, Bs, tmpf, rtmpf, hb, rhb):
        P = self.P
        npart = self.tile_np(t)
        c0 = self.tok0(t)
        P.op("dve", I("tensor_tensor", out=tmpf[0:npart, :], in0=self.x_tm[0:npart, t, :], in1=self.bc[0:npart, Gs, :], op=ALU.mult),
             reads=[self.rX[t], self.rBC[Gs]], writes=[rtmpf])
        P.op("dve", I("tensor_tensor", out=hb[0:npart, :], in0=tmpf[0:npart, :], in1=self.bc[0:npart, Bs, :], op=ALU.add),
             reads=[rtmpf, self.rBC[Bs]], writes=[rhb])
        pt, rp = self.nextT()
        fns = [(I("transpose", pt[:, k, 0:npart], hb[0:npart, k * 128:(k + 1) * 128], self.ident[0:npart, 0:npart]))
               for k in range(8)]
        P.op("pe", fns, reads=[rhb, self.rConst], writes=[rp])
        P.op("act", I("activation", out=self.h_fm[:, :, c0:c0 + npart], in_=pt[:, :, 0:npart], func=AF.Copy),
             reads=[rp], writes=[self.rH[t]])

    def ln_tiles(self, tiles, st, rst, tmps):
        P = self.P
        for i, t in enumerate(tiles):
            npart = self.tile_np(t)
            s = st[:, t, :]
            P.op("dve", I("bn_stats", out=s[0:npart, 0:6], in_=self.x_tm[0:npart, t, 0:512]),
                 reads=[self.rX[t]], writes=[rst[t]])
            P.op("dve", I("bn_stats", out=s[0:npart, 6:12], in_=self.x_tm[0:npart, t, 512:1024]),
                 reads=[self.rX[t]], writes=[rst[t]])
            P.op("dve", I("bn_aggr", out=s[0:npart, 12:14], in_=s[0:npart, 0:12]),
                 reads=[rst[t]], writes=[rst[t]])
            P.op("dve", I("tensor_scalar", out=s[0:npart, 14:15], in0=s[0:npart, 13:14], scalar1=EPS, scalar2=None, op0=ALU.add),
                 reads=[rst[t]], writes=[rst[t]])
            P.op("act", I("activation", out=s[0:npart, 15:16], in_=s[0:npart, 14:15], func=AF.Sqrt),
                 reads=[rst[t]], writes=[rst[t]])
            P.op("dve", I("reciprocal", out=s[0:npart, 16:17], in_=s[0:npart, 15:16]),
                 reads=[rst[t]], writes=[rst[t]])
            P.op("dve", I("scalar_tensor_tensor", out=s[0:npart, 17:18], in0=s[0:npart, 12:13], scalar=-1.0, in1=s[0:npart, 16:17],
                          op0=ALU.mult, op1=ALU.mult), reads=[rst[t]], writes=[rst[t]])
            P.op("act", I("activation", out=self.x_tm[0:npart, t, :], in_=self.x_tm[0:npart, t, :], func=AF.Identity,
                          scale=s[0:npart, 16:17], bias=s[0:npart, 17:18]), reads=[rst[t], self.rX[t]], writes=[self.rX[t]])
            tmpf, rtmpf, hb, rhb = tmps[i % 2]
            self.h_tile(t, 2, 3, tmpf, rtmpf, hb, rhb)
            P.op("dve", I("tensor_tensor", out=self.x_tm[0:npart, t, :], in0=self.x_tm[0:npart, t, :], in1=self.bc[0:npart, 0, :], op=ALU.mult),
                 reads=[self.rX[t], self.rBC[0]], writes=[self.rX[t]])
            P.op("dve", I("tensor_tensor", out=self.x_tm[0:npart, t, :], in0=self.x_tm[0:npart, t, :], in1=self.bc[0:npart, 1, :], op=ALU.add),
                 reads=[self.rX[t], self.rBC[1]], writes=[self.rX[t]])

    def ln_prep(self, ln_idx, sc_idx, sh_idx, wbuf, rW):
        P = self.P
        self.load_row_bc(self.lng[ln_idx:ln_idx + 1, :], 0)
        self.load_row_bc(self.lnb[ln_idx:ln_idx + 1, :], 1)
        if sc_idx is None:
            P.op("pool", I("tensor_copy", out=self.bc[:, 2, :], in_=self.bc[:, 0, :]), reads=[self.rBC[0]], writes=[self.rBC[2]])
            P.op("pool", I("tensor_copy", out=self.bc[:, 3, :], in_=self.bc[:, 1, :]), reads=[self.rBC[1]], writes=[self.rBC[3]])
            return
        self.ada_vec(sc_idx, 2, True, wbuf, rW)
        self.ada_vec(sh_idx, 3, False, wbuf, rW)
        P.op("pool", I("tensor_tensor", out=self.bc[:, 5, :], in0=self.bc[:, 1, :], in1=self.bc[:, 2, :], op=ALU.mult),
             reads=[self.rBC[1], self.rBC[2]], writes=[self.rBC[5]])
        P.op("pool", I("tensor_tensor", out=self.bc[:, 3, :], in0=self.bc[:, 3, :], in1=self.bc[:, 5, :], op=ALU.add),
             reads=[self.rBC[3], self.rBC[5]], writes=[self.rBC[3]])
        P.op("pool", I("tensor_tensor", out=self.bc[:, 2, :], in0=self.bc[:, 0, :], in1=self.bc[:, 2, :], op=ALU.mult),
             reads=[self.rBC[0], self.rBC[2]], writes=[self.rBC[2]])

    def mk_tmps(self):
        tf, rtf = self.alloc([128, D], F32), Res()
        return [(tf, rtf, self.alloc([128, D], BF16), Res()) for _ in range(2)]

    def scale_x(self, tiles):
        for t in tiles:
            npart = self.tile_np(t)
            self.P.op("act", I("activation", out=self.x_tm[0:npart, t, :], in_=self.x_tm[0:npart, t, :], func=AF.Copy, scale=ALPHA),
                      reads=[self.rX[t]], writes=[self.rX[t]])

    def ffn_phase(self, experts, FT, n_ft, groups, g_idx, ln_idx, next_sc, next_sh, router=None):
        P = self.P
        self.arena_reset()
        fs = FT * 128
        W13 = [self.alloc([128, 8, 1024], BF16) for _ in range(2)]
        W2 = [self.alloc([128, 4, D], BF16) for _ in range(2)]
        rW13 = [Res() for _ in range(2)]
        rW2 = [Res() for _ in range(2)]
        hid = [self.alloc([128, 4, 512], BF16) for _ in range(2)]
        rhid = [Res() for _ in range(2)]
        sil = [self.alloc([128, 512], BF16) for _ in range(2)]
        rsil = [Res() for _ in range(2)]
        st = self.alloc([128, NT + 1, 32], F32)
        rst = [Res() for _ in range(NT + 1)]
        tmps = self.mk_tmps()
        all_tiles = [t for g in groups for t in g[2]]
        self.ln_prep(ln_idx, next_sc, next_sh, W13[1], rW13[1])
        self.ada_vec(g_idx, 4, True, W13[0], rW13[0])
        if router is not None:
            self.moe_router(router, W13[1], rW13[1], groups)
        self.scale_x(all_tiles)
        items = [(ei, ft, g) for ei in range(len(experts)) for ft in range(n_ft) for g in groups]
        slot_of = {}
        it_w = 0

        def load_w(ei, ft):
            nonlocal it_w
            s = it_w % 2
            it_w += 1
            w13d, w2d, _ = experts[ei]
            P.dma("pool", I("dma_start", out=W13[s][:, :, 0:2 * fs], in_=w13d[ft]), f"d_w13_{s}", writes=[rW13[s]])
            P.dma("pool", I("dma_start", out=W2[s][:, 0:FT, :], in_=w2d[ft]), f"d_w2_{s}", writes=[rW2[s]])
            P.op("pool", I("tensor_tensor", out=W2[s][:, 0:FT, :], in0=W2[s][:, 0:FT, :],
                                                   in1=self.bc[:, 4, :].unsqueeze(1).to_broadcast([128, FT, D]), op=ALU.mult),
                 reads=[self.rBC[4]], writes=[rW2[s]])
            slot_of[(ei, ft)] = s

        def phaseA(i):
            ei, ft, (c0, n, tiles) = items[i]
            s = slot_of[(ei, ft)]
            hs = i % 2
            for fc in range(FT):
                pa, ra = self.nextA()
                pb, rb = self.nextA()
                rt = [self.rH[t] for t in tiles]
                P.op("pe", [(I("matmul", pa[:, 0:n], W13[s][:, k, fc * 128:(fc + 1) * 128], self.h_fm[:, k, c0:c0 + n],
                                                    start=(k == 0), stop=(k == 7))) for k in range(8)],
                     reads=[rW13[s]] + rt, writes=[ra])
                P.op("pe", [(I("matmul", pb[:, 0:n], W13[s][:, k, fs + fc * 128:fs + (fc + 1) * 128], self.h_fm[:, k, c0:c0 + n],
                                                    start=(k == 0), stop=(k == 7))) for k in range(8)],
                     reads=[rW13[s]] + rt, writes=[rb])
                ss = fc % 2
                P.op("act", I("activation", out=sil[ss][:, 0:n], in_=pa[:, 0:n], func=AF.Silu),
                     reads=[ra], writes=[rsil[ss]])
                P.op("dve", I("tensor_tensor", out=hid[hs][:, fc, 0:n], in0=sil[ss][:, 0:n], in1=pb[:, 0:n], op=ALU.mult),
                     reads=[rsil[ss], rb], writes=[rhid[hs]])

        def phaseB(i):
            ei, ft, (c0, n, tiles) = items[i]
            s = slot_of[(ei, ft)]
            hs = i % 2
            gate = experts[ei][2]
            for j, t in enumerate(tiles):
                npart = self.tile_np(t)
                for half in range(2):
                    po, ro = self.nextO()
                    P.op("pe", [(I("matmul", po[0:npart, :], hid[hs][:, fc, j * 128:j * 128 + npart],
                                                          W2[s][:, fc, half * 512:(half + 1) * 512],
                                                          start=(fc == 0), stop=(fc == FT - 1))) for fc in range(FT)],
                         reads=[rhid[hs], rW2[s]], writes=[ro])
                    xs = self.x_tm[0:npart, t, half * 512:(half + 1) * 512]
                    if gate is None:
                        P.op("dve", I("tensor_tensor", out=xs, in0=po[0:npart, :], in1=xs, op=ALU.add),
                             reads=[ro, self.rX[t]], writes=[self.rX[t]])
                    else:
                        P.op("dve", I("scalar_tensor_tensor",
                            out=xs, in0=po[0:npart, :], scalar=self.gates[0:npart, t, gate:gate + 1], in1=xs, op0=ALU.mult, op1=ALU.add),
                            reads=[ro, self.rX[t], self.rGates], writes=[self.rX[t]])

        wl = [(ei, ft) for ei in range(len(experts)) for ft in range(n_ft)]
        ng = len(groups)
        load_w(*wl[0])
        if len(wl) > 1:
            load_w(*wl[1])
        ln_started = False
        for i in range(len(items)):
            if i == 0:
                phaseA(0)
            if i + 1 < len(items):
                phaseA(i + 1)
            phaseB(i)
            if (i + 1) % ng == 0:
                w = (i + 1) // ng - 1
                if w + 2 < len(wl):
                    load_w(*wl[w + 2])
        for (c0, n, tiles) in groups:
            self.ln_tiles(tiles, st, rst, tmps)

    def moe_router(self, router, wbuf, rW, groups):
        P = self.P
        rw_d, rb_d = router
        rwb = wbuf[:, :, 0:8]
        P.dma("pool", I("dma_start", out=rwb, in_=rw_d), "d_rw", writes=[rW])
        lg = self.small[:, 0:128].rearrange("p (t e) -> p t e", e=NE)
        wk = self.small[:, 128:256].rearrange("p (t e) -> p t e", e=NE)
        ex = self.small[:, 256:384].rearrange("p (t e) -> p t e", e=NE)
        m1 = self.small[:, 384:400]
        m2 = self.small[:, 400:416]
        ssum = self.small[:, 416:432]
        rb = self.small[:, 440:448]
        P.dma("sp", I("dma_start", out=rb, in_=rb_d.partition_broadcast(128)), "d_rb", writes=[self.rSmall])
        pt, rp = self.nextA()
        for t in range(NT):
            P.op("pe", [(I("matmul", pt[:, t * 8:(t + 1) * 8], self.h_fm[:, k, t * 128:(t + 1) * 128], rwb[:, k, :],
                                                     start=(k == 0), stop=(k == 7))) for k in range(8)],
                 reads=[rW, self.rH[t]], writes=[rp])
        rS = self.rSmall
        P.op("dve", I("tensor_tensor", out=lg, in0=pt[:, 0:128].rearrange("p (t e) -> p t e", e=NE),
                                              in1=rb.unsqueeze(1).to_broadcast([128, NT, NE]), op=ALU.add), reads=[rp, rS], writes=[rS])
        P.op("dve", I("tensor_reduce", out=m1, in_=lg, axis=AX.X, op=ALU.max), reads=[rS], writes=[rS])
        P.op("dve", I("tensor_tensor", out=wk, in0=lg, in1=m1.unsqueeze(2).to_broadcast([128, NT, NE]), op=ALU.is_ge), reads=[rS], writes=[rS])
        P.op("dve", I("scalar_tensor_tensor", out=wk, in0=wk, scalar=-1e30, in1=lg, op0=ALU.mult, op1=ALU.add), reads=[rS], writes=[rS])
        P.op("dve", I("tensor_reduce", out=m2, in_=wk, axis=AX.X, op=ALU.max), reads=[rS], writes=[rS])
        P.op("dve", I("tensor_tensor", out=wk, in0=lg, in1=m2.unsqueeze(2).to_broadcast([128, NT, NE]), op=ALU.is_ge), reads=[rS], writes=[rS])
        P.op("dve", I("tensor_tensor", out=ex, in0=lg, in1=m1.unsqueeze(2).to_broadcast([128, NT, NE]), op=ALU.subtract), reads=[rS], writes=[rS])
        P.op("act", I("activation", out=ex, in_=ex, func=AF.Exp), reads=[rS], writes=[rS])
        P.op("dve", I("tensor_tensor", out=ex, in0=ex, in1=wk, op=ALU.mult), reads=[rS], writes=[rS])
        P.op("dve", I("tensor_reduce", out=ssum, in_=ex, axis=AX.X, op=ALU.add), reads=[rS], writes=[rS])
        P.op("dve", I("reciprocal", out=ssum, in_=ssum), reads=[rS], writes=[rS])
        P.op("dve", I("tensor_tensor", out=self.gates[:], in0=ex, in1=ssum.unsqueeze(2).to_broadcast([128, NT, NE]), op=ALU.mult),
             reads=[rS], writes=[self.rGates])

    def conv_phase(self, l, g1_idx, ln_idx, next_sc, next_sh):
        P = self.P
        self.arena_reset()
        ciw_d = self.dram["conv_in_w"]
        cow_d = self.dram["conv_out_w"]
        cvec_d = self.dram["cvec"]
        cob_d = self.dram["conv_out_b"]
        Wci = self.alloc([128, 8, 2048], BF16)
        Wco = self.alloc([128, 8, D], BF16)
        rWci, rWco = Res(), Res()
        U = [self.alloc([128, 30 + 128], F32) for _ in range(8)]
        V = [self.alloc([128, 128], F32) for _ in range(8)]
        rU = [Res() for _ in range(8)]
        rV = [Res() for _ in range(8)]
        vb = [self.alloc([128, 128], BF16) for _ in range(2)]
        vq = [self.alloc([128, 128], BF16) for _ in range(2)]
        rvb = [Res() for _ in range(2)]
        rvq = [Res() for _ in range(2)]
        sig = [self.alloc([128, 128], F32) for _ in range(2)]
        rsig = [Res() for _ in range(2)]
        t1 = [self.alloc([128, 128], F32) for _ in range(2)]
        rt1 = [Res() for _ in range(2)]
        ptmp = [self.alloc([128, 128], F32) for _ in range(2)]
        rptmp = [Res() for _ in range(2)]
        stt_ = self.alloc([128, 4, 128], F32)
        rstt = Res()
        cv = self.alloc([128, 72], F32)
        dww = self.alloc([128, 8, CK], F32)
        rcv = Res()
        st = self.alloc([128, NT + 1, 32], F32)
        rst = [Res() for _ in range(NT + 1)]
        tmps = self.mk_tmps()

        self.ln_prep(ln_idx, next_sc, next_sh, Wci[:, :, 0:D], rWci)
        self.ada_vec(g1_idx, 4, True, Wco, rWco)
        P.dma("pool", I("dma_start", out=Wci, in_=ciw_d[l]), "d_wci", writes=[rWci])
        P.dma("pool", I("dma_start", out=Wco, in_=cow_d[l]), "d_wco", writes=[rWco])
        P.op("pool", I("tensor_tensor", out=Wco, in0=Wco, in1=self.bc[:, 4, :].unsqueeze(1).to_broadcast([128, 8, D]), op=ALU.mult),
             reads=[self.rBC[4]], writes=[rWco])
        P.dma("sp", I("dma_start", out=cv[:, 0:40], in_=cvec_d[l]), "d_cv", writes=[rcv])
        P.dma("sp", I("dma_start", out=dww, in_=self.dram["dww"][l]), "d_cv2", writes=[rcv])
        self.load_row_bc(cob_d[l:l + 1, :], 5)
        P.op("pool", I("tensor_tensor", out=self.bc[:, 5, :], in0=self.bc[:, 5, :], in1=self.bc[:, 4, :], op=ALU.mult),
             reads=[self.rBC[4], self.rBC[5]], writes=[self.rBC[5]])
        for c in range(8):
            P.op("pool", I("memset", U[c][:, 0:30], 0.0), writes=[rU[c]])
        for t in range(NT + 1):
            npart = self.tile_np(t)
            P.op("dve", I("scalar_tensor_tensor",
                out=self.x_tm[0:npart, t, :], in0=self.x_tm[0:npart, t, :], scalar=ALPHA, in1=self.bc[0:npart, 5, :],
                op0=ALU.mult, op1=ALU.add), reads=[self.rX[t], self.rBC[5]], writes=[self.rX[t]])

        order = [NT] + list(range(NT))
        for gi, t in enumerate(order):
            n = self.tile_np(t)
            c0 = self.tok0(t)
            rht = self.rH[t]
            for c in range(8):
                pa, ra = self.nextA()
                pg, rg = self.nextA()
                P.op("pe", [(I("matmul", pa[:, 0:n], Wci[:, k, c * 128:(c + 1) * 128], self.h_fm[:, k, c0:c0 + n],
                                                    start=(k == 0), stop=(k == 7))) for k in range(8)], reads=[rWci, rht], writes=[ra])
                P.op("pe", [(I("matmul", pg[:, 0:n], Wci[:, k, D + c * 128:D + (c + 1) * 128], self.h_fm[:, k, c0:c0 + n],
                                                    start=(k == 0), stop=(k == 7))) for k in range(8)], reads=[rWci, rht], writes=[rg])
                ss = c % 2
                P.op("act", I("activation", out=sig[ss][:, 0:n], in_=pg[:, 0:n], func=AF.Sigmoid, bias=cv[:, 8 + c:9 + c]),
                     reads=[rg, rcv], writes=[rsig[ss]])
                P.op("dve", I("scalar_tensor_tensor", out=U[c][:, 30:30 + n], in0=pa[:, 0:n], scalar=cv[:, c:c + 1],
                                                                        in1=sig[ss][:, 0:n], op0=ALU.add, op1=ALU.mult),
                     reads=[ra, rsig[ss], rcv], writes=[rU[c]])
                if t == NT:
                    P.op("dve", I("tensor_scalar", out=U[c][:, 30:30 + n], in0=U[c][:, 30:30 + n], scalar1=self.hm_ap,
                                                              scalar2=None, op0=ALU.mult), reads=[rU[c], self.rConst], writes=[rU[c]])
            for eng, cs in (("dve", (0, 1, 2, 3)), ("dve", (4, 5, 6, 7))):
                for k in range(CK):
                    for c in cs:
                        if k == 0:
                            P.op(eng, I("tensor_scalar", out=V[c][:, 0:n], in0=U[c][:, 0:n], scalar1=dww[:, c, 0:1],
                                        scalar2=cv[:, 16 + c:17 + c], op0=ALU.mult, op1=ALU.add),
                                 reads=[rU[c], rcv], writes=[rV[c]])
                        elif eng == "dve":
                            P.op(eng, I("scalar_tensor_tensor", out=V[c][:, 0:n], in0=U[c][:, k:k + n], scalar=dww[:, c, k:k + 1],
                                        in1=V[c][:, 0:n], op0=ALU.mult, op1=ALU.add),
                                 reads=[rU[c], rcv, rV[c]], writes=[rV[c]])
                        else:
                            ss = c % 2
                            P.op(eng, I("tensor_scalar", out=ptmp[ss][:, 0:n], in0=U[c][:, k:k + n], scalar1=dww[:, c, k:k + 1],
                                        scalar2=None, op0=ALU.mult), reads=[rU[c], rcv], writes=[rptmp[ss]])
                            P.op(eng, I("tensor_tensor", out=V[c][:, 0:n], in0=V[c][:, 0:n], in1=ptmp[ss][:, 0:n], op=ALU.add),
                                 reads=[rptmp[ss], rV[c]], writes=[rV[c]])
            for c in range(8):
                P.op("act", I("activation", out=U[c][:, 0:30], in_=U[c][:, n:n + 30], func=AF.Copy), reads=[rU[c]], writes=[rU[c]])
            pS1, rS1 = self.nextA()
            pS2, rS2 = self.nextA()
            for c in range(8):
                ss = c % 2
                P.op("act", I("activation", out=vb[ss][:, 0:n], in_=V[c][:, 0:n], func=AF.Copy), reads=[rV[c]], writes=[rvb[ss]])
                P.op("act", I("activation", out=vq[ss][:, 0:n], in_=V[c][:, 0:n], func=AF.Square), reads=[rV[c]], writes=[rvq[ss]])
                P.op("pe", I("matmul", pS1[:, 0:n], self.ones[:], vb[ss][:, 0:n], start=(c == 0), stop=(c == 7)),
                     reads=[rvb[ss], self.rConst], writes=[rS1])
                P.op("pe", I("matmul", pS2[:, 0:n], self.ones[:], vq[ss][:, 0:n], start=(c == 0), stop=(c == 7)),
                     reads=[rvq[ss], self.rConst], writes=[rS2])
            mean, msq, sq, rstd = stt_[:, 0, 0:n], stt_[:, 1, 0:n], stt_[:, 2, 0:n], stt_[:, 3, 0:n]
            P.op("dve", I("tensor_scalar", out=mean, in0=pS1[:, 0:n], scalar1=1.0 / D, scalar2=None, op0=ALU.mult), reads=[rS1], writes=[rstt])
            P.op("dve", I("tensor_tensor", out=msq, in0=mean, in1=mean, op=ALU.mult), reads=[rstt], writes=[rstt])
            P.op("dve", I("scalar_tensor_tensor", out=msq, in0=pS2[:, 0:n], scalar=1.0 / D, in1=msq, op0=ALU.mult, op1=ALU.subtract),
                 reads=[rS2, rstt], writes=[rstt])
            P.op("dve", I("tensor_scalar", out=msq, in0=msq, scalar1=0.0, scalar2=EPS, op0=ALU.max, op1=ALU.add), reads=[rstt], writes=[rstt])
            P.op("act", I("activation", out=sq, in_=msq, func=AF.Sqrt), reads=[rstt], writes=[rstt])
            P.op("dve", I("reciprocal", out=rstd, in_=sq), reads=[rstt], writes=[rstt])
            for c in range(8):
                ss = c % 2
                P.op("dve", I("tensor_tensor", out=t1[ss][:, 0:n], in0=V[c][:, 0:n], in1=mean, op=ALU.subtract),
                     reads=[rV[c], rstt], writes=[rt1[ss]])
                P.op("pool", I("tensor_tensor", out=t1[ss][:, 0:n], in0=t1[ss][:, 0:n], in1=rstd, op=ALU.mult),
                     reads=[rt1[ss], rstt], writes=[rt1[ss]])
                P.op("act", I("activation", out=self.h_fm[:, c, c0:c0 + n], in_=t1[ss][:, 0:n], func=AF.Silu,
                                                              scale=cv[:, 24 + c:25 + c], bias=cv[:, 32 + c:33 + c]),
                     reads=[rt1[ss], rcv], writes=[rht])
            for half in range(2):
                po, ro = self.nextO()
                fns = [(I("matmul", po[0:n, :], self.h_fm[:, c, c0:c0 + n], Wco[:, c, half * 512:(half + 1) * 512],
                                               start=(c == 0), stop=(c == 7))) for c in range(8)]
                P.op("pe", fns, reads=[rht, rWco], writes=[ro])
                xs = self.x_tm[0:n, t, half * 512:(half + 1) * 512]
                P.op("dve", I("tensor_tensor", out=xs, in0=po[0:n, :], in1=xs, op=ALU.add),
                     reads=[ro, self.rX[t]], writes=[self.rX[t]])
            self.ln_tiles([t], st, rst, tmps)

    def decl_A(self):
        self.din("conv_in_w", [2, 128, 8, 2048])
        self.din("conv_out_w", [2, 128, 8, D])
        self.din("cvec", [2, 128, 40])
        self.din("dww", [2, 128, 8, CK])
        self.din("conv_out_b", [2, D])
        self.din("ffn13", [11, 128, 8, 512])
        self.din("ffn2", [11, 128, 2, D])
        self.din("moe13", [NE * 7, 128, 8, 1024])
        self.din("moe2", [NE * 7, 128, 4, D])
        self.din("router_w", [128, 8, NE])
        self.din("router_b", [1, NE])
        self.din("wkv", [128, 8, 2048])

    def body_A(self, kt_out, v_out, kv_res=None):
        dr = self.dram
        ffn13, ffn2, moe13, moe2 = dr["ffn13"], dr["ffn2"], dr["moe13"], dr["moe2"]
        main_groups = [(g * 512, 512, [4 * g + j for j in range(4)]) for g in range(4)]
        halo_group = (TOK, HALO, [NT])
        self.arena_reset()
        tmps = self.mk_tmps()
        abuf, rab = self.alloc([128, 8, D], BF16), Res()
        self.ada_vec(0 * 6 + 1, 2, True, abuf, rab)
        self.ada_vec(0 * 6 + 0, 3, False, abuf, rab)
        for i, t in enumerate([NT] + list(range(NT))):
            self.h_tile(t, 2, 3, *tmps[i % 2])
        for l in range(2):
            b = l * 6
            self.conv_phase(l, b + 2, l * 2 + 0, b + 4, b + 3)
            if l == 0:
                experts = [(ffn13, ffn2, None)]
                self.ffn_phase(experts, 2, 11, [halo_group] + main_groups, b + 5, l * 2 + 1, 6 + 1, 6 + 0)
            else:
                experts = [(moe13[e * 7:(e + 1) * 7], moe2[e * 7:(e + 1) * 7], e) for e in range(NE)]
                self.ffn_phase(experts, 4, 7, main_groups, b + 5, l * 2 + 1, 25, 24, router=(dr["router_w"], dr["router_b"]))
        self.kv_phase(dr["wkv"], kt_out, v_out, kv_res)

    def emit_A(self):
        P = self.P
        self.setup_common()
        self.decl_A()
        xout = self.dout("xout", [NT, 128, D])
        kt_out = self.dout("kt_out", [H, DH, TOK], BF16)
        v_out = self.dout("v_out", [NT, 128, D], BF16)
        self.body_A(kt_out, v_out)
        for t in range(NT):
            self.out_tickets.append(P.dma("sp", I("dma_start", out=xout[t], in_=self.x_tm[:, t, :]), "d_out", reads=[self.rX[t]]))

    def emit_F(self):
        P = self.P
        nc = self.nc
        self.setup_common(load_x=False)
        self.decl_A()
        self.decl_B("b")
        xprev = self.din("xin_prev", [NT + 1, 128, D])
        yout = self.dout("yout", [NT, 128, D])
        kt_p = nc.dram_tensor("kt_prev_scr", [H, DH, TOK], BF16).ap()
        v_p = nc.dram_tensor("v_prev_scr", [NT, 128, D], BF16).ap()
        kt_o = nc.dram_tensor("kt_own_scr", [H, DH, TOK], BF16).ap()
        v_o = nc.dram_tensor("v_own_scr", [NT, 128, D], BF16).ap()
        res_p, res_o = [], []
        self.load_x(xprev)
        self.hm_ap = self.hmask[:, 1:2]
        self.body_A(kt_p, v_p, res_p)
        self.load_x(self.dram["xin"])
        self.hm_ap = self.hmask[:, 0:1]
        self.body_A(kt_o, v_o, res_o)

        def kv_load(h, s, Kb, Vb, rK, rV):
            P.dma("sp", I("dma_start", out=Kb[s][0:DH, 0:TOK], in_=kt_p[h]), f"d_k{s}", reads=res_p, writes=[rK[s]])
            P.dma("sp", I("dma_start", out=Kb[s][0:DH, TOK:2 * TOK], in_=kt_o[h]), f"d_k{s}", reads=res_o, writes=[rK[s]])
            vp = v_p.rearrange("t p (h d) -> h p t d", d=DH)
            vo = v_o.rearrange("t p (h d) -> h p t d", d=DH)
            P.dma("sp", I("dma_start", out=Vb[s][:, 0:16, 0:DH], in_=vp[h]), f"d_v{s}", reads=res_p, writes=[rV[s]])
            P.dma("sp", I("dma_start", out=Vb[s][:, 16:32, 0:DH], in_=vo[h]), f"d_v{s}", reads=res_o, writes=[rV[s]])

        self.body_B(yout, kv_load, "b", ones_col=True)

    def kv_phase(self, wkv_d, kt_out, v_out, kv_res=None):
        P = self.P
        self.arena_reset()
        Wkv = self.alloc([128, 8, 2048], BF16)
        rW = Res()
        kts = [self.alloc([128, 512], BF16) for _ in range(2)]
        rk = [Res() for _ in range(2)]
        vts = [self.alloc([128, D], BF16) for _ in range(2)]
        rv = [Res() for _ in range(2)]
        P.dma("pool", I("dma_start", out=Wkv, in_=wkv_d), "d_wkv", writes=[rW])
        i = 0
        for g in range(4):
            c0 = g * 512
            rt = [self.rH[4 * g + j] for j in range(4)]
            for c in range(8):
                pa, ra = self.nextA()
                P.op("pe", [(I("matmul", pa[:], Wkv[:, k, c * 128:(c + 1) * 128], self.h_fm[:, k, c0:c0 + 512],
                                                    start=(k == 0), stop=(k == 7))) for k in range(8)], reads=[rW] + rt, writes=[ra])
                s = i % 2
                i += 1
                P.op("act", I("activation", out=kts[s][:], in_=pa[:], func=AF.Copy), reads=[ra], writes=[rk[s]])
                for hh in range(2):
                    self._kv_out(P.dma("sp", I("dma_start",
                        out=kt_out[2 * c + hh, :, c0:c0 + 512], in_=kts[s][hh * 64:(hh + 1) * 64, :]), f"d_ko{s}", reads=[rk[s]]), kv_res)
        for t in range(NT):
            s = t % 2
            for half in range(2):
                po, ro = self.nextO()
                P.op("pe", [(I("matmul", po[:], self.h_fm[:, k, t * 128:(t + 1) * 128], Wkv[:, k, D + half * 512:D + (half + 1) * 512],
                                                    start=(k == 0), stop=(k == 7))) for k in range(8)], reads=[rW, self.rH[t]], writes=[ro])
                P.op("act", I("activation", out=vts[s][:, half * 512:(half + 1) * 512], in_=po[:], func=AF.Copy),
                     reads=[ro], writes=[rv[s]])
            self._kv_out(P.dma("sp", I("dma_start", out=v_out[t], in_=vts[s][:]), f"d_vo{s}", reads=[rv[s]]), kv_res)

    def _kv_out(self, ticket, kv_res):
        if kv_res is None:
            self.out_tickets.append(ticket)
        else:
            r = Res()
            r.w = ticket
            kv_res.append(r)

    def decl_B(self, sfx=""):
        self.din("wq", [2, 128, 8, D])
        self.din("wo", [2, 128, 8, D])
        self.din("ffn13" + sfx, [11, 128, 8, 512])
        self.din("ffn2" + sfx, [11, 128, 2, D])
        self.din("moe13" + sfx, [NE * 7, 128, 8, 1024])
        self.din("moe2" + sfx, [NE * 7, 128, 4, D])
        self.din("router_w" + sfx, [128, 8, NE])
        self.din("router_b" + sfx, [1, NE])
        self.din("kconst", [128, 2 * TOK], BF16)
        self.din("negelig", [128, 256])
        self.din("elig01", [128, 256])
        self.din("dbase", [128, 256])
        self.din("tri", [128, 128], BF16)

    def body_B(self, yout, kv_load, sfx="", ones_col=False):
        P = self.P
        dr = self.dram
        ffn13, ffn2, moe13, moe2 = dr["ffn13" + sfx], dr["ffn2" + sfx], dr["moe13" + sfx], dr["moe2" + sfx]
        main_groups = [(g * 512, 512, [4 * g + j for j in range(4)]) for g in range(4)]
        self.arena_reset()
        tmps = self.mk_tmps()
        abuf, rab = self.alloc([128, 8, D], BF16), Res()
        self.ada_vec(2 * 6 + 1, 2, True, abuf, rab)
        self.ada_vec(2 * 6 + 0, 3, False, abuf, rab)
        for i, t in enumerate(range(NT)):
            self.h_tile(t, 2, 3, *tmps[i % 2])
        for l in (2, 3):
            b = l * 6
            self.attn_phase(l - 2, dr["wq"], dr["wo"], b + 2, l * 2 + 0, b + 4, b + 3, kv_load, ones_col)
            if l == 2:
                self.ffn_phase([(ffn13, ffn2, None)], 2, 11, main_groups, b + 5, l * 2 + 1, 18 + 1, 18 + 0)
            else:
                experts = [(moe13[e * 7:(e + 1) * 7], moe2[e * 7:(e + 1) * 7], e) for e in range(NE)]
                self.ffn_phase(experts, 4, 7, main_groups, b + 5, l * 2 + 1, None, None,
                               router=(dr["router_w" + sfx], dr["router_b" + sfx]))
        for t in range(NT):
            self.out_tickets.append(P.dma("sp", I("dma_start", out=yout[t], in_=self.x_tm[:, t, :]), "d_out", reads=[self.rX[t]]))

    def emit_B(self):
        P = self.P
        self.setup_common()
        self.decl_B()
        ktf = self.din("kt_full", [H, DH, 2 * TOK], BF16)
        vf = self.din("v_full", [H, 128, 32, 65], BF16)
        yout = self.dout("yout", [NT, 128, D])

        def kv_load(h, s, Kb, Vb, rK, rV):
            P.dma("sp", I("dma_start", out=Kb[s][0:DH, :], in_=ktf[h]), f"d_k{s}", writes=[rK[s]])
            P.dma("sp", I("dma_start", out=Vb[s], in_=vf[h]), f"d_v{s}", writes=[rV[s]])

        self.body_B(yout, kv_load)

    def attn_phase(self, j, wq_d, wo_d, g1_idx, ln_idx, next_sc, next_sh, kv_load, ones_col=False):
        P = self.P
        self.arena_reset()
        Wqh = [self.alloc([128, 8, DH], BF16) for _ in range(2)]
        rWq = [Res() for _ in range(2)]
        Kb = [self.alloc([128, 2 * TOK], BF16) for _ in range(2)]
        kb_off = self.last_off - 2 * TOK
        rK = [Res() for _ in range(2)]
        Vb = [self.alloc([128, 32, 65], BF16) for _ in range(2)]
        vb_off = self.last_off - 32 * 65
        rV = [Res() for _ in range(2)]
        qa, rqa = self.alloc([128, TOK], BF16), Res()
        attn = self.alloc([128, NT, D], BF16)
        rAt = [Res() for _ in range(NT)]
        Rst, rRst = self.alloc([128, NT, 64], BF16), Res()
        PT = [self.alloc([128, 2, 256], BF16) for _ in range(3)]
        rPT = [Res() for _ in range(3)]
        gm = self.alloc([128, NT, 16], F32)
        cur = self.alloc([128, NT, 16], F32)
        eq = self.alloc([128, NT, 16], F32)
        mx = self.alloc([128, NT], F32)
        rG = Res()
        neg = self.alloc([128, NT, 16], F32)
        el = self.alloc([128, NT, 16], F32)
        db = self.alloc([128, NT, 16], F32)
        tri = self.alloc([128, 128], BF16)
        km = self.alloc([128, 16], F32)
        kmb = self.alloc([128, 16], BF16)
        rkm = Res()
        rec = self.alloc([128, 2], F32)
        rrec = Res()
        rC = Res()
        for s in range(2):
            P.dma("sp", I("dma_start", out=Kb[s], in_=self.dram["kconst"]), "d_kc", writes=[rK[s]])
        f3 = lambda ap: ap.rearrange("p (a b) -> p a b", b=16)
        P.dma("sp", I("dma_start", out=neg, in_=f3(self.dram["negelig"])), "d_ac", writes=[rC])
        P.dma("sp", I("dma_start", out=el, in_=f3(self.dram["elig01"])), "d_ac", writes=[rC])
        P.dma("sp", I("dma_start", out=db, in_=f3(self.dram["dbase"])), "d_ac", writes=[rC])
        P.dma("sp", I("dma_start", out=tri, in_=self.dram["tri"]), "d_ac", writes=[rC])
        P.op("pool", I("memset", Rst, 0.0), writes=[rRst])
        if ones_col:
            for s_ in range(2):
                P.op("pool", I("memset", Vb[s_][:, :, DH:DH + 1], 1.0), writes=[rV[s_]])
        rHall = [self.rH[t] for t in range(NT)]
        it_s = 0
        for h in range(H):
            s = h % 2
            slope = float(2.0 ** (-8.0 * (h + 1) / H))
            s1 = float(np.float32(slope).astype(ml_dtypes.bfloat16).astype(np.float32))
            s2 = float(np.float32(slope - s1).astype(ml_dtypes.bfloat16).astype(np.float32))
            s3 = float(np.float32(slope - s1 - s2).astype(ml_dtypes.bfloat16).astype(np.float32))
            P.dma("pool", I("dma_start", out=Wqh[s], in_=wq_d[j][:, :, h * DH:(h + 1) * DH]), f"d_wq{s}", writes=[rWq[s]])
            kv_load(h, s, Kb, Vb, rK, rV)
            for g in range(4):
                pa, ra = self.nextA()
                P.op("pe", [I("matmul", pa[0:DH, :], Wqh[s][:, k, :], self.h_fm[:, k, g * 512:(g + 1) * 512], start=(k == 0), stop=(k == 7))
                            for k in range(8)], reads=[rWq[s]] + rHall[4 * g:4 * g + 4], writes=[ra])
                P.op("act", I("activation", out=qa[0:DH, g * 512:(g + 1) * 512], in_=pa[0:DH, :], func=AF.Copy, scale=float(DH ** -0.5)),
                     reads=[ra], writes=[rqa])
            P.op("dve", I("tensor_reduce", out=km[0:DH, :], in_=Kb[s][0:DH, :].rearrange("p (a b) -> p a b", b=BLK), axis=AX.X, op=ALU.add),
                 reads=[rK[s]], writes=[rkm])
            P.op("dve", I("tensor_copy", out=kmb[0:DH, :], in_=km[0:DH, :]), reads=[rkm], writes=[rkm])
            pg, rg = self.nextA()
            P.op("pe", [I("matmul", pg[:, t * 16:(t + 1) * 16], qa[0:DH, t * 128:(t + 1) * 128], kmb[0:DH, :], start=True, stop=True)
                        for t in range(NT)], reads=[rqa, rkm], writes=[rg])
            pg3 = pg[:, 0:256].rearrange("p (a b) -> p a b", b=16)
            bcast = lambda v: v.unsqueeze(2).to_broadcast([128, NT, 16])
            P.op("dve", I("tensor_tensor", out=gm, in0=pg3, in1=neg, op=ALU.add), reads=[rg, rC], writes=[rG])
            P.op("dve", I("tensor_copy", out=cur, in_=gm), reads=[rG], writes=[rG])
            for r in range(4):
                P.op("dve", I("tensor_reduce", out=mx, in_=cur, axis=AX.X, op=ALU.max), reads=[rG], writes=[rG])
                if r < 3:
                    P.op("dve", I("tensor_tensor", out=eq, in0=cur, in1=bcast(mx), op=ALU.is_ge), reads=[rG], writes=[rG])
                    P.op("dve", I("scalar_tensor_tensor", out=cur, in0=eq, scalar=-3e30, in1=cur, op0=ALU.mult, op1=ALU.add), reads=[rG], writes=[rG])
            P.op("dve", I("tensor_tensor", out=eq, in0=gm, in1=bcast(mx), op=ALU.is_ge), reads=[rG], writes=[rG])
            P.op("dve", I("tensor_tensor", out=eq, in0=eq, in1=el, op=ALU.mult), reads=[rG, rC], writes=[rG])
            P.op("dve", I("tensor_scalar", out=eq, in0=eq, scalar1=-1.0, scalar2=BIG, op0=ALU.add, op1=ALU.mult), reads=[rG], writes=[rG])
            P.op("dve", I("scalar_tensor_tensor", out=cur, in0=db, scalar=slope, in1=eq, op0=ALU.mult, op1=ALU.add), reads=[rG, rC], writes=[rG])
            P.op("dve", I("tensor_copy", out=Rst[:, :, 0:16], in_=cur), reads=[rG], writes=[rRst])
            P.op("dve", I("tensor_tensor", out=Rst[:, :, 32:48], in0=cur, in1=Rst[:, :, 0:16], op=ALU.subtract), reads=[rG, rRst], writes=[rRst])
            for ci, sv in enumerate((s1, s2, s3)):
                P.op("pool", I("memset", Rst[:, :, 16 + ci:17 + ci], sv), writes=[rRst])
            for g in range(4):
                pa, ra = self.nextA()
                P.op("pe", [I("matmul", pa[0:64, i * 128:(i + 1) * 128], Rst[:, 4 * g + i, :], self.ident[:], start=True, stop=True)
                            for i in range(4)], reads=[rRst, self.rConst], writes=[ra])
                P.op("dve", I("tensor_copy", out=qa[64:128, g * 512:(g + 1) * 512], in_=pa[0:64, :]), reads=[ra], writes=[rqa])
            items = [(b, jb) for b in range(8) for jb in range(9 + b)]
            accs = {}
            st_of = {}

            def qk_exp(i):
                nonlocal it_s
                b, jb = items[i]
                own = (jb == 8 + b)
                ps_, rs_ = self.nextA()
                S = ps_[:].rearrange("p (a b) -> p a b", b=256)
                q0 = b * 256
                fns = []
                if not own:
                    for kt in range(2):
                        fns.append(I("matmul", S[:, kt, :], Kb[s][:, (2 * jb + kt) * 128:(2 * jb + kt + 1) * 128], qa[:, q0:q0 + 256], start=True, stop=True))
                else:
                    fns.append(I("matmul", S[:, 0, :], Kb[s][:, (2 * jb) * 128:(2 * jb + 1) * 128], qa[:, q0:q0 + 256], start=True, stop=False))
                    fns.append(I("matmul", S[:, 0, 0:128], self.ident[:], tri, start=False, stop=True))
                    fns.append(I("matmul", S[:, 1, 128:256], Kb[s][:, (2 * jb + 1) * 128:(2 * jb + 2) * 128], qa[:, q0 + 128:q0 + 256], start=True, stop=False))
                    fns.append(I("matmul", S[:, 1, 128:256], self.ident[:], tri, start=False, stop=True))
                P.op("pe", fns, reads=[rK[s], rqa, rC, self.rConst], writes=[rs_])
                ts_ = it_s % 3
                it_s += 1
                st_of[i] = ts_
                if not own:
                    P.op("act", I("activation", out=PT[ts_], in_=S, func=AF.Exp), reads=[rs_], writes=[rPT[ts_]])
                else:
                    P.op("act", I("activation", out=PT[ts_][:, 0, :], in_=S[:, 0, :], func=AF.Exp), reads=[rs_], writes=[rPT[ts_]])
                    P.op("act", I("activation", out=PT[ts_][:, 1, 128:256], in_=S[:, 1, 128:256], func=AF.Exp), reads=[rs_], writes=[rPT[ts_]])

            def pv(i):
                b, jb = items[i]
                own = (jb == 8 + b)
                if jb == 0:
                    po, ro = self.nextO()
                    accs[b] = (po[:, 0:130].rearrange("p (a b) -> p a b", b=65), ro)
                acc, ro = accs[b]
                ts_ = st_of[i]
                fns = []
                for qi in range(2):
                    for kt in range(2):
                        if own and kt == 1 and qi == 0:
                            continue
                        last = own and ((qi == 0 and kt == 0) or (qi == 1 and kt == 1))
                        fns.append(I("matmul", acc[:, qi, :], PT[ts_][:, kt, qi * 128:(qi + 1) * 128], Vb[s][:, 2 * jb + kt, :],
                                     start=(jb == 0 and kt == 0), stop=last))
                P.op("pe", fns, reads=[rPT[ts_], rV[s]], writes=[ro], noself=(jb > 0))
                if own:
                    P.op("dve", I("reciprocal", out=rec, in_=acc[:, :, 64]), reads=[ro], writes=[rrec])
                    for qi in range(2):
                        t = 2 * b + qi
                        P.op("dve", I("tensor_scalar", out=attn[:, t, h * DH:(h + 1) * DH], in0=acc[:, qi, 0:64], scalar1=rec[:, qi:qi + 1],
                                      scalar2=None, op0=ALU.mult), reads=[ro, rrec], writes=[rAt[t]])

            qk_exp(0)
            for i in range(len(items)):
                if i + 1 < len(items):
                    qk_exp(i + 1)
                pv(i)
        self.P.barrier()
        Wo, rWo = self.view(kb_off, [128, 8, D], BF16), Res()
        abuf, rab = self.view(vb_off, [128, 8, 512], BF16), Res()
        self.ln_prep(ln_idx, next_sc, next_sh, abuf, rab)
        self.ada_vec(g1_idx, 4, True, abuf, rab)
        P.dma("pool", I("dma_start", out=Wo, in_=wo_d[j]), "d_wo", writes=[rWo])
        P.op("pool", I("tensor_tensor", out=Wo, in0=Wo, in1=self.bc[:, 4, :].unsqueeze(1).to_broadcast([128, 8, D]), op=ALU.mult),
             reads=[self.rBC[4]], writes=[rWo])
        self.scale_x(list(range(NT)))
        for t in range(NT):
            pt, rp = self.nextT()
            P.op("pe", [I("transpose", pt[:, k, :], attn[:, t, k * 128:(k + 1) * 128], self.ident[:]) for k in range(8)],
                 reads=[rAt[t], self.rConst], writes=[rp])
            P.op("act", I("activation", out=self.h_fm[:, :, t * 128:(t + 1) * 128], in_=pt[:], func=AF.Copy), reads=[rp], writes=[self.rH[t]])
            for half in range(2):
                po, ro = self.nextO()
                P.op("pe", [I("matmul", po[:], self.h_fm[:, c, t * 128:(t + 1) * 128], Wo[:, c, half * 512:(half + 1) * 512],
                              start=(c == 0), stop=(c == 7)) for c in range(8)], reads=[self.rH[t], rWo], writes=[ro])
                xs = self.x_tm[:, t, half * 512:(half + 1) * 512]
                P.op("dve", I("tensor_tensor", out=xs, in0=po[:], in1=xs, op=ALU.add), reads=[ro, self.rX[t]], writes=[self.rX[t]])
        self.P.barrier()
        st = Vb[0].rearrange("p a b -> p (a b)")[:, 0:(NT + 1) * 64].bitcast(F32).rearrange("p (a b) -> p a b", b=32)
        rst = [Res() for _ in range(NT + 1)]
        tf = qa.bitcast(F32)
        hb0 = PT[0].rearrange("p a b -> p (a b)")
        tmps = [(tf, Res(), attn[:, 0, :], Res()), (tf, None, attn[:, 1, :], Res())]
        tmps[1] = (tf, tmps[0][1], attn[:, 1, :], Res())
        self.ln_tiles(list(range(NT)), st, rst, tmps)

def _fm(w):
    k = w.shape[0] // 128
    return np.ascontiguousarray(w.reshape(k, 128, w.shape[1]).transpose(1, 0, 2))


def _ffn_tiles(w13, w2, F, fs):
    n_ft = F // fs
    FT = fs // 128
    a, b = w13[:, :F], w13[:, F:]
    t13 = np.stack([_fm(np.concatenate([a[:, i * fs:(i + 1) * fs], b[:, i * fs:(i + 1) * fs]], axis=1)) for i in range(n_ft)])
    t2 = np.stack([np.ascontiguousarray(w2[i * fs:(i + 1) * fs].reshape(FT, 128, D).transpose(1, 0, 2)) for i in range(n_ft)])
    return t13, t2


def _shared_common(inp):
    f32 = np.float32
    sh = {}
    pieces = []
    for l in range(DEPTH):
        for j in range(6):
            pieces.append(_fm(inp["ada_w"][l][:, j * D:(j + 1) * D]))
    for j in range(2):
        pieces.append(_fm(inp["kv_ada_w"][:, j * D:(j + 1) * D]))
    sh["adaw"] = np.stack(pieces).astype(f32)
    sh["adab"] = np.concatenate([inp["ada_b"].reshape(DEPTH * 6, D), inp["kv_ada_b"].reshape(2, D)], axis=0).astype(f32)
    sh["lng"] = np.ascontiguousarray(inp["ln_g"].reshape(8, D)).astype(f32)
    sh["lnb"] = np.ascontiguousarray(inp["ln_b"].reshape(8, D)).astype(f32)
    sh["ident_in"] = np.eye(128, dtype=f32).astype(ml_dtypes.bfloat16)
    return sh


def _ffn_moe(inp, i, sh):
    f32 = np.float32
    sh["ffn13"], sh["ffn2"] = _ffn_tiles(inp["ffn_w13"][i], inp["ffn_w2"][i], FD, 256)
    m13, m2 = [], []
    for e in range(NE):
        a, b = _ffn_tiles(inp["moe_w13"][i][e], inp["moe_w2"][i][e], FE, 512)
        m13.append(a)
        m2.append(b)
    sh["moe13"] = np.concatenate(m13, axis=0)
    sh["moe2"] = np.concatenate(m2, axis=0)
    sh["router_w"] = _fm(inp["router_w"][i]).astype(f32)
    sh["router_b"] = inp["router_b"][i].reshape(1, NE).astype(f32)


def _shared_A(inp):
    f32 = np.float32
    sh = _shared_common(inp)
    sh["conv_in_w"] = np.stack([_fm(inp["conv_in_w"][l]) for l in range(2)]).astype(f32)
    sh["conv_out_w"] = np.stack([_fm(inp["conv_out_w"][l]) for l in range(2)]).astype(f32)
    cvec = np.zeros((2, 128, 40), f32)
    for l in range(2):
        cvec[l, :, 0:16] = inp["conv_in_b"][l].reshape(16, 128).T
        cvec[l, :, 16:24] = inp["conv_dw_b"][l].reshape(8, 128).T
        cvec[l, :, 24:32] = inp["conv_norm_g"][l].reshape(8, 128).T
        cvec[l, :, 32:40] = inp["conv_norm_b"][l].reshape(8, 128).T
    sh["cvec"] = cvec
    sh["dww"] = np.stack([np.ascontiguousarray(inp["conv_dw_w"][l].reshape(CK, 8, 128).transpose(2, 1, 0)) for l in range(2)]).astype(f32)
    sh["conv_out_b"] = np.ascontiguousarray(inp["conv_out_b"]).astype(f32)
    _ffn_moe(inp, 0, sh)
    sh["wkv"] = _fm(inp["w_kv"]).astype(f32)
    return sh


def _shared_B(inp):
    f32 = np.float32
    bf = ml_dtypes.bfloat16
    sh = _shared_common(inp)
    sh["wq"] = np.stack([_fm(inp["w_q"][j]) for j in range(2)]).astype(f32)
    sh["wo"] = np.stack([_fm(inp["w_o"][j]) for j in range(2)]).astype(f32)
    _ffn_moe(inp, 1, sh)
    kc = np.zeros((128, 2 * TOK), f32)
    blk = np.arange(2 * TOK) // BLK
    pos = np.arange(2 * TOK) % BLK
    for jb in range(16):
        kc[64 + jb, blk == jb] = 1.0
        kc[96 + jb, blk == jb] = 1.0
    kc[80:83, :] = pos[None, :]
    sh["kconst"] = kc.astype(bf)
    tri = np.where(np.arange(128)[:, None] > np.arange(128)[None, :], -BIG, 0.0).astype(f32)
    sh["tri"] = tri.astype(bf)
    return sh


def _core_B(half):
    f32 = np.float32
    p = np.arange(128)[:, None, None]
    tau = np.arange(NT)[None, :, None]
    jb = np.arange(16)[None, None, :]
    b = tau // 2
    own = (jb == 8 + b)
    past = (jb < 8 + b) & ((jb >= 8) | (half == 1))
    neg = np.where(own, 1e30, np.where(past, 0.0, -1e30)) + 0.0 * p
    el = np.where(own | past, 1.0, 0.0) + 0.0 * p
    tq = TOK + tau * 128 + p
    db = -(tq - BLK * jb) + 0.0
    return {"negelig": neg.reshape(128, 256).astype(f32), "elig01": el.reshape(128, 256).astype(f32),
            "dbase": db.reshape(128, 256).astype(f32)}


def _kv_full(kt_prev, kt_own, v_prev, v_own):
    bf = ml_dtypes.bfloat16
    ktf = np.zeros((H, DH, 2 * TOK), bf)
    vf = np.zeros((H, 128, 32, 65), bf)
    vf[:, :, :, 64] = 1.0
    if kt_prev is not None:
        ktf[:, :, :TOK] = kt_prev
        vf[:, :, :16, :64] = v_prev.reshape(16, 128, H, DH).transpose(2, 1, 0, 3)
    ktf[:, :, TOK:] = kt_own
    vf[:, :, 16:, :64] = v_own.reshape(16, 128, H, DH).transpose(2, 1, 0, 3)
    return ktf, vf


def _core_common(inp, core, xsrc):
    b, half = core // 2, core % 2
    s0 = half * TOK
    m = {}
    xin = np.zeros((NT + 1, 128, D), np.float32)
    xin[:NT] = xsrc[b, s0:s0 + TOK].reshape(NT, 128, D)
    if half == 1:
        xin[NT, :HALO] = xsrc[b, s0 - HALO:s0]
    m["xin"] = xin
    m["hmask"] = np.full((128, 1), float(half), np.float32)
    m["cfm"] = np.ascontiguousarray(inp["c"][b].reshape(8, 128).T).astype(np.float32)
    return m


_NC_CACHE = {}


def _get_nc(part):
    if part not in _NC_CACHE:
        _NC_CACHE[part] = Builder(part).build()
    return _NC_CACHE[part]


def run_part_A(inp, cores):
    sh = _shared_A(inp)
    maps = []
    for core in cores:
        m = dict(sh)
        m.update(_core_common(inp, core, inp["x"]))
        maps.append(m)
    nc = _get_nc("A")
    res = run_bass_kernel_spmd(nc, maps, core_ids=list(range(len(cores))))
    return res.results


def run_part_B(inp, cores, x1, kts, vs):
    sh = _shared_B(inp)
    maps = []
    for core in cores:
        half = core % 2
        m = dict(sh)
        m.update(_core_common(inp, core, x1))
        m.update(_core_B(half))
        if half == 1:
            ktf, vf = _kv_full(kts[core - 1], kts[core], vs[core - 1], vs[core])
        else:
            ktf, vf = _kv_full(None, kts[core], None, vs[core])
        m["kt_full"], m["v_full"] = ktf, vf
        maps.append(m)
    nc = _get_nc("B")
    res = run_bass_kernel_spmd(nc, maps, core_ids=list(range(len(cores))))
    return res.results


def run_fused(inp, cores):
    shA = _shared_A(inp)
    shB = _shared_B(inp)
    sh = dict(shA)
    for k, v in shB.items():
        if k in ("ffn13", "ffn2", "moe13", "moe2", "router_w", "router_b"):
            sh[k + "b"] = v
        else:
            sh[k] = v
    maps = []
    for core in cores:
        b, half = core // 2, core % 2
        m = dict(sh)
        m.update(_core_common(inp, core, inp["x"]))
        m.update(_core_B(half))
        xp = np.zeros((NT + 1, 128, D), np.float32)
        if half == 1:
            xp[:NT] = inp["x"][b, 0:TOK].reshape(NT, 128, D)
        m["xin_prev"] = xp
        maps.append(m)
    nc = _get_nc("F")
    res = run_bass_kernel_spmd(nc, maps, core_ids=list(range(len(cores))))
    return res.results


MODE = "F"


def kernel(**inputs):
    inp = {k: np.asarray(v) for k, v in inputs.items()}
    cores = list(range(NCORES))
    out = np.zeros((NB, SEQ, D), np.float32)
    if MODE == "F":
        res = run_fused(inp, cores)
        for c in cores:
            b, half = c // 2, c % 2
            out[b, half * TOK:(half + 1) * TOK] = np.asarray(res[c]["yout"]).reshape(TOK, D)
        return out
    resA = run_part_A(inp, cores)
    x1 = np.zeros((NB, SEQ, D), np.float32)
    kts, vs = {}, {}
    for c in cores:
        b, half = c // 2, c % 2
        x1[b, half * TOK:(half + 1) * TOK] = np.asarray(resA[c]["xout"]).reshape(TOK, D)
        kts[c] = np.asarray(resA[c]["kt_out"])
        vs[c] = np.asarray(resA[c]["v_out"]).reshape(TOK, D)
    resB = run_part_B(inp, cores, x1, kts, vs)
    for c in cores:
        b, half = c // 2, c % 2
        out[b, half * TOK:(half + 1) * TOK] = np.asarray(resB[c]["yout"]).reshape(TOK, D)
    return out
```

```python
import numpy as np
import ml_dtypes
import concourse.bass as bass
import concourse.mybir as mybir
from concourse.bass_utils import run_bass_kernel_spmd

F32 = mybir.dt.float32
BF16 = mybir.dt.bfloat16
ALU = mybir.AluOpType
AF = mybir.ActivationFunctionType
AX = mybir.AxisListType

D = 1024
SEQ = 4096
NB = 4
NCORES = 8
TOK = 2048
NT = 16
HALO = 64
H = 16
DH = 64
BLK = 256
DEPTH = 4
NA = 2
FD = 2816
FE = 3584
NE = 8
CK = 31
ALPHA = (2.0 * DEPTH) ** 0.25
EPS = 1e-5
BIG = 30000.0

ENGS = ["pe", "act", "dve", "pool", "sp"]


class Res:
    __slots__ = ("w", "r")

    def __init__(self):
        self.w = None
        self.r = {}


class Prog:
    def __init__(self):
        self.q = {e: [] for e in ENGS}
        self.cnt = {}
        self.seen = {e: {} for e in ENGS}
        self.names = []

    def _sem(self, name):
        if name not in self.cnt:
            self.cnt[name] = 0
            self.names.append(name)

    def _wait(self, eng, deps):
        for name, val in deps:
            if self.seen[eng].get(name, 0) < val:
                self.seen[eng][name] = val
                self.q[eng].append((0, name, val))

    @staticmethod
    def _collect(reads, writes):
        deps = []
        for r in reads:
            if r.w is not None:
                deps.append(r.w)
        for w in writes:
            if w.w is not None:
                deps.append(w.w)
            deps.extend(w.r.items())
        return deps

    @staticmethod
    def _mark(t, reads, writes):
        for r in reads:
            if r.r.get(t[0], 0) < t[1]:
                r.r[t[0]] = t[1]
        for w in writes:
            w.w = t
            w.r = {}

    def op(self, eng, fns, reads=(), writes=(), noself=False):
        if isinstance(fns, tuple):
            fns = [fns]
        deps = self._collect(reads, writes)
        if noself:
            deps = [d for d in deps if d[0] != "c_" + eng]
        self._wait(eng, deps)
        name = "c_" + eng
        self._sem(name)
        self.cnt[name] += 1
        t = (name, self.cnt[name])
        self.q[eng].append((1, fns, name, 1))
        self._mark(t, reads, writes)
        return t

    def dma(self, eng, fn, sem, reads=(), writes=()):
        self._wait(eng, self._collect(reads, writes))
        self._sem(sem)
        self.cnt[sem] += 16
        t = (sem, self.cnt[sem])
        self.q[eng].append((1, [fn], sem, 16))
        self._mark(t, reads, writes)
        return t

    def barrier(self):
        allt = [(n, self.cnt[n]) for n in self.names if self.cnt[n] > 0]
        for e in ENGS:
            self._wait(e, allt)

    def wait_on(self, eng, tickets):
        self._wait(eng, tickets)


def I(name, *a, **k):
    return (name, a, k)


def _replay(e, items, sems):
    for it in items:
        if it[0] == 0:
            e.wait_ge(sems[it[1]], it[2])
        else:
            fns = it[1]
            for f in fns[:-1]:
                getattr(e, f[0])(*f[1], **f[2])
            f = fns[-1]
            getattr(e, f[0])(*f[1], **f[2]).then_inc(sems[it[2]], it[3])


class Builder:
    def __init__(self, part):
        self.part = part
        self.nc = bass.Bass("TRN2", target_bir_lowering=False)
        self.P = Prog()
        self.dram = {}
        self.dmasem_i = 0

    def din(self, name, shape, dt=F32):
        t = self.nc.dram_tensor(name, list(shape), dt, kind="ExternalInput").ap()
        self.dram[name] = t
        return t

    def dout(self, name, shape, dt=F32):
        t = self.nc.dram_tensor(name, list(shape), dt, kind="ExternalOutput").ap()
        self.dram[name] = t
        return t

    def arena_reset(self):
        self.P.barrier()
        self.aoff = 0

    def alloc(self, shape, dt):
        n = int(np.prod(shape[1:]))
        nb = n * (4 if dt == F32 else 2)
        nb = (nb + 63) // 64 * 64
        off = self.aoff
        self.aoff += nb // 2
        assert self.aoff <= self.arena_elems, f"arena overflow {self.aoff*2} > {self.arena_elems*2}"
        self.last_off = off
        return self.view(off, shape, dt)

    def view(self, off, shape, dt):
        n = int(np.prod(shape[1:]))
        nb = n * (4 if dt == F32 else 2)
        nb = (nb + 63) // 64 * 64
        v = self.arena[:, off:off + nb // 2]
        if dt == F32:
            v = v.bitcast(F32)
        v = v[:, 0:n]
        if len(shape) == 3:
            v = v.rearrange("p (a b) -> p a b", b=shape[2])
        elif len(shape) == 4:
            v = v.rearrange("p (a b c) -> p a b c", b=shape[2], c=shape[3])
        return v

    def build(self):
        nc = self.nc
        from contextlib import ExitStack
        with ExitStack() as es:
            def sb(name, shape, dt):
                return es.enter_context(nc.sbuf_tensor(name, list(shape), dt))

            def ps(name, shape, dt):
                return es.enter_context(nc.psum_tensor(name, list(shape), dt))

            self.x_tm = sb("x_tm", [128, NT + 1, D], F32)
            self.h_fm = sb("h_fm", [128, 8, TOK + HALO], BF16)
            self.bc = sb("bc", [128, 6, D], F32)
            self.ident = sb("ident", [128, 128], BF16)
            self.ones = sb("ones", [128, 128], BF16)
            self.cbc = sb("cbc", [128, 8, 128], BF16)
            self.cfm = sb("cfm_sb", [128, 16], F32)
            self.small = sb("small", [128, 512], F32)
            self.gates = sb("gates", [128, NT, NE], F32)
            self.hmask = sb("hmask_sb", [128, 2], F32)
            self.arena_elems = 38 * 1024
            self.arena = sb("arena", [128, self.arena_elems], BF16)
            self.aoff = 0
            self.psA = [ps(f"psA{i}", [128, 512], F32) for i in range(4)]
            self.psO = [ps(f"psO{i}", [128, 512], F32) for i in range(2)]
            self.psT = [ps(f"psT{i}", [128, 8, 128], BF16) for i in range(2)]
            self.rA = [Res() for _ in range(4)]
            self.rO = [Res() for _ in range(2)]
            self.rT = [Res() for _ in range(2)]
            self.rX = [Res() for _ in range(NT + 1)]
            self.rH = [Res() for _ in range(NT + 1)]
            self.rBC = [Res() for _ in range(6)]
            self.rConst = Res()
            self.rCbc = Res()
            self.rGates = Res()
            self.rSmall = Res()
            self.iA = 0
            self.iO = 0
            self.iT = 0
            self.out_tickets = []

            if self.part == "A":
                self.emit_A()
            elif self.part == "B":
                self.emit_B()
            else:
                self.emit_F()

            P = self.P
            P.wait_on("sp", self.out_tickets)
            sems = {n: es.enter_context(nc.semaphore(n)) for n in P.names}
            with nc.Block() as block:
                @block.tensor
                def _(e):
                    _replay(e, P.q["pe"], sems)

                @block.scalar
                def _(e):
                    _replay(e, P.q["act"], sems)

                @block.vector
                def _(e):
                    _replay(e, P.q["dve"], sems)

                @block.gpsimd
                def _(e):
                    _replay(e, P.q["pool"], sems)

                @block.sync
                def _(e):
                    _replay(e, P.q["sp"], sems)
        return nc

    def tile_np(self, t):
        return HALO if t == NT else 128

    def tok0(self, t):
        return TOK if t == NT else t * 128

    def nextA(self):
        i = self.iA
        self.iA = (i + 1) % 4
        return self.psA[i], self.rA[i]

    def nextO(self):
        i = self.iO
        self.iO = (i + 1) % 2
        return self.psO[i], self.rO[i]

    def nextT(self):
        i = self.iT
        self.iT = (i + 1) % 2
        return self.psT[i], self.rT[i]

    def dsem(self, base):
        return "d_" + base

    def load_x(self, x_in):
        for t in range(NT + 1):
            self.P.dma("sp", I("dma_start", out=self.x_tm[:, t, :], in_=x_in[t]), "d_x", writes=[self.rX[t]])

    def setup_common(self, load_x=True):
        P = self.P
        x_in = self.din("xin", [NT + 1, 128, D])
        ident_in = self.din("ident_in", [128, 128], BF16)
        cfm_in = self.din("cfm", [128, 8])
        hm_in = self.din("hmask", [128, 1])
        self.adaw = self.din("adaw", [26, 128, 8, D])
        self.adab = self.din("adab", [26, D])
        self.lng = self.din("lng", [8, D])
        self.lnb = self.din("lnb", [8, D])
        if load_x:
            self.load_x(x_in)
        P.dma("sp", I("dma_start", out=self.ident[:], in_=ident_in), "d_c", writes=[self.rConst])
        P.dma("sp", I("dma_start", out=self.cfm[:, 0:8], in_=cfm_in), "d_c", writes=[self.rSmall])
        P.dma("sp", I("dma_start", out=self.hmask[:, 0:1], in_=hm_in), "d_c", writes=[self.rConst])
        P.op("pool", I("memset", self.hmask[:, 1:2], 0.0), writes=[self.rConst])
        self.hm_ap = self.hmask[:, 0:1]
        P.op("pool", I("memset", self.ones[:], 1.0), writes=[self.rConst])
        P.op("act", I("activation", out=self.cfm[:, 8:16], in_=self.cfm[:, 0:8], func=AF.Silu),
             reads=[self.rSmall], writes=[self.rSmall])
        P.op("dve", I("tensor_copy", out=self.cbc[:], in_=self.cfm[:, 8:16].unsqueeze(2).to_broadcast([128, 8, 128])),
             reads=[self.rSmall], writes=[self.rCbc])

    def ada_vec(self, idx, slot, plus_one, wbuf, rW):
        P = self.P
        wide = wbuf.shape[2] >= 1024
        P.dma("sp", I("dma_start", out=self.bc[:, slot, :], in_=self.adab[idx:idx + 1, :].partition_broadcast(128)),
              "d_adab", writes=[self.rBC[slot]])
        for n in range(2):
            wb = wbuf[:, :, n * 512:(n + 1) * 512] if wide else wbuf[:, :, 0:512]
            P.dma("pool", I("dma_start", out=wb, in_=self.adaw[idx][:, :, n * 512:(n + 1) * 512]), "d_adaw", writes=[rW])
            pt, rp = self.nextA()
            fns = [I("matmul", pt[:], self.cbc[:, k, :], wb[:, k, :], start=(k == 0), stop=(k == 7)) for k in range(8)]
            P.op("pe", fns, reads=[rW, self.rCbc], writes=[rp])
            P.op("dve", I("scalar_tensor_tensor",
                out=self.bc[:, slot, n * 512:(n + 1) * 512], in0=pt[:], scalar=(1.0 if plus_one else 0.0),
                in1=self.bc[:, slot, n * 512:(n + 1) * 512], op0=ALU.add, op1=ALU.add),
                reads=[rp], writes=[self.rBC[slot]])

    def load_row_bc(self, src_row_ap, slot):
        self.P.dma("sp", I("dma_start", out=self.bc[:, slot, :], in_=src_row_ap.partition_broadcast(128)),
                   "d_row", writes=[self.rBC[slot]])

    def h_tile(self, t, Gs, Bs, tmpf, rtmpf, hb, rhb):
        P = self.P
        npart = self.tile_np(t)
        c0 = self.tok0(t)
        P.op("dve", I("tensor_tensor", out=tmpf[0:npart, :], in0=self.x_tm[0:npart, t, :], in1=self.bc[0:npart, Gs, :], op=ALU.mult),
             reads=[self.rX[t], self.rBC[Gs]], writes=[rtmpf])
        P.op("dve", I("tensor_tensor", out=hb[0:npart, :], in0=tmpf[0:npart, :], in1=self.bc[0:npart, Bs, :], op=ALU.add),
             reads=[rtmpf, self.rBC[Bs]], writes=[rhb])
        pt, rp = self.nextT()
        fns = [(I("transpose", pt[:, k, 0:npart], hb[0:npart, k * 128:(k + 1) * 128], self.ident[0:npart, 0:npart]))
               for k in range(8)]
        P.op("pe", fns, reads=[rhb, self.rConst], writes=[rp])
        P.op("act", I("activation", out=self.h_fm[:, :, c0:c0 + npart], in_=pt[:, :, 0:npart], func=AF.Copy),
             reads=[rp], writes=[self.rH[t]])

    def ln_tiles(self, tiles, st, rst, tmps):
        P = self.P
        for i, t in enumerate(tiles):
            npart = self.tile_np(t)
            s = st[:, t, :]
            P.op("dve", I("bn_stats", out=s[0:npart, 0:6], in_=self.x_tm[0:npart, t, 0:512]),
                 reads=[self.rX[t]], writes=[rst[t]])
            P.op("dve", I("bn_stats", out=s[0:npart, 6:12], in_=self.x_tm[0:npart, t, 512:1024]),
                 reads=[self.rX[t]], writes=[rst[t]])
            P.op("dve", I("bn_aggr", out=s[0:npart, 12:14], in_=s[0:npart, 0:12]),
                 reads=[rst[t]], writes=[rst[t]])
            P.op("dve", I("tensor_scalar", out=s[0:npart, 14:15], in0=s[0:npart, 13:14], scalar1=EPS, scalar2=None, op0=ALU.add),
                 reads=[rst[t]], writes=[rst[t]])
            P.op("act", I("activation", out=s[0:npart, 15:16], in_=s[0:npart, 14:15], func=AF.Sqrt),
                 reads=[rst[t]], writes=[rst[t]])
            P.op("dve", I("reciprocal", out=s[0:npart, 16:17], in_=s[0:npart, 15:16]),
                 reads=[rst[t]], writes=[rst[t]])
            P.op("dve", I("scalar_tensor_tensor", out=s[0:npart, 17:18], in0=s[0:npart, 12:13], scalar=-1.0, in1=s[0:npart, 16:17],
                          op0=ALU.mult, op1=ALU.mult), reads=[rst[t]], writes=[rst[t]])
            P.op("act", I("activation", out=self.x_tm[0:npart, t, :], in_=self.x_tm[0:npart, t, :], func=AF.Identity,
                          scale=s[0:npart, 16:17], bias=s[0:npart, 17:18]), reads=[rst[t], self.rX[t]], writes=[self.rX[t]])
            tmpf, rtmpf, hb, rhb = tmps[i % 2]
            self.h_tile(t, 2, 3, tmpf, rtmpf, hb, rhb)
            P.op("dve", I("tensor_tensor", out=self.x_tm[0:npart, t, :], in0=self.x_tm[0:npart, t, :], in1=self.bc[0:npart, 0, :], op=ALU.mult),
                 reads=[self.rX[t], self.rBC[0]], writes=[self.rX[t]])
            P.op("dve", I("tensor_tensor", out=self.x_tm[0:npart, t, :], in0=self.x_tm[0:npart, t, :], in1=self.bc[0:npart, 1, :], op=ALU.add),
                 reads=[self.rX[t], self.rBC[1]], writes=[self.rX[t]])

    def ln_prep(self, ln_idx, sc_idx, sh_idx, wbuf, rW):
        P = self.P
        self.load_row_bc(self.lng[ln_idx:ln_idx + 1, :], 0)
        self.load_row_bc(self.lnb[ln_idx:ln_idx + 1, :], 1)
        if sc_idx is None:
            P.op("pool", I("tensor_copy", out=self.bc[:, 2, :], in_=self.bc[:, 0, :]), reads=[self.rBC[0]], writes=[self.rBC[2]])
            P.op("pool", I("tensor_copy", out=self.bc[:, 3, :], in_=self.bc[:, 1, :]), reads=[self.rBC[1]], writes=[self.rBC[3]])
            return
        self.ada_vec(sc_idx, 2, True, wbuf, rW)
        self.ada_vec(sh_idx, 3, False, wbuf, rW)
        P.op("pool", I("tensor_tensor", out=self.bc[:, 5, :], in0=self.bc[:, 1, :], in1=self.bc[:, 2, :], op=ALU.mult),
             reads=[self.rBC[1], self.rBC[2]], writes=[self.rBC[5]])
        P.op("pool", I("tensor_tensor", out=self.bc[:, 3, :], in0=self.bc[:, 3, :], in1=self.bc[:, 5, :], op=ALU.add),
             reads=[self.rBC[3], self.rBC[5]], writes=[self.rBC[3]])
        P.op("pool", I("tensor_tensor", out=self.bc[:, 2, :], in0=self.bc[:, 0, :], in1=self.bc[:, 2, :], op=ALU.mult),
             reads=[self.rBC[0], self.rBC[2]], writes=[self.rBC[2]])

    def mk_tmps(self):
        tf, rtf = self.alloc([128, D], F32), Res()
        return [(tf, rtf, self.alloc([128, D], BF16), Res()) for _ in range(2)]

    def scale_x(self, tiles):
        for t in tiles:
            npart = self.tile_np(t)
            self.P.op("act", I("activation", out=self.x_tm[0:npart, t, :], in_=self.x_tm[0:npart, t, :], func=AF.Copy, scale=ALPHA),
                      reads=[self.rX[t]], writes=[self.rX[t]])

    def ffn_phase(self, experts, FT, n_ft, groups, g_idx, ln_idx, next_sc, next_sh, router=None):
        P = self.P
        self.arena_reset()
        fs = FT * 128
        W13 = [self.alloc([128, 8, 1024], BF16) for _ in range(2)]
        W2 = [self.alloc([128, 4, D], BF16) for _ in range(2)]
        rW13 = [Res() for _ in range(2)]
        rW2 = [Res() for _ in range(2)]
        hid = [self.alloc([128, 4, 512], BF16) for _ in range(2)]
        rhid = [Res() for _ in range(2)]
        sil = [self.alloc([128, 512], BF16) for _ in range(2)]
        rsil = [Res() for _ in range(2)]
        st = self.alloc([128, NT + 1, 32], F32)
        rst = [Res() for _ in range(NT + 1)]
        tmps = self.mk_tmps()
        all_tiles = [t for g in groups for t in g[2]]
        self.ln_prep(ln_idx, next_sc, next_sh, W13[1], rW13[1])
        self.ada_vec(g_idx, 4, True, W13[0], rW13[0])
        if router is not None:
            self.moe_router(router, W13[1], rW13[1], groups)
        self.scale_x(all_tiles)
        items = [(ei, ft, g) for ei in range(len(experts)) for ft in range(n_ft) for g in groups]
        slot_of = {}
        it_w = 0

        def load_w(ei, ft):
            nonlocal it_w
            s = it_w % 2
            it_w += 1
            w13d, w2d, _ = experts[ei]
            P.dma("pool", I("dma_start", out=W13[s][:, :, 0:2 * fs], in_=w13d[ft]), f"d_w13_{s}", writes=[rW13[s]])
            P.dma("pool", I("dma_start", out=W2[s][:, 0:FT, :], in_=w2d[ft]), f"d_w2_{s}", writes=[rW2[s]])
            P.op("pool", I("tensor_tensor", out=W2[s][:, 0:FT, :], in0=W2[s][:, 0:FT, :],
                                                   in1=self.bc[:, 4, :].unsqueeze(1).to_broadcast([128, FT, D]), op=ALU.mult),
                 reads=[self.rBC[4]], writes=[rW2[s]])
            slot_of[(ei, ft)] = s

        def phaseA(i):
            ei, ft, (c0, n, tiles) = items[i]
            s = slot_of[(ei, ft)]
            hs = i % 2
            for fc in range(FT):
                pa, ra = self.nextA()
                pb, rb = self.nextA()
                rt = [self.rH[t] for t in tiles]
                P.op("pe", [(I("matmul", pa[:, 0:n], W13[s][:, k, fc * 128:(fc + 1) * 128], self.h_fm[:, k, c0:c0 + n],
                                                    start=(k == 0), stop=(k == 7))) for k in range(8)],
                     reads=[rW13[s]] + rt, writes=[ra])
                P.op("pe", [(I("matmul", pb[:, 0:n], W13[s][:, k, fs + fc * 128:fs + (fc + 1) * 128], self.h_fm[:, k, c0:c0 + n],
                                                    start=(k == 0), stop=(k == 7))) for k in range(8)],
                     reads=[rW13[s]] + rt, writes=[rb])
                ss = fc % 2
                P.op("act", I("activation", out=sil[ss][:, 0:n], in_=pa[:, 0:n], func=AF.Silu),
                     reads=[ra], writes=[rsil[ss]])
                P.op("dve", I("tensor_tensor", out=hid[hs][:, fc, 0:n], in0=sil[ss][:, 0:n], in1=pb[:, 0:n], op=ALU.mult),
                     reads=[rsil[ss], rb], writes=[rhid[hs]])

        def phaseB(i):
            ei, ft, (c0, n, tiles) = items[i]
            s = slot_of[(ei, ft)]
            hs = i % 2
            gate = experts[ei][2]
            for j, t in enumerate(tiles):
                npart = self.tile_np(t)
                for half in range(2):
                    po, ro = self.nextO()
                    P.op("pe", [(I("matmul", po[0:npart, :], hid[hs][:, fc, j * 128:j * 128 + npart],
                                                          W2[s][:, fc, half * 512:(half + 1) * 512],
                                                          start=(fc == 0), stop=(fc == FT - 1))) for fc in range(FT)],
                         reads=[rhid[hs], rW2[s]], writes=[ro])
                    xs = self.x_tm[0:npart, t, half * 512:(half + 1) * 512]
                    if gate is None:
                        P.op("dve", I("tensor_tensor", out=xs, in0=po[0:npart, :], in1=xs, op=ALU.add),
                             reads=[ro, self.rX[t]], writes=[self.rX[t]])
                    else:
                        P.op("dve", I("scalar_tensor_tensor",
                            out=xs, in0=po[0:npart, :], scalar=self.gates[0:npart, t, gate:gate + 1], in1=xs, op0=ALU.mult, op1=ALU.add),
                            reads=[ro, self.rX[t], self.rGates], writes=[self.rX[t]])

        wl = [(ei, ft) for ei in range(len(experts)) for ft in range(n_ft)]
        ng = len(groups)
        load_w(*wl[0])
        if len(wl) > 1:
            load_w(*wl[1])
        ln_started = False
        for i in range(len(items)):
            if i == 0:
                phaseA(0)
            if i + 1 < len(items):
                phaseA(i + 1)
            phaseB(i)
            if (i + 1) % ng == 0:
                w = (i + 1) // ng - 1
                if w + 2 < len(wl):
                    load_w(*wl[w + 2])
        for (c0, n, tiles) in groups:
            self.ln_tiles(tiles, st, rst, tmps)

    def moe_router(self, router, wbuf, rW, groups):
        P = self.P
        rw_d, rb_d = router
        rwb = wbuf[:, :, 0:8]
        P.dma("pool", I("dma_start", out=rwb, in_=rw_d), "d_rw", writes=[rW])
        lg = self.small[:, 0:128].rearrange("p (t e) -> p t e", e=NE)
        wk = self.small[:, 128:256].rearrange("p (t e) -> p t e", e=NE)
        ex = self.small[:, 256:384].rearrange("p (t e) -> p t e", e=NE)
        m1 = self.small[:, 384:400]
        m2 = self.small[:, 400:416]
        ssum = self.small[:, 416:432]
        rb = self.small[:, 440:448]
        P.dma("sp", I("dma_start", out=rb, in_=rb_d.partition_broadcast(128)), "d_rb", writes=[self.rSmall])
        pt, rp = self.nextA()
        for t in range(NT):
            P.op("pe", [(I("matmul", pt[:, t * 8:(t + 1) * 8], self.h_fm[:, k, t * 128:(t + 1) * 128], rwb[:, k, :],
                                                     start=(k == 0), stop=(k == 7))) for k in range(8)],
                 reads=[rW, self.rH[t]], writes=[rp])
        rS = self.rSmall
        P.op("dve", I("tensor_tensor", out=lg, in0=pt[:, 0:128].rearrange("p (t e) -> p t e", e=NE),
                                              in1=rb.unsqueeze(1).to_broadcast([128, NT, NE]), op=ALU.add), reads=[rp, rS], writes=[rS])
        P.op("dve", I("tensor_reduce", out=m1, in_=lg, axis=AX.X, op=ALU.max), reads=[rS], writes=[rS])
        P.op("dve", I("tensor_tensor", out=wk, in0=lg, in1=m1.unsqueeze(2).to_broadcast([128, NT, NE]), op=ALU.is_ge), reads=[rS], writes=[rS])
        P.op("dve", I("scalar_tensor_tensor", out=wk, in0=wk, scalar=-1e30, in1=lg, op0=ALU.mult, op1=ALU.add), reads=[rS], writes=[rS])
        P.op("dve", I("tensor_reduce", out=m2, in_=wk, axis=AX.X, op=ALU.max), reads=[rS], writes=[rS])
        P.op("dve", I("tensor_tensor", out=wk, in0=lg, in1=m2.unsqueeze(2).to_broadcast([128, NT, NE]), op=ALU.is_ge), reads=[rS], writes=[rS])
        P.op("dve", I("tensor_tensor", out=ex, in0=lg, in1=m1.unsqueeze(2).to_broadcast([128, NT, NE]), op=ALU.subtract), reads=[rS], writes=[rS])
        P.op("act", I("activation", out=ex, in_=ex, func=AF.Exp), reads=[rS], writes=[rS])
        P.op("dve", I("tensor_tensor", out=ex, in0=ex, in1=wk, op=ALU.mult), reads=[rS], writes=[rS])
        P.op("dve", I("tensor_reduce", out=ssum, in_=ex, axis=AX.X, op=ALU.add), reads=[rS], writes=[rS])
        P.op("dve", I("reciprocal", out=ssum, in_=ssum), reads=[rS], writes=[rS])
        P.op("dve", I("tensor_tensor", out=self.gates[:], in0=ex, in1=ssum.unsqueeze(2).to_broadcast([128, NT, NE]), op=ALU.mult),
             reads=[rS], writes=[self.rGates])

    def conv_phase(self, l, g1_idx, ln_idx, next_sc, next_sh):
        P = self.P
        self.arena_reset()
        ciw_d = self.dram["conv_in_w"]
        cow_d = self.dram["conv_out_w"]
        cvec_d = self.dram["cvec"]
        cob_d = self.dram["conv_out_b"]
        Wci = self.alloc([128, 8, 2048], BF16)
        Wco = self.alloc([128, 8, D], BF16)
        rWci, rWco = Res(), Res()
        U = [self.alloc([128, 30 + 128], F32) for _ in range(8)]
        V = [self.alloc([128, 128], F32) for _ in range(8)]
        rU = [Res() for _ in range(8)]
        rV = [Res() for _ in range(8)]
        vb = [self.alloc([128, 128], BF16) for _ in range(2)]
        vq = [self.alloc([128, 128], BF16) for _ in range(2)]
        rvb = [Res() for _ in range(2)]
        rvq = [Res() for _ in range(2)]
        sig = [self.alloc([128, 128], F32) for _ in range(2)]
        rsig = [Res() for _ in range(2)]
        t1 = [self.alloc([128, 128], F32) for _ in range(2)]
        rt1 = [Res() for _ in range(2)]
        pk = [self.alloc([128, 128], BF16) for _ in range(4)]
        rpk = [Res() for _ in range(4)]
        self.pk_i = 0
        stt_ = self.alloc([128, 4, 128], F32)
        rstt = Res()
        cv = self.alloc([128, 72], F32)
        dww = self.alloc([128, 8, CK], F32)
        rcv = Res()
        st = self.alloc([128, NT + 1, 32], F32)
        rst = [Res() for _ in range(NT + 1)]
        tmps = self.mk_tmps()

        self.ln_prep(ln_idx, next_sc, next_sh, Wci[:, :, 0:D], rWci)
        self.ada_vec(g1_idx, 4, True, Wco, rWco)
        P.dma("pool", I("dma_start", out=Wci, in_=ciw_d[l]), "d_wci", writes=[rWci])
        P.dma("pool", I("dma_start", out=Wco, in_=cow_d[l]), "d_wco", writes=[rWco])
        P.op("pool", I("tensor_tensor", out=Wco, in0=Wco, in1=self.bc[:, 4, :].unsqueeze(1).to_broadcast([128, 8, D]), op=ALU.mult),
             reads=[self.rBC[4]], writes=[rWco])
        P.dma("sp", I("dma_start", out=cv[:, 0:40], in_=cvec_d[l]), "d_cv", writes=[rcv])
        P.dma("sp", I("dma_start", out=dww, in_=self.dram["dww"][l]), "d_cv2", writes=[rcv])
        self.load_row_bc(cob_d[l:l + 1, :], 5)
        P.op("pool", I("tensor_tensor", out=self.bc[:, 5, :], in0=self.bc[:, 5, :], in1=self.bc[:, 4, :], op=ALU.mult),
             reads=[self.rBC[4], self.rBC[5]], writes=[self.rBC[5]])
        for c in range(8):
            P.op("pool", I("memset", U[c][:, 0:30], 0.0), writes=[rU[c]])
        for t in range(NT + 1):
            npart = self.tile_np(t)
            P.op("dve", I("scalar_tensor_tensor",
                out=self.x_tm[0:npart, t, :], in0=self.x_tm[0:npart, t, :], scalar=ALPHA, in1=self.bc[0:npart, 5, :],
                op0=ALU.mult, op1=ALU.add), reads=[self.rX[t], self.rBC[5]], writes=[self.rX[t]])

        order = [NT] + list(range(NT))
        for gi, t in enumerate(order):
            n = self.tile_np(t)
            c0 = self.tok0(t)
            rht = self.rH[t]
            for c in range(8):
                pa, ra = self.nextA()
                pg, rg = self.nextA()
                P.op("pe", [(I("matmul", pa[:, 0:n], Wci[:, k, c * 128:(c + 1) * 128], self.h_fm[:, k, c0:c0 + n],
                                                    start=(k == 0), stop=(k == 7))) for k in range(8)], reads=[rWci, rht], writes=[ra])
                P.op("pe", [(I("matmul", pg[:, 0:n], Wci[:, k, D + c * 128:D + (c + 1) * 128], self.h_fm[:, k, c0:c0 + n],
                                                    start=(k == 0), stop=(k == 7))) for k in range(8)], reads=[rWci, rht], writes=[rg])
                ss = c % 2
                P.op("act", I("activation", out=sig[ss][:, 0:n], in_=pg[:, 0:n], func=AF.Sigmoid, bias=cv[:, 8 + c:9 + c]),
                     reads=[rg, rcv], writes=[rsig[ss]])
                P.op("dve", I("scalar_tensor_tensor", out=U[c][:, 30:30 + n], in0=pa[:, 0:n], scalar=cv[:, c:c + 1],
                                                                        in1=sig[ss][:, 0:n], op0=ALU.add, op1=ALU.mult),
                     reads=[ra, rsig[ss], rcv], writes=[rU[c]])
                if t == NT:
                    P.op("dve", I("tensor_scalar", out=U[c][:, 30:30 + n], in0=U[c][:, 30:30 + n], scalar1=self.hm_ap,
                                                              scalar2=None, op0=ALU.mult), reads=[rU[c], self.rConst], writes=[rU[c]])
            for k in range(CK):
                for c in (0, 1, 2, 3):
                    if k == 0:
                        P.op("dve", I("tensor_scalar", out=V[c][:, 0:n], in0=U[c][:, 0:n], scalar1=dww[:, c, 0:1],
                                      scalar2=cv[:, 16 + c:17 + c], op0=ALU.mult, op1=ALU.add),
                             reads=[rU[c], rcv], writes=[rV[c]])
                    else:
                        P.op("dve", I("scalar_tensor_tensor", out=V[c][:, 0:n], in0=U[c][:, k:k + n], scalar=dww[:, c, k:k + 1],
                                      in1=V[c][:, 0:n], op0=ALU.mult, op1=ALU.add),
                             reads=[rU[c], rcv, rV[c]], writes=[rV[c]])
            for c in (4, 5, 6, 7):
                pf, rpf = self.nextA()
                for k in range(CK):
                    r = self.pk_i % 4
                    self.pk_i += 1
                    P.op("act", I("activation", out=pk[r][:, 0:n], in_=U[c][:, k:k + n], func=AF.Identity, scale=dww[:, c, k:k + 1]),
                         reads=[rU[c], rcv], writes=[rpk[r]])
                    P.op("pe", I("matmul", pf[:, 0:n], self.ident[:], pk[r][:, 0:n], start=(k == 0), stop=(k == CK - 1)),
                         reads=[rpk[r], self.rConst], writes=[rpf], noself=(k > 0))
                P.op("act", I("activation", out=V[c][:, 0:n], in_=pf[:, 0:n], func=AF.Identity, bias=cv[:, 16 + c:17 + c]),
                     reads=[rpf, rcv], writes=[rV[c]])
            for c in range(8):
                P.op("act", I("activation", out=U[c][:, 0:30], in_=U[c][:, n:n + 30], func=AF.Copy), reads=[rU[c]], writes=[rU[c]])
            pS1, rS1 = self.nextA()
            pS2, rS2 = self.nextA()
            for c in range(8):
                ss = c % 2
                P.op("act", I("activation", out=vb[ss][:, 0:n], in_=V[c][:, 0:n], func=AF.Copy), reads=[rV[c]], writes=[rvb[ss]])
                P.op("act", I("activation", out=vq[ss][:, 0:n], in_=V[c][:, 0:n], func=AF.Square), reads=[rV[c]], writes=[rvq[ss]])
                P.op("pe", I("matmul", pS1[:, 0:n], self.ones[:], vb[ss][:, 0:n], start=(c == 0), stop=(c == 7)),
                     reads=[rvb[ss], self.rConst], writes=[rS1])
                P.op("pe", I("matmul", pS2[:, 0:n], self.ones[:], vq[ss][:, 0:n], start=(c == 0), stop=(c == 7)),
                     reads=[rvq[ss], self.rConst], writes=[rS2])
            mean, msq, sq, rstd = stt_[:, 0, 0:n], stt_[:, 1, 0:n], stt_[:, 2, 0:n], stt_[:, 3, 0:n]
            P.op("dve", I("tensor_scalar", out=mean, in0=pS1[:, 0:n], scalar1=1.0 / D, scalar2=None, op0=ALU.mult), reads=[rS1], writes=[rstt])
            P.op("dve", I("tensor_tensor", out=msq, in0=mean, in1=mean, op=ALU.mult), reads=[rstt], writes=[rstt])
            P.op("dve", I("scalar_tensor_tensor", out=msq, in0=pS2[:, 0:n], scalar=1.0 / D, in1=msq, op0=ALU.mult, op1=ALU.subtract),
                 reads=[rS2, rstt], writes=[rstt])
            P.op("dve", I("tensor_scalar", out=msq, in0=msq, scalar1=0.0, scalar2=EPS, op0=ALU.max, op1=ALU.add), reads=[rstt], writes=[rstt])
            P.op("act", I("activation", out=sq, in_=msq, func=AF.Sqrt), reads=[rstt], writes=[rstt])
            P.op("dve", I("reciprocal", out=rstd, in_=sq), reads=[rstt], writes=[rstt])
            for c in range(8):
                ss = c % 2
                P.op("dve", I("tensor_tensor", out=t1[ss][:, 0:n], in0=V[c][:, 0:n], in1=mean, op=ALU.subtract),
                     reads=[rV[c], rstt], writes=[rt1[ss]])
                P.op("pool", I("tensor_tensor", out=t1[ss][:, 0:n], in0=t1[ss][:, 0:n], in1=rstd, op=ALU.mult),
                     reads=[rt1[ss], rstt], writes=[rt1[ss]])
                P.op("act", I("activation", out=self.h_fm[:, c, c0:c0 + n], in_=t1[ss][:, 0:n], func=AF.Silu,
                                                              scale=cv[:, 24 + c:25 + c], bias=cv[:, 32 + c:33 + c]),
                     reads=[rt1[ss], rcv], writes=[rht])
            for half in range(2):
                po, ro = self.nextO()
                fns = [(I("matmul", po[0:n, :], self.h_fm[:, c, c0:c0 + n], Wco[:, c, half * 512:(half + 1) * 512],
                                               start=(c == 0), stop=(c == 7))) for c in range(8)]
                P.op("pe", fns, reads=[rht, rWco], writes=[ro])
                xs = self.x_tm[0:n, t, half * 512:(half + 1) * 512]
                P.op("dve", I("tensor_tensor", out=xs, in0=po[0:n, :], in1=xs, op=ALU.add),
                     reads=[ro, self.rX[t]], writes=[self.rX[t]])
            self.ln_tiles([t], st, rst, tmps)

    def decl_A(self):
        self.din("conv_in_w", [2, 128, 8, 2048])
        self.din("conv_out_w", [2, 128, 8, D])
        self.din("cvec", [2, 128, 40])
        self.din("dww", [2, 128, 8, CK])
        self.din("conv_out_b", [2, D])
        self.din("ffn13", [11, 128, 8, 512])
        self.din("ffn2", [11, 128, 2, D])
        self.din("moe13", [NE * 7, 128, 8, 1024])
        self.din("moe2", [NE * 7, 128, 4, D])
        self.din("router_w", [128, 8, NE])
        self.din("router_b", [1, NE])
        self.din("wkv", [128, 8, 2048])

    def body_A(self, kt_out, v_out, kv_res=None):
        dr = self.dram
        ffn13, ffn2, moe13, moe2 = dr["ffn13"], dr["ffn2"], dr["moe13"], dr["moe2"]
        main_groups = [(g * 512, 512, [4 * g + j for j in range(4)]) for g in range(4)]
        halo_group = (TOK, HALO, [NT])
        self.arena_reset()
        tmps = self.mk_tmps()
        abuf, rab = self.alloc([128, 8, D], BF16), Res()
        self.ada_vec(0 * 6 + 1, 2, True, abuf, rab)
        self.ada_vec(0 * 6 + 0, 3, False, abuf, rab)
        for i, t in enumerate([NT] + list(range(NT))):
            self.h_tile(t, 2, 3, *tmps[i % 2])
        for l in range(2):
            b = l * 6
            self.conv_phase(l, b + 2, l * 2 + 0, b + 4, b + 3)
            if l == 0:
                experts = [(ffn13, ffn2, None)]
                self.ffn_phase(experts, 2, 11, [halo_group] + main_groups, b + 5, l * 2 + 1, 6 + 1, 6 + 0)
            else:
                experts = [(moe13[e * 7:(e + 1) * 7], moe2[e * 7:(e + 1) * 7], e) for e in range(NE)]
                self.ffn_phase(experts, 4, 7, main_groups, b + 5, l * 2 + 1, 25, 24, router=(dr["router_w"], dr["router_b"]))
        self.kv_phase(dr["wkv"], kt_out, v_out, kv_res)

    def emit_A(self):
        P = self.P
        self.setup_common()
        self.decl_A()
        xout = self.dout("xout", [NT, 128, D])
        kt_out = self.dout("kt_out", [H, DH, TOK], BF16)
        v_out = self.dout("v_out", [NT, 128, D], BF16)
        self.body_A(kt_out, v_out)
        for t in range(NT):
            self.out_tickets.append(P.dma("sp", I("dma_start", out=xout[t], in_=self.x_tm[:, t, :]), "d_out", reads=[self.rX[t]]))

    def emit_F(self):
        P = self.P
        nc = self.nc
        self.setup_common(load_x=False)
        self.decl_A()
        self.decl_B("b")
        xprev = self.din("xin_prev", [NT + 1, 128, D])
        yout = self.dout("yout", [NT, 128, D])
        kt_p = nc.dram_tensor("kt_prev_scr", [H, DH, TOK], BF16).ap()
        v_p = nc.dram_tensor("v_prev_scr", [NT, 128, D], BF16).ap()
        kt_o = nc.dram_tensor("kt_own_scr", [H, DH, TOK], BF16).ap()
        v_o = nc.dram_tensor("v_own_scr", [NT, 128, D], BF16).ap()
        res_p, res_o = [], []
        self.load_x(xprev)
        self.hm_ap = self.hmask[:, 1:2]
        self.body_A(kt_p, v_p, res_p)
        self.load_x(self.dram["xin"])
        self.hm_ap = self.hmask[:, 0:1]
        self.body_A(kt_o, v_o, res_o)

        def kv_load(h, s, Kb, Vb, rK, rV):
            P.dma("sp", I("dma_start", out=Kb[s][0:DH, 0:TOK], in_=kt_p[h]), f"d_k{s}", reads=res_p, writes=[rK[s]])
            P.dma("sp", I("dma_start", out=Kb[s][0:DH, TOK:2 * TOK], in_=kt_o[h]), f"d_k{s}", reads=res_o, writes=[rK[s]])
            vp = v_p.rearrange("t p (h d) -> h p t d", d=DH)
            vo = v_o.rearrange("t p (h d) -> h p t d", d=DH)
            P.dma("sp", I("dma_start", out=Vb[s][:, 0:16, 0:DH], in_=vp[h]), f"d_v{s}", reads=res_p, writes=[rV[s]])
            P.dma("sp", I("dma_start", out=Vb[s][:, 16:32, 0:DH], in_=vo[h]), f"d_v{s}", reads=res_o, writes=[rV[s]])

        self.body_B(yout, kv_load, "b", ones_col=True)

    def kv_phase(self, wkv_d, kt_out, v_out, kv_res=None):
        P = self.P
        self.arena_reset()
        Wkv = self.alloc([128, 8, 2048], BF16)
        rW = Res()
        kts = [self.alloc([128, 512], BF16) for _ in range(2)]
        rk = [Res() for _ in range(2)]
        vts = [self.alloc([128, D], BF16) for _ in range(2)]
        rv = [Res() for _ in range(2)]
        P.dma("pool", I("dma_start", out=Wkv, in_=wkv_d), "d_wkv", writes=[rW])
        i = 0
        for g in range(4):
            c0 = g * 512
            rt = [self.rH[4 * g + j] for j in range(4)]
            for c in range(8):
                pa, ra = self.nextA()
                P.op("pe", [(I("matmul", pa[:], Wkv[:, k, c * 128:(c + 1) * 128], self.h_fm[:, k, c0:c0 + 512],
                                                    start=(k == 0), stop=(k == 7))) for k in range(8)], reads=[rW] + rt, writes=[ra])
                s = i % 2
                i += 1
                P.op("act", I("activation", out=kts[s][:], in_=pa[:], func=AF.Copy), reads=[ra], writes=[rk[s]])
                for hh in range(2):
                    self._kv_out(P.dma("sp", I("dma_start",
                        out=kt_out[2 * c + hh, :, c0:c0 + 512], in_=kts[s][hh * 64:(hh + 1) * 64, :]), f"d_ko{s}", reads=[rk[s]]), kv_res)
        for t in range(NT):
            s = t % 2
            for half in range(2):
                po, ro = self.nextO()
                P.op("pe", [(I("matmul", po[:], self.h_fm[:, k, t * 128:(t + 1) * 128], Wkv[:, k, D + half * 512:D + (half + 1) * 512],
                                                    start=(k == 0), stop=(k == 7))) for k in range(8)], reads=[rW, self.rH[t]], writes=[ro])
                P.op("act", I("activation", out=vts[s][:, half * 512:(half + 1) * 512], in_=po[:], func=AF.Copy),
                     reads=[ro], writes=[rv[s]])
            self._kv_out(P.dma("sp", I("dma_start", out=v_out[t], in_=vts[s][:]), f"d_vo{s}", reads=[rv[s]]), kv_res)

    def _kv_out(self, ticket, kv_res):
        if kv_res is None:
            self.out_tickets.append(ticket)
        else:
            r = Res()
            r.w = ticket
            kv_res.append(r)

    def decl_B(self, sfx=""):
        self.din("wq", [2, 128, 8, D])
        self.din("wo", [2, 128, 8, D])
        self.din("ffn13" + sfx, [11, 128, 8, 512])
        self.din("ffn2" + sfx, [11, 128, 2, D])
        self.din("moe13" + sfx, [NE * 7, 128, 8, 1024])
        self.din("moe2" + sfx, [NE * 7, 128, 4, D])
        self.din("router_w" + sfx, [128, 8, NE])
        self.din("router_b" + sfx, [1, NE])
        self.din("kconst", [128, 2 * TOK], BF16)
        self.din("negelig", [128, 256])
        self.din("elig01", [128, 256])
        self.din("dbase", [128, 256])
        self.din("tri", [128, 128], BF16)

    def body_B(self, yout, kv_load, sfx="", ones_col=False):
        P = self.P
        dr = self.dram
        ffn13, ffn2, moe13, moe2 = dr["ffn13" + sfx], dr["ffn2" + sfx], dr["moe13" + sfx], dr["moe2" + sfx]
        main_groups = [(g * 512, 512, [4 * g + j for j in range(4)]) for g in range(4)]
        self.arena_reset()
        tmps = self.mk_tmps()
        abuf, rab = self.alloc([128, 8, D], BF16), Res()
        self.ada_vec(2 * 6 + 1, 2, True, abuf, rab)
        self.ada_vec(2 * 6 + 0, 3, False, abuf, rab)
        for i, t in enumerate(range(NT)):
            self.h_tile(t, 2, 3, *tmps[i % 2])
        for l in (2, 3):
            b = l * 6
            self.attn_phase(l - 2, dr["wq"], dr["wo"], b + 2, l * 2 + 0, b + 4, b + 3, kv_load, ones_col)
            if l == 2:
                self.ffn_phase([(ffn13, ffn2, None)], 2, 11, main_groups, b + 5, l * 2 + 1, 18 + 1, 18 + 0)
            else:
                experts = [(moe13[e * 7:(e + 1) * 7], moe2[e * 7:(e + 1) * 7], e) for e in range(NE)]
                self.ffn_phase(experts, 4, 7, main_groups, b + 5, l * 2 + 1, None, None,
                               router=(dr["router_w" + sfx], dr["router_b" + sfx]))
        for t in range(NT):
            self.out_tickets.append(P.dma("sp", I("dma_start", out=yout[t], in_=self.x_tm[:, t, :]), "d_out", reads=[self.rX[t]]))

    def emit_B(self):
        P = self.P
        self.setup_common()
        self.decl_B()
        ktf = self.din("kt_full", [H, DH, 2 * TOK], BF16)
        vf = self.din("v_full", [H, 128, 32, 65], BF16)
        yout = self.dout("yout", [NT, 128, D])

        def kv_load(h, s, Kb, Vb, rK, rV):
            P.dma("sp", I("dma_start", out=Kb[s][0:DH, :], in_=ktf[h]), f"d_k{s}", writes=[rK[s]])
            P.dma("sp", I("dma_start", out=Vb[s], in_=vf[h]), f"d_v{s}", writes=[rV[s]])

        self.body_B(yout, kv_load)

    def attn_phase(self, j, wq_d, wo_d, g1_idx, ln_idx, next_sc, next_sh, kv_load, ones_col=False):
        P = self.P
        self.arena_reset()
        Wqh = [self.alloc([128, 8, DH], BF16) for _ in range(2)]
        rWq = [Res() for _ in range(2)]
        Kb = [self.alloc([128, 2 * TOK], BF16) for _ in range(2)]
        kb_off = self.last_off - 2 * TOK
        rK = [Res() for _ in range(2)]
        Vb = [self.alloc([128, 32, 65], BF16) for _ in range(2)]
        vb_off = self.last_off - 32 * 65
        rV = [Res() for _ in range(2)]
        qa, rqa = self.alloc([128, TOK], BF16), Res()
        attn = self.alloc([128, NT, D], BF16)
        rAt = [Res() for _ in range(NT)]
        Rst, rRst = self.alloc([128, NT, 64], BF16), Res()
        PT = [self.alloc([128, 2, 256], BF16) for _ in range(3)]
        rPT = [Res() for _ in range(3)]
        gm = self.alloc([128, NT, 16], F32)
        cur = self.alloc([128, NT, 16], F32)
        eq = self.alloc([128, NT, 16], F32)
        mx = self.alloc([128, NT], F32)
        rG = Res()
        neg = self.alloc([128, NT, 16], F32)
        el = self.alloc([128, NT, 16], F32)
        db = self.alloc([128, NT, 16], F32)
        tri = self.alloc([128, 128], BF16)
        km = self.alloc([128, 16], F32)
        kmb = self.alloc([128, 16], BF16)
        rkm = Res()
        rec = self.alloc([128, 2], F32)
        rrec = Res()
        rC = Res()
        for s in range(2):
            P.dma("sp", I("dma_start", out=Kb[s], in_=self.dram["kconst"]), "d_kc", writes=[rK[s]])
        f3 = lambda ap: ap.rearrange("p (a b) -> p a b", b=16)
        P.dma("sp", I("dma_start", out=neg, in_=f3(self.dram["negelig"])), "d_ac", writes=[rC])
        P.dma("sp", I("dma_start", out=el, in_=f3(self.dram["elig01"])), "d_ac", writes=[rC])
        P.dma("sp", I("dma_start", out=db, in_=f3(self.dram["dbase"])), "d_ac", writes=[rC])
        P.dma("sp", I("dma_start", out=tri, in_=self.dram["tri"]), "d_ac", writes=[rC])
        P.op("pool", I("memset", Rst, 0.0), writes=[rRst])
        if ones_col:
            for s_ in range(2):
                P.op("pool", I("memset", Vb[s_][:, :, DH:DH + 1], 1.0), writes=[rV[s_]])
        rHall = [self.rH[t] for t in range(NT)]
        it_s = 0
        for h in range(H):
            s = h % 2
            slope = float(2.0 ** (-8.0 * (h + 1) / H))
            s1 = float(np.float32(slope).astype(ml_dtypes.bfloat16).astype(np.float32))
            s2 = float(np.float32(slope - s1).astype(ml_dtypes.bfloat16).astype(np.float32))
            s3 = float(np.float32(slope - s1 - s2).astype(ml_dtypes.bfloat16).astype(np.float32))
            P.dma("pool", I("dma_start", out=Wqh[s], in_=wq_d[j][:, :, h * DH:(h + 1) * DH]), f"d_wq{s}", writes=[rWq[s]])
            kv_load(h, s, Kb, Vb, rK, rV)
            for g in range(4):
                pa, ra = self.nextA()
                P.op("pe", [I("matmul", pa[0:DH, :], Wqh[s][:, k, :], self.h_fm[:, k, g * 512:(g + 1) * 512], start=(k == 0), stop=(k == 7))
                            for k in range(8)], reads=[rWq[s]] + rHall[4 * g:4 * g + 4], writes=[ra])
                P.op("act", I("activation", out=qa[0:DH, g * 512:(g + 1) * 512], in_=pa[0:DH, :], func=AF.Copy, scale=float(DH ** -0.5)),
                     reads=[ra], writes=[rqa])
            P.op("dve", I("tensor_reduce", out=km[0:DH, :], in_=Kb[s][0:DH, :].rearrange("p (a b) -> p a b", b=BLK), axis=AX.X, op=ALU.add),
                 reads=[rK[s]], writes=[rkm])
            P.op("dve", I("tensor_copy", out=kmb[0:DH, :], in_=km[0:DH, :]), reads=[rkm], writes=[rkm])
            pg, rg = self.nextA()
            P.op("pe", [I("matmul", pg[:, t * 16:(t + 1) * 16], qa[0:DH, t * 128:(t + 1) * 128], kmb[0:DH, :], start=True, stop=True)
                        for t in range(NT)], reads=[rqa, rkm], writes=[rg])
            pg3 = pg[:, 0:256].rearrange("p (a b) -> p a b", b=16)
            bcast = lambda v: v.unsqueeze(2).to_broadcast([128, NT, 16])
            P.op("dve", I("tensor_tensor", out=gm, in0=pg3, in1=neg, op=ALU.add), reads=[rg, rC], writes=[rG])
            P.op("dve", I("tensor_copy", out=cur, in_=gm), reads=[rG], writes=[rG])
            for r in range(4):
                P.op("dve", I("tensor_reduce", out=mx, in_=cur, axis=AX.X, op=ALU.max), reads=[rG], writes=[rG])
                if r < 3:
                    P.op("dve", I("tensor_tensor", out=eq, in0=cur, in1=bcast(mx), op=ALU.is_ge), reads=[rG], writes=[rG])
                    P.op("dve", I("scalar_tensor_tensor", out=cur, in0=eq, scalar=-3e30, in1=cur, op0=ALU.mult, op1=ALU.add), reads=[rG], writes=[rG])
            P.op("dve", I("tensor_tensor", out=eq, in0=gm, in1=bcast(mx), op=ALU.is_ge), reads=[rG], writes=[rG])
            P.op("dve", I("tensor_tensor", out=eq, in0=eq, in1=el, op=ALU.mult), reads=[rG, rC], writes=[rG])
            P.op("dve", I("tensor_scalar", out=eq, in0=eq, scalar1=-1.0, scalar2=BIG, op0=ALU.add, op1=ALU.mult), reads=[rG], writes=[rG])
            P.op("dve", I("scalar_tensor_tensor", out=cur, in0=db, scalar=slope, in1=eq, op0=ALU.mult, op1=ALU.add), reads=[rG, rC], writes=[rG])
            P.op("dve", I("tensor_copy", out=Rst[:, :, 0:16], in_=cur), reads=[rG], writes=[rRst])
            P.op("dve", I("tensor_tensor", out=Rst[:, :, 32:48], in0=cur, in1=Rst[:, :, 0:16], op=ALU.subtract), reads=[rG, rRst], writes=[rRst])
            for ci, sv in enumerate((s1, s2, s3)):
                P.op("pool", I("memset", Rst[:, :, 16 + ci:17 + ci], sv), writes=[rRst])
            for g in range(4):
                pa, ra = self.nextA()
                P.op("pe", [I("matmul", pa[0:64, i * 128:(i + 1) * 128], Rst[:, 4 * g + i, :], self.ident[:], start=True, stop=True)
                            for i in range(4)], reads=[rRst, self.rConst], writes=[ra])
                P.op("dve", I("tensor_copy", out=qa[64:128, g * 512:(g + 1) * 512], in_=pa[0:64, :]), reads=[ra], writes=[rqa])
            items = [(b, jb) for b in range(8) for jb in range(9 + b)]
            accs = {}
            st_of = {}

            def qk_exp(i):
                nonlocal it_s
                b, jb = items[i]
                own = (jb == 8 + b)
                ps_, rs_ = self.nextA()
                S = ps_[:].rearrange("p (a b) -> p a b", b=256)
                q0 = b * 256
                fns = []
                if not own:
                    for kt in range(2):
                        fns.append(I("matmul", S[:, kt, :], Kb[s][:, (2 * jb + kt) * 128:(2 * jb + kt + 1) * 128], qa[:, q0:q0 + 256], start=True, stop=True))
                else:
                    fns.append(I("matmul", S[:, 0, :], Kb[s][:, (2 * jb) * 128:(2 * jb + 1) * 128], qa[:, q0:q0 + 256], start=True, stop=False))
                    fns.append(I("matmul", S[:, 0, 0:128], self.ident[:], tri, start=False, stop=True))
                    fns.append(I("matmul", S[:, 1, 128:256], Kb[s][:, (2 * jb + 1) * 128:(2 * jb + 2) * 128], qa[:, q0 + 128:q0 + 256], start=True, stop=False))
                    fns.append(I("matmul", S[:, 1, 128:256], self.ident[:], tri, start=False, stop=True))
                P.op("pe", fns, reads=[rK[s], rqa, rC, self.rConst], writes=[rs_])
                ts_ = it_s % 3
                it_s += 1
                st_of[i] = ts_
                if not own:
                    P.op("act", I("activation", out=PT[ts_], in_=S, func=AF.Exp), reads=[rs_], writes=[rPT[ts_]])
                else:
                    P.op("act", I("activation", out=PT[ts_][:, 0, :], in_=S[:, 0, :], func=AF.Exp), reads=[rs_], writes=[rPT[ts_]])
                    P.op("act", I("activation", out=PT[ts_][:, 1, 128:256], in_=S[:, 1, 128:256], func=AF.Exp), reads=[rs_], writes=[rPT[ts_]])

            def pv(i):
                b, jb = items[i]
                own = (jb == 8 + b)
                if jb == 0:
                    po, ro = self.nextO()
                    accs[b] = (po[:, 0:130].rearrange("p (a b) -> p a b", b=65), ro)
                acc, ro = accs[b]
                ts_ = st_of[i]
                fns = []
                for qi in range(2):
                    for kt in range(2):
                        if own and kt == 1 and qi == 0:
                            continue
                        last = own and ((qi == 0 and kt == 0) or (qi == 1 and kt == 1))
                        fns.append(I("matmul", acc[:, qi, :], PT[ts_][:, kt, qi * 128:(qi + 1) * 128], Vb[s][:, 2 * jb + kt, :],
                                     start=(jb == 0 and kt == 0), stop=last))
                P.op("pe", fns, reads=[rPT[ts_], rV[s]], writes=[ro], noself=(jb > 0))
                if own:
                    P.op("dve", I("reciprocal", out=rec, in_=acc[:, :, 64]), reads=[ro], writes=[rrec])
                    for qi in range(2):
                        t = 2 * b + qi
                        P.op("dve", I("tensor_scalar", out=attn[:, t, h * DH:(h + 1) * DH], in0=acc[:, qi, 0:64], scalar1=rec[:, qi:qi + 1],
                                      scalar2=None, op0=ALU.mult), reads=[ro, rrec], writes=[rAt[t]])

            qk_exp(0)
            for i in range(len(items)):
                if i + 1 < len(items):
                    qk_exp(i + 1)
                pv(i)
        self.P.barrier()
        Wo, rWo = self.view(kb_off, [128, 8, D], BF16), Res()
        abuf, rab = self.view(vb_off, [128, 8, 512], BF16), Res()
        self.ln_prep(ln_idx, next_sc, next_sh, abuf, rab)
        self.ada_vec(g1_idx, 4, True, abuf, rab)
        P.dma("pool", I("dma_start", out=Wo, in_=wo_d[j]), "d_wo", writes=[rWo])
        P.op("pool", I("tensor_tensor", out=Wo, in0=Wo, in1=self.bc[:, 4, :].unsqueeze(1).to_broadcast([128, 8, D]), op=ALU.mult),
             reads=[self.rBC[4]], writes=[rWo])
        self.scale_x(list(range(NT)))
        for t in range(NT):
            pt, rp = self.nextT()
            P.op("pe", [I("transpose", pt[:, k, :], attn[:, t, k * 128:(k + 1) * 128], self.ident[:]) for k in range(8)],
                 reads=[rAt[t], self.rConst], writes=[rp])
            P.op("act", I("activation", out=self.h_fm[:, :, t * 128:(t + 1) * 128], in_=pt[:], func=AF.Copy), reads=[rp], writes=[self.rH[t]])
            for half in range(2):
                po, ro = self.nextO()
                P.op("pe", [I("matmul", po[:], self.h_fm[:, c, t * 128:(t + 1) * 128], Wo[:, c, half * 512:(half + 1) * 512],
                              start=(c == 0), stop=(c == 7)) for c in range(8)], reads=[self.rH[t], rWo], writes=[ro])
                xs = self.x_tm[:, t, half * 512:(half + 1) * 512]
                P.op("dve", I("tensor_tensor", out=xs, in0=po[:], in1=xs, op=ALU.add), reads=[ro, self.rX[t]], writes=[self.rX[t]])
        self.P.barrier()
        st = Vb[0].rearrange("p a b -> p (a b)")[:, 0:(NT + 1) * 64].bitcast(F32).rearrange("p (a b) -> p a b", b=32)
        rst = [Res() for _ in range(NT + 1)]
        tf = qa.bitcast(F32)
        hb0 = PT[0].rearrange("p a b -> p (a b)")
        tmps = [(tf, Res(), attn[:, 0, :], Res()), (tf, None, attn[:, 1, :], Res())]
        tmps[1] = (tf, tmps[0][1], attn[:, 1, :], Res())
        self.ln_tiles(list(range(NT)), st, rst, tmps)

def _fm(w):
    k = w.shape[0] // 128
    return np.ascontiguousarray(w.reshape(k, 128, w.shape[1]).transpose(1, 0, 2))


def _ffn_tiles(w13, w2, F, fs):
    n_ft = F // fs
    FT = fs // 128
    a, b = w13[:, :F], w13[:, F:]
    t13 = np.stack([_fm(np.concatenate([a[:, i * fs:(i + 1) * fs], b[:, i * fs:(i + 1) * fs]], axis=1)) for i in range(n_ft)])
    t2 = np.stack([np.ascontiguousarray(w2[i * fs:(i + 1) * fs].reshape(FT, 128, D).transpose(1, 0, 2)) for i in range(n_ft)])
    return t13, t2


def _shared_common(inp):
    f32 = np.float32
    sh = {}
    pieces = []
    for l in range(DEPTH):
        for j in range(6):
            pieces.append(_fm(inp["ada_w"][l][:, j * D:(j + 1) * D]))
    for j in range(2):
        pieces.append(_fm(inp["kv_ada_w"][:, j * D:(j + 1) * D]))
    sh["adaw"] = np.stack(pieces).astype(f32)
    sh["adab"] = np.concatenate([inp["ada_b"].reshape(DEPTH * 6, D), inp["kv_ada_b"].reshape(2, D)], axis=0).astype(f32)
    sh["lng"] = np.ascontiguousarray(inp["ln_g"].reshape(8, D)).astype(f32)
    sh["lnb"] = np.ascontiguousarray(inp["ln_b"].reshape(8, D)).astype(f32)
    sh["ident_in"] = np.eye(128, dtype=f32).astype(ml_dtypes.bfloat16)
    return sh


def _ffn_moe(inp, i, sh):
    f32 = np.float32
    sh["ffn13"], sh["ffn2"] = _ffn_tiles(inp["ffn_w13"][i], inp["ffn_w2"][i], FD, 256)
    m13, m2 = [], []
    for e in range(NE):
        a, b = _ffn_tiles(inp["moe_w13"][i][e], inp["moe_w2"][i][e], FE, 512)
        m13.append(a)
        m2.append(b)
    sh["moe13"] = np.concatenate(m13, axis=0)
    sh["moe2"] = np.concatenate(m2, axis=0)
    sh["router_w"] = _fm(inp["router_w"][i]).astype(f32)
    sh["router_b"] = inp["router_b"][i].reshape(1, NE).astype(f32)


def _shared_A(inp):
    f32 = np.float32
    sh = _shared_common(inp)
    sh["conv_in_w"] = np.stack([_fm(inp["conv_in_w"][l]) for l in range(2)]).astype(f32)
    sh["conv_out_w"] = np.stack([_fm(inp["conv_out_w"][l]) for l in range(2)]).astype(f32)
    cvec = np.zeros((2, 128, 40), f32)
    for l in range(2):
        cvec[l, :, 0:16] = inp["conv_in_b"][l].reshape(16, 128).T
        cvec[l, :, 16:24] = inp["conv_dw_b"][l].reshape(8, 128).T
        cvec[l, :, 24:32] = inp["conv_norm_g"][l].reshape(8, 128).T
        cvec[l, :, 32:40] = inp["conv_norm_b"][l].reshape(8, 128).T
    sh["cvec"] = cvec
    sh["dww"] = np.stack([np.ascontiguousarray(inp["conv_dw_w"][l].reshape(CK, 8, 128).transpose(2, 1, 0)) for l in range(2)]).astype(f32)
    sh["conv_out_b"] = np.ascontiguousarray(inp["conv_out_b"]).astype(f32)
    _ffn_moe(inp, 0, sh)
    sh["wkv"] = _fm(inp["w_kv"]).astype(f32)
    return sh


def _shared_B(inp):
    f32 = np.float32
    bf = ml_dtypes.bfloat16
    sh = _shared_common(inp)
    sh["wq"] = np.stack([_fm(inp["w_q"][j]) for j in range(2)]).astype(f32)
    sh["wo"] = np.stack([_fm(inp["w_o"][j]) for j in range(2)]).astype(f32)
    _ffn_moe(inp, 1, sh)
    kc = np.zeros((128, 2 * TOK), f32)
    blk = np.arange(2 * TOK) // BLK
    pos = np.arange(2 * TOK) % BLK
    for jb in range(16):
        kc[64 + jb, blk == jb] = 1.0
        kc[96 + jb, blk == jb] = 1.0
    kc[80:83, :] = pos[None, :]
    sh["kconst"] = kc.astype(bf)
    tri = np.where(np.arange(128)[:, None] > np.arange(128)[None, :], -BIG, 0.0).astype(f32)
    sh["tri"] = tri.astype(bf)
    return sh


def _core_B(half):
    f32 = np.float32
    p = np.arange(128)[:, None, None]
    tau = np.arange(NT)[None, :, None]
    jb = np.arange(16)[None, None, :]
    b = tau // 2
    own = (jb == 8 + b)
    past = (jb < 8 + b) & ((jb >= 8) | (half == 1))
    neg = np.where(own, 1e30, np.where(past, 0.0, -1e30)) + 0.0 * p
    el = np.where(own | past, 1.0, 0.0) + 0.0 * p
    tq = TOK + tau * 128 + p
    db = -(tq - BLK * jb) + 0.0
    return {"negelig": neg.reshape(128, 256).astype(f32), "elig01": el.reshape(128, 256).astype(f32),
            "dbase": db.reshape(128, 256).astype(f32)}


def _kv_full(kt_prev, kt_own, v_prev, v_own):
    bf = ml_dtypes.bfloat16
    ktf = np.zeros((H, DH, 2 * TOK), bf)
    vf = np.zeros((H, 128, 32, 65), bf)
    vf[:, :, :, 64] = 1.0
    if kt_prev is not None:
        ktf[:, :, :TOK] = kt_prev
        vf[:, :, :16, :64] = v_prev.reshape(16, 128, H, DH).transpose(2, 1, 0, 3)
    ktf[:, :, TOK:] = kt_own
    vf[:, :, 16:, :64] = v_own.reshape(16, 128, H, DH).transpose(2, 1, 0, 3)
    return ktf, vf


def _core_common(inp, core, xsrc):
    b, half = core // 2, core % 2
    s0 = half * TOK
    m = {}
    xin = np.zeros((NT + 1, 128, D), np.float32)
    xin[:NT] = xsrc[b, s0:s0 + TOK].reshape(NT, 128, D)
    if half == 1:
        xin[NT, :HALO] = xsrc[b, s0 - HALO:s0]
    m["xin"] = xin
    m["hmask"] = np.full((128, 1), float(half), np.float32)
    m["cfm"] = np.ascontiguousarray(inp["c"][b].reshape(8, 128).T).astype(np.float32)
    return m


_NC_CACHE = {}


def _get_nc(part):
    if part not in _NC_CACHE:
        _NC_CACHE[part] = Builder(part).build()
    return _NC_CACHE[part]


def run_part_A(inp, cores):
    sh = _shared_A(inp)
    maps = []
    for core in cores:
        m = dict(sh)
        m.update(_core_common(inp, core, inp["x"]))
        maps.append(m)
    nc = _get_nc("A")
    res = run_bass_kernel_spmd(nc, maps, core_ids=list(range(len(cores))))
    return res.results


def run_part_B(inp, cores, x1, kts, vs):
    sh = _shared_B(inp)
    maps = []
    for core in cores:
        half = core % 2
        m = dict(sh)
        m.update(_core_common(inp, core, x1))
        m.update(_core_B(half))
        if half == 1:
            ktf, vf = _kv_full(kts[core - 1], kts[core], vs[core - 1], vs[core])
        else:
            ktf, vf = _kv_full(None, kts[core], None, vs[core])
        m["kt_full"], m["v_full"] = ktf, vf
        maps.append(m)
    nc = _get_nc("B")
    res = run_bass_kernel_spmd(nc, maps, core_ids=list(range(len(cores))))
    return res.results


def run_fused(inp, cores):
    shA = _shared_A(inp)
    shB = _shared_B(inp)
    sh = dict(shA)
    for k, v in shB.items():
        if k in ("ffn13", "ffn2", "moe13", "moe2", "router_w", "router_b"):
            sh[k + "b"] = v
        else:
            sh[k] = v
    maps = []
    for core in cores:
        b, half = core // 2, core % 2
        m = dict(sh)
        m.update(_core_common(inp, core, inp["x"]))
        m.update(_core_B(half))
        xp = np.zeros((NT + 1, 128, D), np.float32)
        if half == 1:
            xp[:NT] = inp["x"][b, 0:TOK].reshape(NT, 128, D)
        m["xin_prev"] = xp
        maps.append(m)
    nc = _get_nc("F")
    res = run_bass_kernel_spmd(nc, maps, core_ids=list(range(len(cores))))
    return res.results


MODE = "F"


def kernel(**inputs):
    inp = {k: np.asarray(v) for k, v in inputs.items()}
    cores = list(range(NCORES))
    out = np.zeros((NB, SEQ, D), np.float32)
    if MODE == "F":
        res = run_fused(inp, cores)
        for c in cores:
            b, half = c // 2, c % 2
            out[b, half * TOK:(half + 1) * TOK] = np.asarray(res[c]["yout"]).reshape(TOK, D)
        return out
    resA = run_part_A(inp, cores)
    x1 = np.zeros((NB, SEQ, D), np.float32)
    kts, vs = {}, {}
    for c in cores:
        b, half = c // 2, c % 2
        x1[b, half * TOK:(half + 1) * TOK] = np.asarray(resA[c]["xout"]).reshape(TOK, D)
        kts[c] = np.asarray(resA[c]["kt_out"])
        vs[c] = np.asarray(resA[c]["v_out"]).reshape(TOK, D)
    resB = run_part_B(inp, cores, x1, kts, vs)
    for c in cores:
        b, half = c // 2, c % 2
        out[b, half * TOK:(half + 1) * TOK] = np.asarray(resB[c]["yout"]).reshape(TOK, D)
    return out
```

```python
import numpy as np
import ml_dtypes
import concourse.bass as bass
import concourse.mybir as mybir
from concourse.bass_utils import run_bass_kernel_spmd

F32 = mybir.dt.float32
BF16 = mybir.dt.bfloat16
ALU = mybir.AluOpType
AF = mybir.ActivationFunctionType
AX = mybir.AxisListType

D = 1024
SEQ = 4096
NB = 4
NCORES = 8
TOK = 2048
NT = 16
HALO = 64
H = 16
DH = 64
BLK = 256
DEPTH = 4
NA = 2
FD = 2816
FE = 3584
NE = 8
CK = 31
ALPHA = (2.0 * DEPTH) ** 0.25
EPS = 1e-5
BIG = 30000.0

ENGS = ["pe", "act", "dve", "pool", "sp"]


class Res:
    __slots__ = ("w", "r")

    def __init__(self):
        self.w = None
        self.r = {}


class Prog:
    def __init__(self):
        self.q = {e: [] for e in ENGS}
        self.cnt = {}
        self.seen = {e: {} for e in ENGS}
        self.names = []

    def _sem(self, name):
        if name not in self.cnt:
            self.cnt[name] = 0
            self.names.append(name)

    def _wait(self, eng, deps):
        for name, val in deps:
            if self.seen[eng].get(name, 0) < val:
                self.seen[eng][name] = val
                self.q[eng].append((0, name, val))

    @staticmethod
    def _collect(reads, writes):
        deps = []
        for r in reads:
            if r.w is not None:
                deps.append(r.w)
        for w in writes:
            if w.w is not None:
                deps.append(w.w)
            deps.extend(w.r.items())
        return deps

    @staticmethod
    def _mark(t, reads, writes):
        for r in reads:
            if r.r.get(t[0], 0) < t[1]:
                r.r[t[0]] = t[1]
        for w in writes:
            w.w = t
            w.r = {}

    def op(self, eng, fns, reads=(), writes=(), noself=False):
        if isinstance(fns, tuple):
            fns = [fns]
        deps = self._collect(reads, writes)
        if noself:
            deps = [d for d in deps if d[0] != "c_" + eng]
        self._wait(eng, deps)
        name = "c_" + eng
        self._sem(name)
        self.cnt[name] += 1
        t = (name, self.cnt[name])
        self.q[eng].append((1, fns, name, 1))
        self._mark(t, reads, writes)
        return t

    def dma(self, eng, fn, sem, reads=(), writes=()):
        self._wait(eng, self._collect(reads, writes))
        self._sem(sem)
        self.cnt[sem] += 16
        t = (sem, self.cnt[sem])
        self.q[eng].append((1, [fn], sem, 16))
        self._mark(t, reads, writes)
        return t

    def barrier(self):
        allt = [(n, self.cnt[n]) for n in self.names if self.cnt[n] > 0]
        for e in ENGS:
            self._wait(e, allt)

    def wait_on(self, eng, tickets):
        self._wait(eng, tickets)


def I(name, *a, **k):
    return (name, a, k)


def _replay(e, items, sems):
    for it in items:
        if it[0] == 0:
            e.wait_ge(sems[it[1]], it[2])
        else:
            fns = it[1]
            for f in fns[:-1]:
                getattr(e, f[0])(*f[1], **f[2])
            f = fns[-1]
            getattr(e, f[0])(*f[1], **f[2]).then_inc(sems[it[2]], it[3])


class Builder:
    def __init__(self, part):
        self.part = part
        self.nc = bass.Bass("TRN2", target_bir_lowering=False)
        self.P = Prog()
        self.dram = {}
        self.dmasem_i = 0

    def din(self, name, shape, dt=F32):
        t = self.nc.dram_tensor(name, list(shape), dt, kind="ExternalInput").ap()
        self.dram[name] = t
        return t

    def dout(self, name, shape, dt=F32):
        t = self.nc.dram_tensor(name, list(shape), dt, kind="ExternalOutput").ap()
        self.dram[name] = t
        return t

    def arena_reset(self):
        self.P.barrier()
        self.aoff = 0

    def alloc(self, shape, dt):
        n = int(np.prod(shape[1:]))
        nb = n * (4 if dt == F32 else 2)
        nb = (nb + 63) // 64 * 64
        off = self.aoff
        self.aoff += nb // 2
        assert self.aoff <= self.arena_elems, f"arena overflow {self.aoff*2} > {self.arena_elems*2}"
        self.last_off = off
        return self.view(off, shape, dt)

    def view(self, off, shape, dt):
        n = int(np.prod(shape[1:]))
        nb = n * (4 if dt == F32 else 2)
        nb = (nb + 63) // 64 * 64
        v = self.arena[:, off:off + nb // 2]
        if dt == F32:
            v = v.bitcast(F32)
        v = v[:, 0:n]
        if len(shape) == 3:
            v = v.rearrange("p (a b) -> p a b", b=shape[2])
        elif len(shape) == 4:
            v = v.rearrange("p (a b c) -> p a b c", b=shape[2], c=shape[3])
        return v

    def build(self):
        nc = self.nc
        from contextlib import ExitStack
        with ExitStack() as es:
            def sb(name, shape, dt):
                return es.enter_context(nc.sbuf_tensor(name, list(shape), dt))

            def ps(name, shape, dt):
                return es.enter_context(nc.psum_tensor(name, list(shape), dt))

            self.x_tm = sb("x_tm", [128, NT + 1, D], F32)
            self.h_fm = sb("h_fm", [128, 8, TOK + HALO], BF16)
            self.bc = sb("bc", [128, 6, D], F32)
            self.ident = sb("ident", [128, 128], BF16)
            self.ones = sb("ones", [128, 128], BF16)
            self.cbc = sb("cbc", [128, 8, 128], BF16)
            self.cfm = sb("cfm_sb", [128, 16], F32)
            self.small = sb("small", [128, 512], F32)
            self.gates = sb("gates", [128, NT, NE], F32)
            self.hmask = sb("hmask_sb", [128, 2], F32)
            self.arena_elems = 38 * 1024
            self.arena = sb("arena", [128, self.arena_elems], BF16)
            self.aoff = 0
            self.psA = [ps(f"psA{i}", [128, 512], F32) for i in range(4)]
            self.psO = [ps(f"psO{i}", [128, 512], F32) for i in range(2)]
            self.psT = [ps(f"psT{i}", [128, 8, 128], BF16) for i in range(2)]
            self.rA = [Res() for _ in range(4)]
            self.rO = [Res() for _ in range(2)]
            self.rT = [Res() for _ in range(2)]
            self.rX = [Res() for _ in range(NT + 1)]
            self.rH = [Res() for _ in range(NT + 1)]
            self.rBC = [Res() for _ in range(6)]
            self.rConst = Res()
            self.rCbc = Res()
            self.rGates = Res()
            self.rSmall = Res()
            self.iA = 0
            self.iO = 0
            self.iT = 0
            self.out_tickets = []

            if self.part == "A":
                self.emit_A()
            elif self.part == "B":
                self.emit_B()
            else:
                self.emit_F()

            P = self.P
            P.wait_on("sp", self.out_tickets)
            sems = {n: es.enter_context(nc.semaphore(n)) for n in P.names}
            with nc.Block() as block:
                @block.tensor
                def _(e):
                    _replay(e, P.q["pe"], sems)

                @block.scalar
                def _(e):
                    _replay(e, P.q["act"], sems)

                @block.vector
                def _(e):
                    _replay(e, P.q["dve"], sems)

                @block.gpsimd
                def _(e):
                    _replay(e, P.q["pool"], sems)

                @block.sync
                def _(e):
                    _replay(e, P.q["sp"], sems)
        return nc

    def tile_np(self, t):
        return HALO if t == NT else 128

    def tok0(self, t):
        return TOK if t == NT else t * 128

    def nextA(self):
        i = self.iA
        self.iA = (i + 1) % 4
        return self.psA[i], self.rA[i]

    def nextO(self):
        i = self.iO
        self.iO = (i + 1) % 2
        return self.psO[i], self.rO[i]

    def nextT(self):
        i = self.iT
        self.iT = (i + 1) % 2
        return self.psT[i], self.rT[i]

    def dsem(self, base):
        return "d_" + base

    def load_x(self, x_in):
        for t in range(NT + 1):
            self.P.dma("sp", I("dma_start", out=self.x_tm[:, t, :], in_=x_in[t]), "d_x", writes=[self.rX[t]])

    def setup_common(self, load_x=True):
        P = self.P
        x_in = self.din("xin", [NT + 1, 128, D])
        ident_in = self.din("ident_in", [128, 128], BF16)
        cfm_in = self.din("cfm", [128, 8])
        hm_in = self.din("hmask", [128, 1])
        self.adaw = self.din("adaw", [26, 128, 8, D])
        self.adab = self.din("adab", [26, D])
        self.lng = self.din("lng", [8, D])
        self.lnb = self.din("lnb", [8, D])
        if load_x:
            self.load_x(x_in)
        P.dma("sp", I("dma_start", out=self.ident[:], in_=ident_in), "d_c", writes=[self.rConst])
        P.dma("sp", I("dma_start", out=self.cfm[:, 0:8], in_=cfm_in), "d_c", writes=[self.rSmall])
        P.dma("sp", I("dma_start", out=self.hmask[:, 0:1], in_=hm_in), "d_c", writes=[self.rConst])
        P.op("pool", I("memset", self.hmask[:, 1:2], 0.0), writes=[self.rConst])
        self.hm_ap = self.hmask[:, 0:1]
        P.op("pool", I("memset", self.ones[:], 1.0), writes=[self.rConst])
        P.op("act", I("activation", out=self.cfm[:, 8:16], in_=self.cfm[:, 0:8], func=AF.Silu),
             reads=[self.rSmall], writes=[self.rSmall])
        P.op("dve", I("tensor_copy", out=self.cbc[:], in_=self.cfm[:, 8:16].unsqueeze(2).to_broadcast([128, 8, 128])),
             reads=[self.rSmall], writes=[self.rCbc])

    def ada_vec(self, idx, slot, plus_one, wbuf, rW):
        P = self.P
        wide = wbuf.shape[2] >= 1024
        P.dma("sp", I("dma_start", out=self.bc[:, slot, :], in_=self.adab[idx:idx + 1, :].partition_broadcast(128)),
              "d_adab", writes=[self.rBC[slot]])
        for n in range(2):
            wb = wbuf[:, :, n * 512:(n + 1) * 512] if wide else wbuf[:, :, 0:512]
            P.dma("pool", I("dma_start", out=wb, in_=self.adaw[idx][:, :, n * 512:(n + 1) * 512]), "d_adaw", writes=[rW])
            pt, rp = self.nextA()
            fns = [I("matmul", pt[:], self.cbc[:, k, :], wb[:, k, :], start=(k == 0), stop=(k == 7)) for k in range(8)]
            P.op("pe", fns, reads=[rW, self.rCbc], writes=[rp])
            P.op("dve", I("scalar_tensor_tensor",
                out=self.bc[:, slot, n * 512:(n + 1) * 512], in0=pt[:], scalar=(1.0 if plus_one else 0.0),
                in1=self.bc[:, slot, n * 512:(n + 1) * 512], op0=ALU.add, op1=ALU.add),
                reads=[rp], writes=[self.rBC[slot]])

    def load_row_bc(self, src_row_ap, slot):
        self.P.dma("sp", I("dma_start", out=self.bc[:, slot, :], in_=src_row_ap.partition_broadcast(128)),
                   "d_row", writes=[self.rBC[slot]])

    def h_tile(self, t, Gs, Bs, tmpf, rtmpf, hb, rhb):
        P = self.P
        npart = self.tile_np(t)
        c0 = self.tok0(t)
        P.op("dve", I("tensor_tensor", out=tmpf[0:npart, :], in0=self.x_tm[0:npart, t, :], in1=self.bc[0:npart, Gs, :], op=ALU.mult),
             reads=[self.rX[t], self.rBC[Gs]], writes=[rtmpf])
        P.op("dve", I("tensor_tensor", out=hb[0:npart, :], in0=tmpf[0:npart, :], in1=self.bc[0:npart, Bs, :], op=ALU.add),
             reads=[rtmpf, self.rBC[Bs]], writes=[rhb])
        pt, rp = self.nextT()
        fns = [(I("transpose", pt[:, k, 0:npart], hb[0:npart, k * 128:(k + 1) * 128], self.ident[0:npart, 0:npart]))
               for k in range(8)]
        P.op("pe", fns, reads=[rhb, self.rConst], writes=[rp])
        P.op("act", I("activation", out=self.h_fm[:, :, c0:c0 + npart], in_=pt[:, :, 0:npart], func=AF.Copy),
             reads=[rp], writes=[self.rH[t]])

    def ln_tiles(self, tiles, st, rst, tmps):
        P = self.P
        for i, t in enumerate(tiles):
            npart = self.tile_np(t)
            s = st[:, t, :]
            P.op("dve", I("bn_stats", out=s[0:npart, 0:6], in_=self.x_tm[0:npart, t, 0:512]),
                 reads=[self.rX[t]], writes=[rst[t]])
            P.op("dve", I("bn_stats", out=s[0:npart, 6:12], in_=self.x_tm[0:npart, t, 512:1024]),
                 reads=[self.rX[t]], writes=[rst[t]])
            P.op("dve", I("bn_aggr", out=s[0:npart, 12:14], in_=s[0:npart, 0:12]),
                 reads=[rst[t]], writes=[rst[t]])
            P.op("dve", I("tensor_scalar", out=s[0:npart, 14:15], in0=s[0:npart, 13:14], scalar1=EPS, scalar2=None, op0=ALU.add),
                 reads=[rst[t]], writes=[rst[t]])
            P.op("act", I("activation", out=s[0:npart, 15:16], in_=s[0:npart, 14:15], func=AF.Sqrt),
                 reads=[rst[t]], writes=[rst[t]])
            P.op("dve", I("reciprocal", out=s[0:npart, 16:17], in_=s[0:npart, 15:16]),
                 reads=[rst[t]], writes=[rst[t]])
            P.op("dve", I("scalar_tensor_tensor", out=s[0:npart, 17:18], in0=s[0:npart, 12:13], scalar=-1.0, in1=s[0:npart, 16:17],
                          op0=ALU.mult, op1=ALU.mult), reads=[rst[t]], writes=[rst[t]])
            P.op("act", I("activation", out=self.x_tm[0:npart, t, :], in_=self.x_tm[0:npart, t, :], func=AF.Identity,
                          scale=s[0:npart, 16:17], bias=s[0:npart, 17:18]), reads=[rst[t], self.rX[t]], writes=[self.rX[t]])
            tmpf, rtmpf, hb, rhb = tmps[i % 2]
            self.h_tile(t, 2, 3, tmpf, rtmpf, hb, rhb)
            P.op("dve", I("tensor_tensor", out=self.x_tm[0:npart, t, :], in0=self.x_tm[0:npart, t, :], in1=self.bc[0:npart, 0, :], op=ALU.mult),
                 reads=[self.rX[t], self.rBC[0]], writes=[self.rX[t]])
            P.op("dve", I("tensor_tensor", out=self.x_tm[0:npart, t, :], in0=self.x_tm[0:npart, t, :], in1=self.bc[0:npart, 1, :], op=ALU.add),
                 reads=[self.rX[t], self.rBC[1]], writes=[self.rX[t]])

    def ln_prep(self, ln_idx, sc_idx, sh_idx, wbuf, rW):
        P = self.P
        self.load_row_bc(self.lng[ln_idx:ln_idx + 1, :], 0)
        self.load_row_bc(self.lnb[ln_idx:ln_idx + 1, :], 1)
        if sc_idx is None:
            P.op("pool", I("tensor_copy", out=self.bc[:, 2, :], in_=self.bc[:, 0, :]), reads=[self.rBC[0]], writes=[self.rBC[2]])
            P.op("pool", I("tensor_copy", out=self.bc[:, 3, :], in_=self.bc[:, 1, :]), reads=[self.rBC[1]], writes=[self.rBC[3]])
            return
        self.ada_vec(sc_idx, 2, True, wbuf, rW)
        self.ada_vec(sh_idx, 3, False, wbuf, rW)
        P.op("pool", I("tensor_tensor", out=self.bc[:, 5, :], in0=self.bc[:, 1, :], in1=self.bc[:, 2, :], op=ALU.mult),
             reads=[self.rBC[1], self.rBC[2]], writes=[self.rBC[5]])
        P.op("pool", I("tensor_tensor", out=self.bc[:, 3, :], in0=self.bc[:, 3, :], in1=self.bc[:, 5, :], op=ALU.add),
             reads=[self.rBC[3], self.rBC[5]], writes=[self.rBC[3]])
        P.op("pool", I("tensor_tensor", out=self.bc[:, 2, :], in0=self.bc[:, 0, :], in1=self.bc[:, 2, :], op=ALU.mult),
             reads=[self.rBC[0], self.rBC[2]], writes=[self.rBC[2]])

    def mk_tmps(self):
        tf, rtf = self.alloc([128, D], F32), Res()
        return [(tf, rtf, self.alloc([128, D], BF16), Res()) for _ in range(2)]

    def scale_x(self, tiles):
        for t in tiles:
            npart = self.tile_np(t)
            self.P.op("act", I("activation", out=self.x_tm[0:npart, t, :], in_=self.x_tm[0:npart, t, :], func=AF.Copy, scale=ALPHA),
                      reads=[self.rX[t]], writes=[self.rX[t]])

    def ffn_phase(self, experts, FT, n_ft, groups, g_idx, ln_idx, next_sc, next_sh, router=None):
        P = self.P
        self.arena_reset()
        fs = FT * 128
        W13 = [self.alloc([128, 8, 1024], BF16) for _ in range(2)]
        W2 = [self.alloc([128, 4, D], BF16) for _ in range(2)]
        rW13 = [Res() for _ in range(2)]
        rW2 = [Res() for _ in range(2)]
        hid = [self.alloc([128, 4, 512], BF16) for _ in range(2)]
        rhid = [Res() for _ in range(2)]
        sil = [self.alloc([128, 512], BF16) for _ in range(2)]
        rsil = [Res() for _ in range(2)]
        st = self.alloc([128, NT + 1, 32], F32)
        rst = [Res() for _ in range(NT + 1)]
        tmps = self.mk_tmps()
        all_tiles = [t for g in groups for t in g[2]]
        self.ln_prep(ln_idx, next_sc, next_sh, W13[1], rW13[1])
        self.ada_vec(g_idx, 4, True, W13[0], rW13[0])
        if router is not None:
            self.moe_router(router, W13[1], rW13[1], groups)
        self.scale_x(all_tiles)
        items = [(ei, ft, g) for ei in range(len(experts)) for ft in range(n_ft) for g in groups]
        slot_of = {}
        it_w = 0

        def load_w(ei, ft):
            nonlocal it_w
            s = it_w % 2
            it_w += 1
            w13d, w2d, _ = experts[ei]
            P.dma("pool", I("dma_start", out=W13[s][:, :, 0:2 * fs], in_=w13d[ft]), f"d_w13_{s}", writes=[rW13[s]])
            P.dma("pool", I("dma_start", out=W2[s][:, 0:FT, :], in_=w2d[ft]), f"d_w2_{s}", writes=[rW2[s]])
            P.op("pool", I("tensor_tensor", out=W2[s][:, 0:FT, :], in0=W2[s][:, 0:FT, :],
                                                   in1=self.bc[:, 4, :].unsqueeze(1).to_broadcast([128, FT, D]), op=ALU.mult),
                 reads=[self.rBC[4]], writes=[rW2[s]])
            slot_of[(ei, ft)] = s

        def phaseA(i):
            ei, ft, (c0, n, tiles) = items[i]
            s = slot_of[(ei, ft)]
            hs = i % 2
            for fc in range(FT):
                pa, ra = self.nextA()
                pb, rb = self.nextA()
                rt = [self.rH[t] for t in tiles]
                P.op("pe", [(I("matmul", pa[:, 0:n], W13[s][:, k, fc * 128:(fc + 1) * 128], self.h_fm[:, k, c0:c0 + n],
                                                    start=(k == 0), stop=(k == 7))) for k in range(8)],
                     reads=[rW13[s]] + rt, writes=[ra])
                P.op("pe", [(I("matmul", pb[:, 0:n], W13[s][:, k, fs + fc * 128:fs + (fc + 1) * 128], self.h_fm[:, k, c0:c0 + n],
                                                    start=(k == 0), stop=(k == 7))) for k in range(8)],
                     reads=[rW13[s]] + rt, writes=[rb])
                ss = fc % 2
                P.op("act", I("activation", out=sil[ss][:, 0:n], in_=pa[:, 0:n], func=AF.Silu),
                     reads=[ra], writes=[rsil[ss]])
                P.op("dve", I("tensor_tensor", out=hid[hs][:, fc, 0:n], in0=sil[ss][:, 0:n], in1=pb[:, 0:n], op=ALU.mult),
                     reads=[rsil[ss], rb], writes=[rhid[hs]])

        def phaseB(i):
            ei, ft, (c0, n, tiles) = items[i]
            s = slot_of[(ei, ft)]
            hs = i % 2
            gate = experts[ei][2]
            for j, t in enumerate(tiles):
                npart = self.tile_np(t)
                for half in range(2):
                    po, ro = self.nextO()
                    P.op("pe", [(I("matmul", po[0:npart, :], hid[hs][:, fc, j * 128:j * 128 + npart],
                                                          W2[s][:, fc, half * 512:(half + 1) * 512],
                                                          start=(fc == 0), stop=(fc == FT - 1))) for fc in range(FT)],
                         reads=[rhid[hs], rW2[s]], writes=[ro])
                    xs = self.x_tm[0:npart, t, half * 512:(half + 1) * 512]
                    if gate is None:
                        P.op("dve", I("tensor_tensor", out=xs, in0=po[0:npart, :], in1=xs, op=ALU.add),
                             reads=[ro, self.rX[t]], writes=[self.rX[t]])
                    else:
                        P.op("dve", I("scalar_tensor_tensor",
                            out=xs, in0=po[0:npart, :], scalar=self.gates[0:npart, t, gate:gate + 1], in1=xs, op0=ALU.mult, op1=ALU.add),
                            reads=[ro, self.rX[t], self.rGates], writes=[self.rX[t]])

        wl = [(ei, ft) for ei in range(len(experts)) for ft in range(n_ft)]
        ng = len(groups)
        load_w(*wl[0])
        if len(wl) > 1:
            load_w(*wl[1])
        ln_started = False
        for i in range(len(items)):
            if i == 0:
                phaseA(0)
            if i + 1 < len(items):
                phaseA(i + 1)
            phaseB(i)
            if (i + 1) % ng == 0:
                w = (i + 1) // ng - 1
                if w + 2 < len(wl):
                    load_w(*wl[w + 2])
        for (c0, n, tiles) in groups:
            self.ln_tiles(tiles, st, rst, tmps)

    def moe_router(self, router, wbuf, rW, groups):
        P = self.P
        rw_d, rb_d = router
        rwb = wbuf[:, :, 0:8]
        P.dma("pool", I("dma_start", out=rwb, in_=rw_d), "d_rw", writes=[rW])
        lg = self.small[:, 0:128].rearrange("p (t e) -> p t e", e=NE)
        wk = self.small[:, 128:256].rearrange("p (t e) -> p t e", e=NE)
        ex = self.small[:, 256:384].rearrange("p (t e) -> p t e", e=NE)
        m1 = self.small[:, 384:400]
        m2 = self.small[:, 400:416]
        ssum = self.small[:, 416:432]
        rb = self.small[:, 440:448]
        P.dma("sp", I("dma_start", out=rb, in_=rb_d.partition_broadcast(128)), "d_rb", writes=[self.rSmall])
        pt, rp = self.nextA()
        for t in range(NT):
            P.op("pe", [(I("matmul", pt[:, t * 8:(t + 1) * 8], self.h_fm[:, k, t * 128:(t + 1) * 128], rwb[:, k, :],
                                                     start=(k == 0), stop=(k == 7))) for k in range(8)],
                 reads=[rW, self.rH[t]], writes=[rp])
        rS = self.rSmall
        P.op("dve", I("tensor_tensor", out=lg, in0=pt[:, 0:128].rearrange("p (t e) -> p t e", e=NE),
                                              in1=rb.unsqueeze(1).to_broadcast([128, NT, NE]), op=ALU.add), reads=[rp, rS], writes=[rS])
        P.op("dve", I("tensor_reduce", out=m1, in_=lg, axis=AX.X, op=ALU.max), reads=[rS], writes=[rS])
        P.op("dve", I("tensor_tensor", out=wk, in0=lg, in1=m1.unsqueeze(2).to_broadcast([128, NT, NE]), op=ALU.is_ge), reads=[rS], writes=[rS])
        P.op("dve", I("scalar_tensor_tensor", out=wk, in0=wk, scalar=-1e30, in1=lg, op0=ALU.mult, op1=ALU.add), reads=[rS], writes=[rS])
        P.op("dve", I("tensor_reduce", out=m2, in_=wk, axis=AX.X, op=ALU.max), reads=[rS], writes=[rS])
        P.op("dve", I("tensor_tensor", out=wk, in0=lg, in1=m2.unsqueeze(2).to_broadcast([128, NT, NE]), op=ALU.is_ge), reads=[rS], writes=[rS])
        P.op("dve", I("tensor_tensor", out=ex, in0=lg, in1=m1.unsqueeze(2).to_broadcast([128, NT, NE]), op=ALU.subtract), reads=[rS], writes=[rS])
        P.op("act", I("activation", out=ex, in_=ex, func=AF.Exp), reads=[rS], writes=[rS])
        P.op("dve", I("tensor_tensor", out=ex, in0=ex, in1=wk, op=ALU.mult), reads=[rS], writes=[rS])
        P.op("dve", I("tensor_reduce", out=ssum, in_=ex, axis=AX.X, op=ALU.add), reads=[rS], writes=[rS])
        P.op("dve", I("reciprocal", out=ssum, in_=ssum), reads=[rS], writes=[rS])
        P.op("dve", I("tensor_tensor", out=self.gates[:], in0=ex, in1=ssum.unsqueeze(2).to_broadcast([128, NT, NE]), op=ALU.mult),
             reads=[rS], writes=[self.rGates])

    def conv_phase(self, l, g1_idx, ln_idx, next_sc, next_sh):
        P = self.P
        self.arena_reset()
        ciw_d = self.dram["conv_in_w"]
        cow_d = self.dram["conv_out_w"]
        cvec_d = self.dram["cvec"]
        cob_d = self.dram["conv_out_b"]
        Wci = self.alloc([128, 8, 2048], BF16)
        Wco = self.alloc([128, 8, D], BF16)
        rWci, rWco = Res(), Res()
        U = [self.alloc([128, 30 + 128], F32) for _ in range(8)]
        V = [self.alloc([128, 128], F32) for _ in range(8)]
        rU = [Res() for _ in range(8)]
        rV = [Res() for _ in range(8)]
        vb = [self.alloc([128, 128], BF16) for _ in range(2)]
        vq = [self.alloc([128, 128], BF16) for _ in range(2)]
        rvb = [Res() for _ in range(2)]
        rvq = [Res() for _ in range(2)]
        sig = [self.alloc([128, 128], F32) for _ in range(2)]
        rsig = [Res() for _ in range(2)]
        t1 = [self.alloc([128, 128], F32) for _ in range(2)]
        rt1 = [Res() for _ in range(2)]
        pk = [self.alloc([128, 128], BF16) for _ in range(4)]
        rpk = [Res() for _ in range(4)]
        self.pk_i = 0
        stt_ = self.alloc([128, 4, 128], F32)
        rstt = Res()
        cv = self.alloc([128, 72], F32)
        dww = self.alloc([128, 8, CK], F32)
        rcv = Res()
        st = self.alloc([128, NT + 1, 32], F32)
        rst = [Res() for _ in range(NT + 1)]
        tmps = self.mk_tmps()

        self.ln_prep(ln_idx, next_sc, next_sh, Wci[:, :, 0:D], rWci)
        self.ada_vec(g1_idx, 4, True, Wco, rWco)
        P.dma("pool", I("dma_start", out=Wci, in_=ciw_d[l]), "d_wci", writes=[rWci])
        P.dma("pool", I("dma_start", out=Wco, in_=cow_d[l]), "d_wco", writes=[rWco])
        P.op("pool", I("tensor_tensor", out=Wco, in0=Wco, in1=self.bc[:, 4, :].unsqueeze(1).to_broadcast([128, 8, D]), op=ALU.mult),
             reads=[self.rBC[4]], writes=[rWco])
        P.dma("sp", I("dma_start", out=cv[:, 0:40], in_=cvec_d[l]), "d_cv", writes=[rcv])
        P.dma("sp", I("dma_start", out=dww, in_=self.dram["dww"][l]), "d_cv2", writes=[rcv])
        self.load_row_bc(cob_d[l:l + 1, :], 5)
        P.op("pool", I("tensor_tensor", out=self.bc[:, 5, :], in0=self.bc[:, 5, :], in1=self.bc[:, 4, :], op=ALU.mult),
             reads=[self.rBC[4], self.rBC[5]], writes=[self.rBC[5]])
        for c in range(8):
            P.op("pool", I("memset", U[c][:, 0:30], 0.0), writes=[rU[c]])
        for t in range(NT + 1):
            npart = self.tile_np(t)
            P.op("dve", I("scalar_tensor_tensor",
                out=self.x_tm[0:npart, t, :], in0=self.x_tm[0:npart, t, :], scalar=ALPHA, in1=self.bc[0:npart, 5, :],
                op0=ALU.mult, op1=ALU.add), reads=[self.rX[t], self.rBC[5]], writes=[self.rX[t]])

        order = [NT] + list(range(NT))
        for gi, t in enumerate(order):
            n = self.tile_np(t)
            c0 = self.tok0(t)
            rht = self.rH[t]
            for c in range(8):
                pa, ra = self.nextA()
                pg, rg = self.nextA()
                P.op("pe", [(I("matmul", pa[:, 0:n], Wci[:, k, c * 128:(c + 1) * 128], self.h_fm[:, k, c0:c0 + n],
                                                    start=(k == 0), stop=(k == 7))) for k in range(8)], reads=[rWci, rht], writes=[ra])
                P.op("pe", [(I("matmul", pg[:, 0:n], Wci[:, k, D + c * 128:D + (c + 1) * 128], self.h_fm[:, k, c0:c0 + n],
                                                    start=(k == 0), stop=(k == 7))) for k in range(8)], reads=[rWci, rht], writes=[rg])
                ss = c % 2
                P.op("act", I("activation", out=sig[ss][:, 0:n], in_=pg[:, 0:n], func=AF.Sigmoid, bias=cv[:, 8 + c:9 + c]),
                     reads=[rg, rcv], writes=[rsig[ss]])
                P.op("dve", I("scalar_tensor_tensor", out=U[c][:, 30:30 + n], in0=pa[:, 0:n], scalar=cv[:, c:c + 1],
                                                                        in1=sig[ss][:, 0:n], op0=ALU.add, op1=ALU.mult),
                     reads=[ra, rsig[ss], rcv], writes=[rU[c]])
                if t == NT:
                    P.op("dve", I("tensor_scalar", out=U[c][:, 30:30 + n], in0=U[c][:, 30:30 + n], scalar1=self.hm_ap,
                                                              scalar2=None, op0=ALU.mult), reads=[rU[c], self.rConst], writes=[rU[c]])
            for k in range(CK):
                for c in (0, 1, 2, 3):
                    if k == 0:
                        P.op("dve", I("tensor_scalar", out=V[c][:, 0:n], in0=U[c][:, 0:n], scalar1=dww[:, c, 0:1],
                                      scalar2=cv[:, 16 + c:17 + c], op0=ALU.mult, op1=ALU.add),
                             reads=[rU[c], rcv], writes=[rV[c]])
                    else:
                        P.op("dve", I("scalar_tensor_tensor", out=V[c][:, 0:n], in0=U[c][:, k:k + n], scalar=dww[:, c, k:k + 1],
                                      in1=V[c][:, 0:n], op0=ALU.mult, op1=ALU.add),
                             reads=[rU[c], rcv, rV[c]], writes=[rV[c]])
            for c in (4, 5, 6, 7):
                pf, rpf = self.nextA()
                for k in range(CK):
                    r = self.pk_i % 4
                    self.pk_i += 1
                    P.op("act", I("activation", out=pk[r][:, 0:n], in_=U[c][:, k:k + n], func=AF.Identity, scale=dww[:, c, k:k + 1]),
                         reads=[rU[c], rcv], writes=[rpk[r]])
                    P.op("pe", I("matmul", pf[:, 0:n], self.ident[:], pk[r][:, 0:n], start=(k == 0), stop=(k == CK - 1)),
                         reads=[rpk[r], self.rConst], writes=[rpf], noself=(k > 0))
                P.op("act", I("activation", out=V[c][:, 0:n], in_=pf[:, 0:n], func=AF.Identity, bias=cv[:, 16 + c:17 + c]),
                     reads=[rpf, rcv], writes=[rV[c]])
            for c in range(8):
                P.op("act", I("activation", out=U[c][:, 0:30], in_=U[c][:, n:n + 30], func=AF.Copy), reads=[rU[c]], writes=[rU[c]])
            pS1, rS1 = self.nextA()
            pS2, rS2 = self.nextA()
            for c in range(8):
                ss = c % 2
                P.op("act", I("activation", out=vb[ss][:, 0:n], in_=V[c][:, 0:n], func=AF.Copy), reads=[rV[c]], writes=[rvb[ss]])
                P.op("act", I("activation", out=vq[ss][:, 0:n], in_=V[c][:, 0:n], func=AF.Square), reads=[rV[c]], writes=[rvq[ss]])
                P.op("pe", I("matmul", pS1[:, 0:n], self.ones[:], vb[ss][:, 0:n], start=(c == 0), stop=(c == 7)),
                     reads=[rvb[ss], self.rConst], writes=[rS1])
                P.op("pe", I("matmul", pS2[:, 0:n], self.ones[:], vq[ss][:, 0:n], start=(c == 0), stop=(c == 7)),
                     reads=[rvq[ss], self.rConst], writes=[rS2])
            mean, msq, sq, rstd = stt_[:, 0, 0:n], stt_[:, 1, 0:n], stt_[:, 2, 0:n], stt_[:, 3, 0:n]
            P.op("dve", I("tensor_scalar", out=mean, in0=pS1[:, 0:n], scalar1=1.0 / D, scalar2=None, op0=ALU.mult), reads=[rS1], writes=[rstt])
            P.op("dve", I("tensor_tensor", out=msq, in0=mean, in1=mean, op=ALU.mult), reads=[rstt], writes=[rstt])
            P.op("dve", I("scalar_tensor_tensor", out=msq, in0=pS2[:, 0:n], scalar=1.0 / D, in1=msq, op0=ALU.mult, op1=ALU.subtract),
                 reads=[rS2, rstt], writes=[rstt])
            P.op("dve", I("tensor_scalar", out=msq, in0=msq, scalar1=0.0, scalar2=EPS, op0=ALU.max, op1=ALU.add), reads=[rstt], writes=[rstt])
            P.op("act", I("activation", out=sq, in_=msq, func=AF.Sqrt), reads=[rstt], writes=[rstt])
            P.op("dve", I("reciprocal", out=rstd, in_=sq), reads=[rstt], writes=[rstt])
            for c in range(8):
                ss = c % 2
                P.op("dve", I("tensor_tensor", out=t1[ss][:, 0:n], in0=V[c][:, 0:n], in1=mean, op=ALU.subtract),
                     reads=[rV[c], rstt], writes=[rt1[ss]])
                P.op("pool", I("tensor_tensor", out=t1[ss][:, 0:n], in0=t1[ss][:, 0:n], in1=rstd, op=ALU.mult),
                     reads=[rt1[ss], rstt], writes=[rt1[ss]])
                P.op("act", I("activation", out=self.h_fm[:, c, c0:c0 + n], in_=t1[ss][:, 0:n], func=AF.Silu,
                                                              scale=cv[:, 24 + c:25 + c], bias=cv[:, 32 + c:33 + c]),
                     reads=[rt1[ss], rcv], writes=[rht])
            for half in range(2):
                po, ro = self.nextO()
                fns = [(I("matmul", po[0:n, :], self.h_fm[:, c, c0:c0 + n], Wco[:, c, half * 512:(half + 1) * 512],
                                               start=(c == 0), stop=(c == 7))) for c in range(8)]
                P.op("pe", fns, reads=[rht, rWco], writes=[ro])
                xs = self.x_tm[0:n, t, half * 512:(half + 1) * 512]
                P.op("dve", I("tensor_tensor", out=xs, in0=po[0:n, :], in1=xs, op=ALU.add),
                     reads=[ro, self.rX[t]], writes=[self.rX[t]])
            self.ln_tiles([t], st, rst, tmps)

    def decl_A(self):
        self.din("conv_in_w", [2, 128, 8, 2048])
        self.din("conv_out_w", [2, 128, 8, D])
        self.din("cvec", [2, 128, 40])
        self.din("dww", [2, 128, 8, CK])
        self.din("conv_out_b", [2, D])
        self.din("ffn13", [11, 128, 8, 512])
        self.din("ffn2", [11, 128, 2, D])
        self.din("moe13", [NE * 7, 128, 8, 1024])
        self.din("moe2", [NE * 7, 128, 4, D])
        self.din("router_w", [128, 8, NE])
        self.din("router_b", [1, NE])
        self.din("wkv", [128, 8, 2048])

    def body_A(self, kt_out, v_out, kv_res=None):
        dr = self.dram
        ffn13, ffn2, moe13, moe2 = dr["ffn13"], dr["ffn2"], dr["moe13"], dr["moe2"]
        main_groups = [(g * 512, 512, [4 * g + j for j in range(4)]) for g in range(4)]
        halo_group = (TOK, HALO, [NT])
        self.arena_reset()
        tmps = self.mk_tmps()
        abuf, rab = self.alloc([128, 8, D], BF16), Res()
        self.ada_vec(0 * 6 + 1, 2, True, abuf, rab)
        self.ada_vec(0 * 6 + 0, 3, False, abuf, rab)
        for i, t in enumerate([NT] + list(range(NT))):
            self.h_tile(t, 2, 3, *tmps[i % 2])
        for l in range(2):
            b = l * 6
            self.conv_phase(l, b + 2, l * 2 + 0, b + 4, b + 3)
            if l == 0:
                experts = [(ffn13, ffn2, None)]
                self.ffn_phase(experts, 2, 11, [halo_group] + main_groups, b + 5, l * 2 + 1, 6 + 1, 6 + 0)
            else:
                experts = [(moe13[e * 7:(e + 1) * 7], moe2[e * 7:(e + 1) * 7], e) for e in range(NE)]
                self.ffn_phase(experts, 4, 7, main_groups, b + 5, l * 2 + 1, 25, 24, router=(dr["router_w"], dr["router_b"]))
        self.kv_phase(dr["wkv"], kt_out, v_out, kv_res)

    def emit_A(self):
        P = self.P
        self.setup_common()
        self.decl_A()
        xout = self.dout("xout", [NT, 128, D])
        kt_out = self.dout("kt_out", [H, DH, TOK], BF16)
        v_out = self.dout("v_out", [NT, 128, D], BF16)
        self.body_A(kt_out, v_out)
        for t in range(NT):
            self.out_tickets.append(P.dma("sp", I("dma_start", out=xout[t], in_=self.x_tm[:, t, :]), "d_out", reads=[self.rX[t]]))

    def emit_F(self):
        P = self.P
        nc = self.nc
        self.setup_common(load_x=False)
        self.decl_A()
        self.decl_B("b")
        xprev = self.din("xin_prev", [NT + 1, 128, D])
        yout = self.dout("yout", [NT, 128, D])
        kt_p = nc.dram_tensor("kt_prev_scr", [H, DH, TOK], BF16).ap()
        v_p = nc.dram_tensor("v_prev_scr", [NT, 128, D], BF16).ap()
        kt_o = nc.dram_tensor("kt_own_scr", [H, DH, TOK], BF16).ap()
        v_o = nc.dram_tensor("v_own_scr", [NT, 128, D], BF16).ap()
        res_p, res_o = [], []
        self.load_x(xprev)
        self.hm_ap = self.hmask[:, 1:2]
        self.body_A(kt_p, v_p, res_p)
        self.load_x(self.dram["xin"])
        self.hm_ap = self.hmask[:, 0:1]
        self.body_A(kt_o, v_o, res_o)

        def kv_load(h, s, Kb, Vb, rK, rV):
            P.dma("sp", I("dma_start", out=Kb[s][0:DH, 0:TOK], in_=kt_p[h]), f"d_k{s}", reads=res_p, writes=[rK[s]])
            P.dma("sp", I("dma_start", out=Kb[s][0:DH, TOK:2 * TOK], in_=kt_o[h]), f"d_k{s}", reads=res_o, writes=[rK[s]])
            vp = v_p.rearrange("t p (h d) -> h p t d", d=DH)
            vo = v_o.rearrange("t p (h d) -> h p t d", d=DH)
            P.dma("sp", I("dma_start", out=Vb[s][:, 0:16, 0:DH], in_=vp[h]), f"d_v{s}", reads=res_p, writes=[rV[s]])
            P.dma("sp", I("dma_start", out=Vb[s][:, 16:32, 0:DH], in_=vo[h]), f"d_v{s}", reads=res_o, writes=[rV[s]])

        self.body_B(yout, kv_load, "b", ones_col=True)

    def kv_phase(self, wkv_d, kt_out, v_out, kv_res=None):
        P = self.P
        self.arena_reset()
        Wkv = self.alloc([128, 8, 2048], BF16)
        rW = Res()
        kts = [self.alloc([128, 512], BF16) for _ in range(2)]
        rk = [Res() for _ in range(2)]
        vts = [self.alloc([128, D], BF16) for _ in range(2)]
        rv = [Res() for _ in range(2)]
        P.dma("pool", I("dma_start", out=Wkv, in_=wkv_d), "d_wkv", writes=[rW])
        i = 0
        for g in range(4):
            c0 = g * 512
            rt = [self.rH[4 * g + j] for j in range(4)]
            for c in range(8):
                pa, ra = self.nextA()
                P.op("pe", [(I("matmul", pa[:], Wkv[:, k, c * 128:(c + 1) * 128], self.h_fm[:, k, c0:c0 + 512],
                                                    start=(k == 0), stop=(k == 7))) for k in range(8)], reads=[rW] + rt, writes=[ra])
                s = i % 2
                i += 1
                P.op("act", I("activation", out=kts[s][:], in_=pa[:], func=AF.Copy), reads=[ra], writes=[rk[s]])
                for hh in range(2):
                    self._kv_out(P.dma("sp", I("dma_start",
                        out=kt_out[2 * c + hh, :, c0:c0 + 512], in_=kts[s][hh * 64:(hh + 1) * 64, :]), f"d_ko{s}", reads=[rk[s]]), kv_res)
        for t in range(NT):
            s = t % 2
            for half in range(2):
                po, ro = self.nextO()
                P.op("pe", [(I("matmul", po[:], self.h_fm[:, k, t * 128:(t + 1) * 128], Wkv[:, k, D + half * 512:D + (half + 1) * 512],
                                                    start=(k == 0), stop=(k == 7))) for k in range(8)], reads=[rW, self.rH[t]], writes=[ro])
                P.op("act", I("activation", out=vts[s][:, half * 512:(half + 1) * 512], in_=po[:], func=AF.Copy),
                     reads=[ro], writes=[rv[s]])
            self._kv_out(P.dma("sp", I("dma_start", out=v_out[t], in_=vts[s][:]), f"d_vo{s}", reads=[rv[s]]), kv_res)

    def _kv_out(self, ticket, kv_res):
        if kv_res is None:
            self.out_tickets.append(ticket)
        else:
            r = Res()
            r.w = ticket
            kv_res.append(r)

    def decl_B(self, sfx=""):
        self.din("wq", [2, 128, 8, D])
        self.din("wo", [2, 128, 8, D])
        self.din("ffn13" + sfx, [11, 128, 8, 512])
        self.din("ffn2" + sfx, [11, 128, 2, D])
        self.din("moe13" + sfx, [NE * 7, 128, 8, 1024])
        self.din("moe2" + sfx, [NE * 7, 128, 4, D])
        self.din("router_w" + sfx, [128, 8, NE])
        self.din("router_b" + sfx, [1, NE])
        self.din("kconst", [128, 2 * TOK], BF16)
        self.din("negelig", [128, 256])
        self.din("elig01", [128, 256])
        self.din("dbase", [128, 256])
        self.din("tri", [128, 128], BF16)

    def body_B(self, yout, kv_load, sfx="", ones_col=False):
        P = self.P
        dr = self.dram
        ffn13, ffn2, moe13, moe2 = dr["ffn13" + sfx], dr["ffn2" + sfx], dr["moe13" + sfx], dr["moe2" + sfx]
        main_groups = [(g * 512, 512, [4 * g + j for j in range(4)]) for g in range(4)]
        self.arena_reset()
        tmps = self.mk_tmps()
        abuf, rab = self.alloc([128, 8, D], BF16), Res()
        self.ada_vec(2 * 6 + 1, 2, True, abuf, rab)
        self.ada_vec(2 * 6 + 0, 3, False, abuf, rab)
        for i, t in enumerate(range(NT)):
            self.h_tile(t, 2, 3, *tmps[i % 2])
        for l in (2, 3):
            b = l * 6
            self.attn_phase(l - 2, dr["wq"], dr["wo"], b + 2, l * 2 + 0, b + 4, b + 3, kv_load, ones_col)
            if l == 2:
                self.ffn_phase([(ffn13, ffn2, None)], 2, 11, main_groups, b + 5, l * 2 + 1, 18 + 1, 18 + 0)
            else:
                experts = [(moe13[e * 7:(e + 1) * 7], moe2[e * 7:(e + 1) * 7], e) for e in range(NE)]
                self.ffn_phase(experts, 4, 7, main_groups, b + 5, l * 2 + 1, None, None,
                               router=(dr["router_w" + sfx], dr["router_b" + sfx]))
        for t in range(NT):
            self.out_tickets.append(P.dma("sp", I("dma_start", out=yout[t], in_=self.x_tm[:, t, :]), "d_out", reads=[self.rX[t]]))

    def emit_B(self):
        P = self.P
        self.setup_common()
        self.decl_B()
        ktf = self.din("kt_full", [H, DH, 2 * TOK], BF16)
        vf = self.din("v_full", [H, 128, 32, 65], BF16)
        yout = self.dout("yout", [NT, 128, D])

        def kv_load(h, s, Kb, Vb, rK, rV):
            P.dma("sp", I("dma_start", out=Kb[s][0:DH, :], in_=ktf[h]), f"d_k{s}", writes=[rK[s]])
            P.dma("sp", I("dma_start", out=Vb[s], in_=vf[h]), f"d_v{s}", writes=[rV[s]])

        self.body_B(yout, kv_load)

    def attn_phase(self, j, wq_d, wo_d, g1_idx, ln_idx, next_sc, next_sh, kv_load, ones_col=False):
        P = self.P
        self.arena_reset()
        Wqh = [self.alloc([128, 8, DH], BF16) for _ in range(2)]
        rWq = [Res() for _ in range(2)]
        Kb = [self.alloc([128, 2 * TOK], BF16) for _ in range(2)]
        kb_off = self.last_off - 2 * TOK
        rK = [Res() for _ in range(2)]
        Vb = [self.alloc([128, 32, 65], BF16) for _ in range(2)]
        vb_off = self.last_off - 32 * 65
        rV = [Res() for _ in range(2)]
        qa, rqa = self.alloc([128, TOK], BF16), Res()
        attn = self.alloc([128, NT, D], BF16)
        rAt = [Res() for _ in range(NT)]
        Rst, rRst = self.alloc([128, NT, 64], BF16), Res()
        PT = [self.alloc([128, 2, 256], BF16) for _ in range(3)]
        rPT = [Res() for _ in range(3)]
        gm = self.alloc([128, NT, 16], F32)
        cur = self.alloc([128, NT, 16], F32)
        eq = self.alloc([128, NT, 16], F32)
        mx = self.alloc([128, NT], F32)
        rG = Res()
        neg = self.alloc([128, NT, 16], F32)
        el = self.alloc([128, NT, 16], F32)
        db = self.alloc([128, NT, 16], F32)
        tri = self.alloc([128, 128], BF16)
        km = self.alloc([128, 16], F32)
        kmb = self.alloc([128, 16], BF16)
        rkm = Res()
        rec = self.alloc([128, 2], F32)
        rrec = Res()
        rC = Res()
        for s in range(2):
            P.dma("sp", I("dma_start", out=Kb[s], in_=self.dram["kconst"]), "d_kc", writes=[rK[s]])
        f3 = lambda ap: ap.rearrange("p (a b) -> p a b", b=16)
        P.dma("sp", I("dma_start", out=neg, in_=f3(self.dram["negelig"])), "d_ac", writes=[rC])
        P.dma("sp", I("dma_start", out=el, in_=f3(self.dram["elig01"])), "d_ac", writes=[rC])
        P.dma("sp", I("dma_start", out=db, in_=f3(self.dram["dbase"])), "d_ac", writes=[rC])
        P.dma("sp", I("dma_start", out=tri, in_=self.dram["tri"]), "d_ac", writes=[rC])
        P.op("pool", I("memset", Rst, 0.0), writes=[rRst])
        if ones_col:
            for s_ in range(2):
                P.op("pool", I("memset", Vb[s_][:, :, DH:DH + 1], 1.0), writes=[rV[s_]])
        rHall = [self.rH[t] for t in range(NT)]
        it_s = 0
        for h in range(H):
            s = h % 2
            slope = float(2.0 ** (-8.0 * (h + 1) / H))
            s1 = float(np.float32(slope).astype(ml_dtypes.bfloat16).astype(np.float32))
            s2 = float(np.float32(slope - s1).astype(ml_dtypes.bfloat16).astype(np.float32))
            s3 = float(np.float32(slope - s1 - s2).astype(ml_dtypes.bfloat16).astype(np.float32))
            P.dma("pool", I("dma_start", out=Wqh[s], in_=wq_d[j][:, :, h * DH:(h + 1) * DH]), f"d_wq{s}", writes=[rWq[s]])
            kv_load(h, s, Kb, Vb, rK, rV)
            for g in range(4):
                pa, ra = self.nextA()
                P.op("pe", [I("matmul", pa[0:DH, :], Wqh[s][:, k, :], self.h_fm[:, k, g * 512:(g + 1) * 512], start=(k == 0), stop=(k == 7))
                            for k in range(8)], reads=[rWq[s]] + rHall[4 * g:4 * g + 4], writes=[ra])
                P.op("act", I("activation", out=qa[0:DH, g * 512:(g + 1) * 512], in_=pa[0:DH, :], func=AF.Copy, scale=float(DH ** -0.5)),
                     reads=[ra], writes=[rqa])
            P.op("dve", I("tensor_reduce", out=km[0:DH, :], in_=Kb[s][0:DH, :].rearrange("p (a b) -> p a b", b=BLK), axis=AX.X, op=ALU.add),
                 reads=[rK[s]], writes=[rkm])
            P.op("dve", I("tensor_copy", out=kmb[0:DH, :], in_=km[0:DH, :]), reads=[rkm], writes=[rkm])
            pg, rg = self.nextA()
            P.op("pe", [I("matmul", pg[:, t * 16:(t + 1) * 16], qa[0:DH, t * 128:(t + 1) * 128], kmb[0:DH, :], start=True, stop=True)
                        for t in range(NT)], reads=[rqa, rkm], writes=[rg])
            pg3 = pg[:, 0:256].rearrange("p (a b) -> p a b", b=16)
            bcast = lambda v: v.unsqueeze(2).to_broadcast([128, NT, 16])
            P.op("dve", I("tensor_tensor", out=gm, in0=pg3, in1=neg, op=ALU.add), reads=[rg, rC], writes=[rG])
            P.op("dve", I("tensor_copy", out=cur, in_=gm), reads=[rG], writes=[rG])
            for r in range(4):
                P.op("dve", I("tensor_reduce", out=mx, in_=cur, axis=AX.X, op=ALU.max), reads=[rG], writes=[rG])
                if r < 3:
                    P.op("dve", I("tensor_tensor", out=eq, in0=cur, in1=bcast(mx), op=ALU.is_ge), reads=[rG], writes=[rG])
                    P.op("dve", I("scalar_tensor_tensor", out=cur, in0=eq, scalar=-3e30, in1=cur, op0=ALU.mult, op1=ALU.add), reads=[rG], writes=[rG])
            P.op("dve", I("tensor_tensor", out=eq, in0=gm, in1=bcast(mx), op=ALU.is_ge), reads=[rG], writes=[rG])
            P.op("dve", I("tensor_tensor", out=eq, in0=eq, in1=el, op=ALU.mult), reads=[rG, rC], writes=[rG])
            P.op("dve", I("tensor_scalar", out=eq, in0=eq, scalar1=-1.0, scalar2=BIG, op0=ALU.add, op1=ALU.mult), reads=[rG], writes=[rG])
            P.op("dve", I("scalar_tensor_tensor", out=cur, in0=db, scalar=slope, in1=eq, op0=ALU.mult, op1=ALU.add), reads=[rG, rC], writes=[rG])
            P.op("dve", I("tensor_copy", out=Rst[:, :, 0:16], in_=cur), reads=[rG], writes=[rRst])
            P.op("dve", I("tensor_tensor", out=Rst[:, :, 32:48], in0=cur, in1=Rst[:, :, 0:16], op=ALU.subtract), reads=[rG, rRst], writes=[rRst])
            for ci, sv in enumerate((s1, s2, s3)):
                P.op("pool", I("memset", Rst[:, :, 16 + ci:17 + ci], sv), writes=[rRst])
            for g in range(4):
                pa, ra = self.nextA()
                P.op("pe", [I("matmul", pa[0:64, i * 128:(i + 1) * 128], Rst[:, 4 * g + i, :], self.ident[:], start=True, stop=True)
                            for i in range(4)], reads=[rRst, self.rConst], writes=[ra])
                P.op("dve", I("tensor_copy", out=qa[64:128, g * 512:(g + 1) * 512], in_=pa[0:64, :]), reads=[ra], writes=[rqa])
            items = [(b, jb) for b in range(8) for jb in range(9 + b)]
            accs = {}
            st_of = {}

            def qk_exp(i):
                nonlocal it_s
                b, jb = items[i]
                own = (jb == 8 + b)
                ps_, rs_ = self.nextA()
                S = ps_[:].rearrange("p (a b) -> p a b", b=256)
                q0 = b * 256
                fns = []
                if not own:
                    for kt in range(2):
                        fns.append(I("matmul", S[:, kt, :], Kb[s][:, (2 * jb + kt) * 128:(2 * jb + kt + 1) * 128], qa[:, q0:q0 + 256], start=True, stop=True))
                else:
                    fns.append(I("matmul", S[:, 0, :], Kb[s][:, (2 * jb) * 128:(2 * jb + 1) * 128], qa[:, q0:q0 + 256], start=True, stop=False))
                    fns.append(I("matmul", S[:, 0, 0:128], self.ident[:], tri, start=False, stop=True))
                    fns.append(I("matmul", S[:, 1, 128:256], Kb[s][:, (2 * jb + 1) * 128:(2 * jb + 2) * 128], qa[:, q0 + 128:q0 + 256], start=True, stop=False))
                    fns.append(I("matmul", S[:, 1, 128:256], self.ident[:], tri, start=False, stop=True))
                P.op("pe", fns, reads=[rK[s], rqa, rC, self.rConst], writes=[rs_])
                ts_ = it_s % 3
                it_s += 1
                st_of[i] = ts_
                if not own:
                    P.op("act", I("activation", out=PT[ts_], in_=S, func=AF.Exp), reads=[rs_], writes=[rPT[ts_]])
                else:
                    P.op("act", I("activation", out=PT[ts_][:, 0, :], in_=S[:, 0, :], func=AF.Exp), reads=[rs_], writes=[rPT[ts_]])
                    P.op("act", I("activation", out=PT[ts_][:, 1, 128:256], in_=S[:, 1, 128:256], func=AF.Exp), reads=[rs_], writes=[rPT[ts_]])

            def pv(i):
                b, jb = items[i]
                own = (jb == 8 + b)
                if jb == 0:
                    po, ro = self.nextO()
                    accs[b] = (po[:, 0:130].rearrange("p (a b) -> p a b", b=65), ro)
                acc, ro = accs[b]
                ts_ = st_of[i]
                fns = []
                for qi in range(2):
                    for kt in range(2):
                        if own and kt == 1 and qi == 0:
                            continue
                        last = own and ((qi == 0 and kt == 0) or (qi == 1 and kt == 1))
                        fns.append(I("matmul", acc[:, qi, :], PT[ts_][:, kt, qi * 128:(qi + 1) * 128], Vb[s][:, 2 * jb + kt, :],
                                     start=(jb == 0 and kt == 0), stop=last))
                P.op("pe", fns, reads=[rPT[ts_], rV[s]], writes=[ro], noself=(jb > 0))
                if own:
                    P.op("dve", I("reciprocal", out=rec, in_=acc[:, :, 64]), reads=[ro], writes=[rrec])
                    for qi in range(2):
                        t = 2 * b + qi
                        P.op("dve", I("tensor_scalar", out=attn[:, t, h * DH:(h + 1) * DH], in0=acc[:, qi, 0:64], scalar1=rec[:, qi:qi + 1],
                                      scalar2=None, op0=ALU.mult), reads=[ro, rrec], writes=[rAt[t]])

            qk_exp(0)
            for i in range(len(items)):
                if i + 1 < len(items):
                    qk_exp(i + 1)
                pv(i)
        self.P.barrier()
        Wo, rWo = self.view(kb_off, [128, 8, D], BF16), Res()
        abuf, rab = self.view(vb_off, [128, 8, 512], BF16), Res()
        self.ln_prep(ln_idx, next_sc, next_sh, abuf, rab)
        self.ada_vec(g1_idx, 4, True, abuf, rab)
        P.dma("pool", I("dma_start", out=Wo, in_=wo_d[j]), "d_wo", writes=[rWo])
        P.op("pool", I("tensor_tensor", out=Wo, in0=Wo, in1=self.bc[:, 4, :].unsqueeze(1).to_broadcast([128, 8, D]), op=ALU.mult),
             reads=[self.rBC[4]], writes=[rWo])
        self.scale_x(list(range(NT)))
        for t in range(NT):
            pt, rp = self.nextT()
            P.op("pe", [I("transpose", pt[:, k, :], attn[:, t, k * 128:(k + 1) * 128], self.ident[:]) for k in range(8)],
                 reads=[rAt[t], self.rConst], writes=[rp])
            P.op("act", I("activation", out=self.h_fm[:, :, t * 128:(t + 1) * 128], in_=pt[:], func=AF.Copy), reads=[rp], writes=[self.rH[t]])
            for half in range(2):
                po, ro = self.nextO()
                P.op("pe", [I("matmul", po[:], self.h_fm[:, c, t * 128:(t + 1) * 128], Wo[:, c, half * 512:(half + 1) * 512],
                              start=(c == 0), stop=(c == 7)) for c in range(8)], reads=[self.rH[t], rWo], writes=[ro])
                xs = self.x_tm[:, t, half * 512:(half + 1) * 512]
                P.op("dve", I("tensor_tensor", out=xs, in0=po[:], in1=xs, op=ALU.add), reads=[ro, self.rX[t]], writes=[self.rX[t]])
        self.P.barrier()
        st = Vb[0].rearrange("p a b -> p (a b)")[:, 0:(NT + 1) * 64].bitcast(F32).rearrange("p (a b) -> p a b", b=32)
        rst = [Res() for _ in range(NT + 1)]
        tf = qa.bitcast(F32)
        hb0 = PT[0].rearrange("p a b -> p (a b)")
        tmps = [(tf, Res(), attn[:, 0, :], Res()), (tf, None, attn[:, 1, :], Res())]
        tmps[1] = (tf, tmps[0][1], attn[:, 1, :], Res())
        self.ln_tiles(list(range(NT)), st, rst, tmps)

def _fm(w):
    k = w.shape[0] // 128
    return np.ascontiguousarray(w.reshape(k, 128, w.shape[1]).transpose(1, 0, 2))


def _ffn_tiles(w13, w2, F, fs):
    n_ft = F // fs
    FT = fs // 128
    a, b = w13[:, :F], w13[:, F:]
    t13 = np.stack([_fm(np.concatenate([a[:, i * fs:(i + 1) * fs], b[:, i * fs:(i + 1) * fs]], axis=1)) for i in range(n_ft)])
    t2 = np.stack([np.ascontiguousarray(w2[i * fs:(i + 1) * fs].reshape(FT, 128, D).transpose(1, 0, 2)) for i in range(n_ft)])
    return t13, t2


def _shared_common(inp):
    f32 = np.float32
    sh = {}
    pieces = []
    for l in range(DEPTH):
        for j in range(6):
            pieces.append(_fm(inp["ada_w"][l][:, j * D:(j + 1) * D]))
    for j in range(2):
        pieces.append(_fm(inp["kv_ada_w"][:, j * D:(j + 1) * D]))
    sh["adaw"] = np.stack(pieces).astype(f32)
    sh["adab"] = np.concatenate([inp["ada_b"].reshape(DEPTH * 6, D), inp["kv_ada_b"].reshape(2, D)], axis=0).astype(f32)
    sh["lng"] = np.ascontiguousarray(inp["ln_g"].reshape(8, D)).astype(f32)
    sh["lnb"] = np.ascontiguousarray(inp["ln_b"].reshape(8, D)).astype(f32)
    sh["ident_in"] = np.eye(128, dtype=f32).astype(ml_dtypes.bfloat16)
    return sh


def _ffn_moe(inp, i, sh):
    f32 = np.float32
    sh["ffn13"], sh["ffn2"] = _ffn_tiles(inp["ffn_w13"][i], inp["ffn_w2"][i], FD, 256)
    m13, m2 = [], []
    for e in range(NE):
        a, b = _ffn_tiles(inp["moe_w13"][i][e], inp["moe_w2"][i][e], FE, 512)
        m13.append(a)
        m2.append(b)
    sh["moe13"] = np.concatenate(m13, axis=0)
    sh["moe2"] = np.concatenate(m2, axis=0)
    sh["router_w"] = _fm(inp["router_w"][i]).astype(f32)
    sh["router_b"] = inp["router_b"][i].reshape(1, NE).astype(f32)


def _shared_A(inp):
    f32 = np.float32
    sh = _shared_common(inp)
    sh["conv_in_w"] = np.stack([_fm(inp["conv_in_w"][l]) for l in range(2)]).astype(f32)
    sh["conv_out_w"] = np.stack([_fm(inp["conv_out_w"][l]) for l in range(2)]).astype(f32)
    cvec = np.zeros((2, 128, 40), f32)
    for l in range(2):
        cvec[l, :, 0:16] = inp["conv_in_b"][l].reshape(16, 128).T
        cvec[l, :, 16:24] = inp["conv_dw_b"][l].reshape(8, 128).T
        cvec[l, :, 24:32] = inp["conv_norm_g"][l].reshape(8, 128).T
        cvec[l, :, 32:40] = inp["conv_norm_b"][l].reshape(8, 128).T
    sh["cvec"] = cvec
    sh["dww"] = np.stack([np.ascontiguousarray(inp["conv_dw_w"][l].reshape(CK, 8, 128).transpose(2, 1, 0)) for l in range(2)]).astype(f32)
    sh["conv_out_b"] = np.ascontiguousarray(inp["conv_out_b"]).astype(f32)
    _ffn_moe(inp, 0, sh)
    sh["wkv"] = _fm(inp["w_kv"]).astype(f32)
    return sh


def _shared_B(inp):
    f32 = np.float32
    bf = ml_dtypes.bfloat16
    sh = _shared_common(inp)
    sh["wq"] = np.stack([_fm(inp["w_q"][j]) for j in range(2)]).astype(f32)
    sh["wo"] = np.stack([_fm(inp["w_o"][j]) for j in range(2)]).astype(f32)
    _ffn_moe(inp, 1, sh)
    kc = np.zeros((128, 2 * TOK), f32)
    blk = np.arange(2 * TOK) // BLK
    pos = np.arange(2 * TOK) % BLK
    for jb in range(16):
        kc[64 + jb, blk == jb] = 1.0
        kc[96 + jb, blk == jb] = 1.0
    kc[80:83, :] = pos[None, :]
    sh["kconst"] = kc.astype(bf)
    tri = np.where(np.arange(128)[:, None] > np.arange(128)[None, :], -BIG, 0.0).astype(f32)
    sh["tri"] = tri.astype(bf)
    return sh


def _core_B(half):
    f32 = np.float32
    p = np.arange(128)[:, None, None]
    tau = np.arange(NT)[None, :, None]
    jb = np.arange(16)[None, None, :]
    b = tau // 2
    own = (jb == 8 + b)
    past = (jb < 8 + b) & ((jb >= 8) | (half == 1))
    neg = np.where(own, 1e30, np.where(past, 0.0, -1e30)) + 0.0 * p
    el = np.where(own | past, 1.0, 0.0) + 0.0 * p
    tq = TOK + tau * 128 + p
    db = -(tq - BLK * jb) + 0.0
    return {"negelig": neg.reshape(128, 256).astype(f32), "elig01": el.reshape(128, 256).astype(f32),
            "dbase": db.reshape(128, 256).astype(f32)}


def _kv_full(kt_prev, kt_own, v_prev, v_own):
    bf = ml_dtypes.bfloat16
    ktf = np.zeros((H, DH, 2 * TOK), bf)
    vf = np.zeros((H, 128, 32, 65), bf)
    vf[:, :, :, 64] = 1.0
    if kt_prev is not None:
        ktf[:, :, :TOK] = kt_prev
        vf[:, :, :16, :64] = v_prev.reshape(16, 128, H, DH).transpose(2, 1, 0, 3)
    ktf[:, :, TOK:] = kt_own
    vf[:, :, 16:, :64] = v_own.reshape(16, 128, H, DH).transpose(2, 1, 0, 3)
    return ktf, vf


def _core_common(inp, core, xsrc):
    b, half = core // 2, core % 2
    s0 = half * TOK
    m = {}
    xin = np.zeros((NT + 1, 128, D), np.float32)
    xin[:NT] = xsrc[b, s0:s0 + TOK].reshape(NT, 128, D)
    if half == 1:
        xin[NT, :HALO] = xsrc[b, s0 - HALO:s0]
    m["xin"] = xin
    m["hmask"] = np.full((128, 1), float(half), np.float32)
    m["cfm"] = np.ascontiguousarray(inp["c"][b].reshape(8, 128).T).astype(np.float32)
    return m


_NC_CACHE = {}


def _get_nc(part):
    if part not in _NC_CACHE:
        _NC_CACHE[part] = Builder(part).build()
    return _NC_CACHE[part]


def run_part_A(inp, cores):
    sh = _shared_A(inp)
    maps = []
    for core in cores:
        m = dict(sh)
        m.update(_core_common(inp, core, inp["x"]))
        maps.append(m)
    nc = _get_nc("A")
    res = run_bass_kernel_spmd(nc, maps, core_ids=list(range(len(cores))))
    return res.results


def run_part_B(inp, cores, x1, kts, vs):
    sh = _shared_B(inp)
    maps = []
    for core in cores:
        half = core % 2
        m = dict(sh)
        m.update(_core_common(inp, core, x1))
        m.update(_core_B(half))
        if half == 1:
            ktf, vf = _kv_full(kts[core - 1], kts[core], vs[core - 1], vs[core])
        else:
            ktf, vf = _kv_full(None, kts[core], None, vs[core])
        m["kt_full"], m["v_full"] = ktf, vf
        maps.append(m)
    nc = _get_nc("B")
    res = run_bass_kernel_spmd(nc, maps, core_ids=list(range(len(cores))))
    return res.results


def run_fused(inp, cores):
    shA = _shared_A(inp)
    shB = _shared_B(inp)
    sh = dict(shA)
    for k, v in shB.items():
        if k in ("ffn13", "ffn2", "moe13", "moe2", "router_w", "router_b"):
            sh[k + "b"] = v
        else:
            sh[k] = v
    maps = []
    for core in cores:
        b, half = core // 2, core % 2
        m = dict(sh)
        m.update(_core_common(inp, core, inp["x"]))
        m.update(_core_B(half))
        xp = np.zeros((NT + 1, 128, D), np.float32)
        if half == 1:
            xp[:NT] = inp["x"][b, 0:TOK].reshape(NT, 128, D)
        m["xin_prev"] = xp
        maps.append(m)
    nc = _get_nc("F")
    res = run_bass_kernel_spmd(nc, maps, core_ids=list(range(len(cores))))
    return res.results


MODE = "AB"


def kernel(**inputs):
    inp = {k: np.asarray(v) for k, v in inputs.items()}
    cores = list(range(NCORES))
    out = np.zeros((NB, SEQ, D), np.float32)
    if MODE == "F":
        res = run_fused(inp, cores)
        for c in cores:
            b, half = c // 2, c % 2
            out[b, half * TOK:(half + 1) * TOK] = np.asarray(res[c]["yout"]).reshape(TOK, D)
        return out
    resA = run_part_A(inp, cores)
    x1 = np.zeros((NB, SEQ, D), np.float32)
    kts, vs = {}, {}
    for c in cores:
        b, half = c // 2, c % 2
        x1[b, half * TOK:(half + 1) * TOK] = np.asarray(resA[c]["xout"]).reshape(TOK, D)
        kts[c] = np.asarray(resA[c]["kt_out"])
        vs[c] = np.asarray(resA[c]["v_out"]).reshape(TOK, D)
    resB = run_part_B(inp, cores, x1, kts, vs)
    for c in cores:
        b, half = c // 2, c % 2
        out[b, half * TOK:(half + 1) * TOK] = np.asarray(resB[c]["yout"]).reshape(TOK, D)
    return out
```
